# Optimizing a Trainium2 kernel written in Bass

```python
import jax
import jax.numpy as jnp
from jax import lax
import numpy as np

D_MODEL = 1024
BATCH = 4
SEQ = 8192
DEPTH = 1

GRID_W = 64
CTX_LEN = 256

NA_HEADS = 8
NA_HEAD_DIM = 64
NA_WIDTH = NA_HEADS * NA_HEAD_DIM
WIN_H = 8
WIN_W = 16
ROPE_BASE = 10000.0

RW_HEADS = 8
RW_HEAD_DIM = 64
RW_WIDTH = RW_HEADS * RW_HEAD_DIM
DECAY_LORA = 64
AAA_LORA = 64
GATE_LORA = 128
RW_COLS = 3 * RW_WIDTH + 2 * DECAY_LORA + 2 * AAA_LORA + GATE_LORA
RW_SPLITS = (RW_WIDTH, 2 * RW_WIDTH, 3 * RW_WIDTH,
             3 * RW_WIDTH + DECAY_LORA, 3 * RW_WIDTH + 2 * DECAY_LORA,
             3 * RW_WIDTH + 2 * DECAY_LORA + AAA_LORA, 3 * RW_WIDTH + 2 * DECAY_LORA + 2 * AAA_LORA)

GATE_COLS = 2 * D_MODEL
P_IN = 3 * NA_WIDTH + RW_COLS + GATE_COLS

N_EXPERTS = 16
CAPACITY_FACTOR = 2
D_EXPERT = 2816

ALPHA = (2.0 * DEPTH) ** 0.25
BETA = (8.0 * DEPTH) ** -0.25
LN_EPS = 1e-6
GN_EPS = 64e-5

kernel_name = 'hybrid_natten_rwkv7_ecmoe_dit_block'


def layer_norm(x, gain=None, bias=None, eps=LN_EPS):
    xf = x.astype(jnp.float32)
    mu = jnp.mean(xf, axis=-1, keepdims=True)
    var = jnp.mean(jnp.square(xf - mu), axis=-1, keepdims=True)
    y = (xf - mu) * lax.rsqrt(var + eps)
    if gain is not None:
        y = y * gain.astype(jnp.float32) + bias.astype(jnp.float32)
    return y.astype(x.dtype)


def modulate(h, shift, scale):
    return layer_norm(h) * (1 + scale) + shift


def axial_rope(x):
    B, N, H, Dh = x.shape
    nf = Dh // 4
    t = jnp.arange(N)
    pos = jnp.stack([t // GRID_W, t % GRID_W], axis=-1).astype(jnp.float32)
    inv_freq = jnp.power(jnp.float32(ROPE_BASE), -jnp.arange(nf, dtype=jnp.float32) / nf)
    ang = pos[:, :, None] * inv_freq
    cos = jnp.cos(ang)[None, :, None]
    sin = jnp.sin(ang)[None, :, None]
    xr = x.astype(jnp.float32).reshape(B, N, H, 2, 2, nf)
    x1, x2 = xr[..., 0, :], xr[..., 1, :]
    out = jnp.stack([x1 * cos - x2 * sin, x2 * cos + x1 * sin], axis=-2)
    return out.reshape(B, N, H, Dh).astype(x.dtype)


def token_shift(z, mu_prev, mu_next):
    z_prev = jnp.pad(z[:, :-1], ((0, 0), (1, 0), (0, 0)))
    z_next = jnp.pad(z[:, 1:], ((0, 0), (0, 1), (0, 0)))
    return z + mu_prev * (z_prev - z) + mu_next * (z_next - z)


def neighbourhood_attention(q, k, v, k_ctx, v_ctx, rpb):
    B, H, N, Dh = q.shape
    rows = N // GRID_W
    kh = min(WIN_H, rows)
    scale = Dh ** -0.5
    grid = lambda t: t.reshape(B, H, rows, GRID_W, Dh)
    q, k, v = grid(q), grid(k), grid(v)
    col = jnp.arange(GRID_W)
    col_start = jnp.clip(col - WIN_W // 2, 0, GRID_W - WIN_W)
    col_idx = col_start[:, None] + jnp.arange(WIN_W)[None, :]
    col_off = col_idx - col[:, None] + (WIN_W - 1)

    def row_block(i):
        rs = jnp.clip(i - kh // 2, 0, rows - kh)
        qi = lax.dynamic_index_in_dim(q, i, axis=2, keepdims=False)
        kb = lax.dynamic_slice_in_dim(k, rs, kh, axis=2)[:, :, :, col_idx]
        vb = lax.dynamic_slice_in_dim(v, rs, kh, axis=2)[:, :, :, col_idx]
        row_off = rs + jnp.arange(kh) - i + (WIN_H - 1)
        bias = jnp.transpose(rpb[:, row_off][:, :, col_off], (0, 2, 1, 3))
        s_loc = jnp.einsum('bhqd,bhrqcd->bhqrc', qi, kb).astype(jnp.float32) * scale + bias.astype(jnp.float32)
        s_ctx = jnp.einsum('bhqd,bhld->bhql', qi, k_ctx).astype(jnp.float32) * scale
        s = jnp.concatenate([s_loc.reshape(B, H, GRID_W, kh * WIN_W), s_ctx], axis=-1)
        p = jax.nn.softmax(s, axis=-1).astype(v.dtype)
        p_loc = p[..., :kh * WIN_W].reshape(B, H, GRID_W, kh, WIN_W)
        return (jnp.einsum('bhqrc,bhrqcd->bhqd', p_loc, vb)
                + jnp.einsum('bhql,bhld->bhqd', p[..., kh * WIN_W:], v_ctx))

    out = lax.map(row_block, jnp.arange(rows))
    return jnp.transpose(out, (1, 0, 3, 2, 4)).reshape(B, N, H * Dh)


def context_attention(qc, kc, vc):
    B, H, L, Dh = qc.shape
    s = jnp.einsum('bhqd,bhkd->bhqk', qc, kc).astype(jnp.float32) * Dh ** -0.5
    p = jax.nn.softmax(s, axis=-1).astype(vc.dtype)
    o = jnp.einsum('bhqk,bhkd->bhqd', p, vc)
    return jnp.transpose(o, (0, 2, 1, 3)).reshape(B, L, H * Dh)


def rwkv_features(zr, p):
    B, T, _ = zr.shape
    hd = lambda t: t.reshape(B, T, RW_HEADS, RW_HEAD_DIM)
    r, k, v, wdf, wdb, adf, adb, gd = jnp.split(zr, RW_SPLITS, axis=-1)
    kkf = hd(k * p['k_k']).astype(jnp.float32)
    kk = (kkf * lax.rsqrt(jnp.sum(jnp.square(kkf), axis=-1, keepdims=True) + 1e-12)).astype(k.dtype)
    dirs = []
    for d, (wd, ad) in enumerate(((wdf, adf), (wdb, adb))):
        w_raw = (p['w0'][d] + jnp.tanh(wd) @ p['w_up'][d]).astype(jnp.float32)
        decay = jnp.exp(-jnp.exp(-jax.nn.softplus(-w_raw) - 0.5))
        a = jax.nn.sigmoid(p['a0'][d] + ad @ p['a_up'][d])
        kmod = k * (1 + (a - 1) * p['k_a'])
        dirs.append((hd(decay), hd(kmod), hd(a)))
    g = jax.nn.sigmoid(gd) @ p['g_up']
    return hd(r), hd(v), kk, dirs, g


def rwkv_scan(r, decay, k, a, v, kk, S0, reverse, emit):
    xs = tuple(jnp.moveaxis(t.astype(jnp.float32), 1, 0) for t in (r, decay, k, a, v, kk))

    def step(S, inp):
        r_t, w_t, k_t, a_t, v_t, kk_t = inp
        sa = jnp.einsum('bhvk,bhk->bhv', S, -kk_t)
        S = (S * w_t[:, :, None, :] + sa[..., None] * (kk_t * a_t)[:, :, None, :]
             + v_t[..., None] * k_t[:, :, None, :])
        y = jnp.einsum('bhvk,bhk->bhv', S, r_t) if emit else None
        return S, y

    S, ys = lax.scan(step, S0, xs, reverse=reverse)
    return S, (jnp.moveaxis(ys, 0, 1) if emit else None)


def rwkv_readout(y_sum, r, v, kmods, g, p):
    B, T = r.shape[:2]
    gn_w = p['gn_w'].reshape(RW_HEADS, RW_HEAD_DIM)
    gn_b = p['gn_b'].reshape(RW_HEADS, RW_HEAD_DIM)
    y = layer_norm(y_sum, gn_w, gn_b, GN_EPS).astype(r.dtype)
    for km in kmods:
        y = y + jnp.sum(r * km * p['r_k'], axis=-1, keepdims=True) * v
    return y.reshape(B, T, RW_WIDTH) * g


def merge_branches(y_na, y_rw, z_gate, p):
    gate_na, gate_rw = jnp.split(jax.nn.sigmoid(z_gate), 2, axis=-1)
    return (gate_na * (y_na @ p['w_pa']) + gate_rw * (y_rw @ p['w_pr'])) @ p['w_o']


def token_mixer(u, uc, p, emit_ctx):
    B = u.shape[0]
    cut = [3 * NA_WIDTH, 3 * NA_WIDTH + RW_COLS]
    z_na, z_rw, z_gate = jnp.split(u @ p['w_in'], cut, axis=-1)
    zc_na, zc_rw, zc_gate = jnp.split(uc @ p['w_in'], cut, axis=-1)

    heads = lambda t: t.reshape(t.shape[0], t.shape[1], NA_HEADS, NA_HEAD_DIM)
    bhnd = lambda t: jnp.transpose(t, (0, 2, 1, 3))
    q, k, v = (heads(t) for t in jnp.split(z_na, 3, axis=-1))
    q, k = axial_rope(q), axial_rope(k)
    qc, kc, vc = (bhnd(heads(t)) for t in jnp.split(zc_na, 3, axis=-1))
    y_na = neighbourhood_attention(bhnd(q), bhnd(k), bhnd(v), kc, vc, p['rpb'])

    r, vr, kk, dirs, g = rwkv_features(token_shift(z_rw, p['mu_prev'], p['mu_next']), p)
    rc, vrc, kkc, dirs_c, gc = rwkv_features(token_shift(zc_rw, p['mu_prev'], p['mu_next']), p)
    ys, ycs = [], []
    for d in range(2):
        rev = d == 1
        S0 = jnp.zeros((B, RW_HEADS, RW_HEAD_DIM, RW_HEAD_DIM), jnp.float32)
        S_ctx, y_c = rwkv_scan(rc, *dirs_c[d], vrc, kkc, S0, rev, emit_ctx)
        _, y_l = rwkv_scan(r, *dirs[d], vr, kk, S_ctx, rev, True)
        ys.append(y_l)
        ycs.append(y_c)
    y_rw = rwkv_readout(ys[0] + ys[1], r, vr, [dirs[0][1], dirs[1][1]], g, p)
    out = merge_branches(y_na, y_rw, z_gate, p)

    out_c = None
    if emit_ctx:
        yc_na = context_attention(qc, kc, vc)
        yc_rw = rwkv_readout(ycs[0] + ycs[1], rc, vrc, [dirs_c[0][1], dirs_c[1][1]], gc, p)
        out_c = merge_branches(yc_na, yc_rw, zc_gate, p)
    return out, out_c


def expert_choice_ffn(u, w_router, w1, w3, w2):
    B, T, D = u.shape
    cap = CAPACITY_FACTOR * T // N_EXPERTS
    aff = jax.nn.softmax((u @ w_router).astype(jnp.float32), axis=-1)
    gate, idx = lax.top_k(jnp.swapaxes(aff, 1, 2), cap)
    xs = jax.vmap(lambda ub, ib: ub[ib])(u, idx)
    hdn = jax.nn.silu(jnp.einsum('becd,edf->becf', xs, w1)) * jnp.einsum('becd,edf->becf', xs, w3)
    ye = jnp.einsum('becf,efd->becd', hdn, w2) * gate[..., None].astype(u.dtype)
    return jax.vmap(lambda ib, yb: jnp.zeros((T, D), yb.dtype).at[ib.reshape(-1)].add(yb.reshape(-1, D)))(idx, ye)


def setup_inputs(seed: int = 0) -> dict:
    key = jax.random.key(seed)
    keys = iter(jax.random.split(key, 40))
    D = D_MODEL

    def nrm(shape, s):
        return s * jax.random.normal(next(keys), shape, jnp.float32)

    def unif(shape, hi):
        return jax.random.uniform(next(keys), shape, jnp.float32, 0.0, hi)

    speed = -7.0 + 5.0 * (jnp.arange(RW_WIDTH, dtype=jnp.float32) / (RW_WIDTH - 1)) ** 0.85
    return {
        'x': nrm((BATCH, SEQ, D), 1.0),
        'c': nrm((BATCH, D), 1.0),
        'ctx': nrm((BATCH, CTX_LEN, D), 1.0),
        'c_ctx': nrm((D,), 1.0),
        'w_mod': nrm((DEPTH, D, 6 * D), 0.5 * D ** -0.5),
        'b_mod': nrm((DEPTH, 6 * D), 0.02),
        'w_in': nrm((DEPTH, D, P_IN), D ** -0.5),
        'rpb': nrm((DEPTH, NA_HEADS, 2 * WIN_H - 1, 2 * WIN_W - 1), 0.5),
        'mu_prev': unif((DEPTH, RW_COLS), 0.5),
        'mu_next': unif((DEPTH, RW_COLS), 0.5),
        'w0': speed + 0.5 + nrm((DEPTH, 2, RW_WIDTH), 0.1),
        'w_up': nrm((DEPTH, 2, DECAY_LORA, RW_WIDTH), 0.1 * DECAY_LORA ** -0.5),
        'a0': nrm((DEPTH, 2, RW_WIDTH), 0.1),
        'a_up': nrm((DEPTH, 2, AAA_LORA, RW_WIDTH), 0.5 * AAA_LORA ** -0.5),
        'g_up': nrm((DEPTH, GATE_LORA, RW_WIDTH), GATE_LORA ** -0.5),
        'k_k': 0.85 + nrm((DEPTH, RW_WIDTH), 0.02),
        'k_a': 1.0 + nrm((DEPTH, RW_WIDTH), 0.02),
        'r_k': nrm((DEPTH, RW_HEADS, RW_HEAD_DIM), 0.1),
        'gn_w': 1.0 + nrm((DEPTH, RW_WIDTH), 0.02),
        'gn_b': nrm((DEPTH, RW_WIDTH), 0.02),
        'w_pa': nrm((DEPTH, NA_WIDTH, D), NA_WIDTH ** -0.5),
        'w_pr': nrm((DEPTH, RW_WIDTH, D), RW_WIDTH ** -0.5),
        'w_o': nrm((DEPTH, D, D), BETA * D ** -0.5),
        'ln1_g': 1.0 + nrm((DEPTH, D), 0.02),
        'ln1_b': nrm((DEPTH, D), 0.02),
        'w_router': nrm((DEPTH, D, N_EXPERTS), D ** -0.5),
        'w_e1': nrm((DEPTH, N_EXPERTS, D, D_EXPERT), D ** -0.5),
        'w_e3': nrm((DEPTH, N_EXPERTS, D, D_EXPERT), D ** -0.5),
        'w_e2': nrm((DEPTH, N_EXPERTS, D_EXPERT, D), BETA * D_EXPERT ** -0.5),
        'ln2_g': 1.0 + nrm((DEPTH, D), 0.02),
        'ln2_b': nrm((DEPTH, D), 0.02),
    }


def reference(x, c, ctx, c_ctx, w_mod, b_mod, w_in, rpb, mu_prev, mu_next, w0, w_up, a0, a_up, g_up,
              k_k, k_a, r_k, gn_w, gn_b, w_pa, w_pr, w_o, ln1_g, ln1_b, w_router, w_e1, w_e3, w_e2,
              ln2_g, ln2_b):
    h, hc = x, ctx
    for l in range(DEPTH):
        emit_ctx = l < DEPTH - 1
        p = {'w_in': w_in[l], 'rpb': rpb[l], 'mu_prev': mu_prev[l], 'mu_next': mu_next[l],
             'w0': w0[l], 'w_up': w_up[l], 'a0': a0[l], 'a_up': a_up[l], 'g_up': g_up[l],
             'k_k': k_k[l], 'k_a': k_a[l], 'r_k': r_k[l], 'gn_w': gn_w[l], 'gn_b': gn_b[l],
             'w_pa': w_pa[l], 'w_pr': w_pr[l], 'w_o': w_o[l]}
        mod = jax.nn.silu(c) @ w_mod[l] + b_mod[l]
        mod_c = jax.nn.silu(c_ctx) @ w_mod[l] + b_mod[l]
        sh1, sc1, gt1, sh2, sc2, gt2 = jnp.split(mod[:, None, :], 6, axis=-1)
        csh1, csc1, cgt1, csh2, csc2, cgt2 = jnp.split(mod_c, 6, axis=-1)

        m, mc = token_mixer(modulate(h, sh1, sc1), modulate(hc, csh1, csc1), p, emit_ctx)
        h = layer_norm(ALPHA * h + gt1 * m, ln1_g[l], ln1_b[l])
        moe = expert_choice_ffn(modulate(h, sh2, sc2), w_router[l], w_e1[l], w_e3[l], w_e2[l])
        h = layer_norm(ALPHA * h + gt2 * moe, ln2_g[l], ln2_b[l])
        if emit_ctx:
            hc = layer_norm(ALPHA * hc + cgt1 * mc, ln1_g[l], ln1_b[l])
            moe_c = expert_choice_ffn(modulate(hc, csh2, csc2), w_router[l], w_e1[l], w_e3[l], w_e2[l])
            hc = layer_norm(ALPHA * hc + cgt2 * moe_c, ln2_g[l], ln2_b[l])
    return h
```

```python
import numpy as np
import ml_dtypes
import concourse.bass as bass
import concourse.mybir as mybir
from concourse.bass_utils import run_bass_kernel_spmd

F32 = mybir.dt.float32
BF16 = mybir.dt.bfloat16
AF = mybir.ActivationFunctionType
ALU = mybir.AluOpType
AX = mybir.AxisListType

D = 1024
T = 8192
L = 256
TT = T + L
NT = T // 128
NTT = TT // 128
P_IN = 5504
RW0 = 1536
G0 = 3456
NE = 16
DE = 2816
CAP = 1024
ALPHA = 2.0 ** 0.25
LN_EPS = 1e-6
GN_EPS = 64e-5


class Buf:
    __slots__ = ("name", "t", "lw", "rs")

    def __init__(self, name, t):
        self.name, self.t, self.lw, self.rs = name, t, None, {}

    def __getitem__(self, idx):
        return self.t[idx]


class Sched:
    NDMA = 24

    def __init__(self, nc):
        self.nc = nc
        self.eng = {"pe": nc.tensor, "dve": nc.vector, "act": nc.scalar, "pool": nc.gpsimd, "sp": nc.sync}
        self.sem = {k: nc.alloc_semaphore(name=f"s_{k}") for k in self.eng}
        self.cnt = {k: 0 for k in self.eng}
        self.dsem = [nc.alloc_semaphore(name=f"d_{i}") for i in range(self.NDMA)]
        self.dcnt = [0] * self.NDMA
        self.dnext = 0
        self.semobj = dict(self.sem)
        for i, s in enumerate(self.dsem):
            self.semobj[("d", i)] = s
        self.seen = {k: {} for k in self.eng}
        self.ninst = 0
        self.nwaits = 0

    def _wait(self, e, key, val, same_ok=False):
        if key == e and same_ok:
            return
        if self.seen[e].get(key, 0) >= val:
            return
        self.eng[e].wait_ge(self.semobj[key], val)
        self.seen[e][key] = val
        self.nwaits += 1

    def _deps(self, e, reads, writes, pe_acc=False):
        for r in reads:
            if r.lw is not None:
                self._wait(e, r.lw[0], r.lw[1])
        for w in writes:
            if w.lw is not None:
                self._wait(e, w.lw[0], w.lw[1], same_ok=(pe_acc and e == "pe"))
            for k, v in w.rs.items():
                self._wait(e, k, v, same_ok=(k == e and e == "pe"))

    def _mark(self, key, val, reads, writes):
        for r in reads:
            r.rs[key] = val
        for w in writes:
            w.lw = (key, val)
            w.rs = {}

    def op(self, e, fn, reads=(), writes=(), pe_acc=False):
        self._deps(e, reads, writes, pe_acc)
        ins = fn(self.eng[e])
        self.cnt[e] += 1
        ins.then_inc(self.sem[e], 1)
        self._mark(e, self.cnt[e], reads, writes)
        self.ninst += 1
        return ins

    def dma(self, e, fn, reads=(), writes=()):
        i = self.dnext
        self.dnext = (self.dnext + 1) % self.NDMA
        key = ("d", i)
        if self.dcnt[i] > 0:
            self._wait(e, key, self.dcnt[i])
        self._deps(e, reads, writes)
        ins = fn(self.eng[e])
        self.dcnt[i] += 16
        ins.then_inc(self.dsem[i], 16)
        self._mark(key, self.dcnt[i], reads, writes)
        self.ninst += 1
        return ins

    def finish(self, e="sp"):
        for i in range(self.NDMA):
            if self.dcnt[i] > 0:
                self._wait(e, ("d", i), self.dcnt[i])
        for k in self.eng:
            if k != e and self.cnt[k] > 0:
                self._wait(e, k, self.cnt[k])


class Ctx:
    def __init__(self, nc):
        self.nc = nc
        self.S = Sched(nc)
        self.stack = []
        self.uid = 0

    def sb(self, name, shape, dt=F32):
        self.uid += 1
        cm = self.nc.sbuf_tensor(f"{name}_{self.uid}", list(shape), dt)
        t = cm.__enter__()
        self.stack.append(cm)
        return Buf(name, t)

    def ps(self, name, shape, dt=F32):
        self.uid += 1
        cm = self.nc.psum_tensor(f"{name}_{self.uid}", list(shape), dt)
        t = cm.__enter__()
        self.stack.append(cm)
        return Buf(name, t)

    def dram(self, name, shape, dt=F32):
        return Buf(name, self.nc.dram_tensor(name, list(shape), dt).ap())

    def mark(self):
        return len(self.stack)

    def release(self, m):
        self.barrier()
        while len(self.stack) > m:
            self.stack.pop().__exit__(None, None, None)

    def barrier(self):
        S = self.S
        for e in ("sp", "pe", "dve", "act", "pool"):
            S.finish(e)


def rr(lst, i):
    return lst[i % len(lst)]


def build_program(stop_after=None, dbg=None):
    nc = bass.Bass("TRN2", target_bir_lowering=False)
    C = Ctx(nc)
    S = C.S
    inp = lambda n, s, dt=F32: Buf(n, nc.dram_tensor(n, list(s), dt, kind="ExternalInput").ap())
    xin = inp("x", [TT, D])
    ccin = inp("cc", [2, D])
    w_mod = inp("w_mod", [D, 6 * D]); b_mod = inp("b_mod", [6 * D])
    w_in = inp("w_in", [D, P_IN])
    ident_d = inp("ident", [128, 128])
    rope_d = inp("rope", [T, 2, 2, 16])
    tri_d = inp("tri", [6, 128, 128])
    rwp = {k: inp(k, shp) for k, shp in [("mu_prev", [1920]), ("mu_next", [1920]), ("k_k", [512]), ("k_a", [512]), ("r_k", [512]),
                                          ("gn_w", [512]), ("gn_b", [512]), ("w0", [2, 512]), ("a0", [2, 512]),
                                          ("w_up", [128, 512]), ("a_up", [128, 512]), ("g_up", [128, 512])]}
    wD = None if stop_after not in (None, "F_short") else {k: inp(k, shp) for k, shp in [("w_pa", [512, D]), ("w_pr", [512, D]), ("w_o", [D, D]), ("w_router", [D, NE]),
                                        ("ln1_g", [D]), ("ln1_b", [D]), ("ln2_g", [D]), ("ln2_b", [D]),
                                        ("w_e1", [NE, D, DE]), ("w_e3", [NE, D, DE]), ("w_e2", [NE, DE, D])]}
    nab_d = inp("nab", [8, 128, 8, 256])
    out_d = Buf("out", nc.dram_tensor("out", [T, D], F32, kind="ExternalOutput").ap())
    zscr = C.dram("zscr", [TT, P_IN])
    modscr = C.dram("modscr", [2, 6 * D])

    ident = C.sb("ident", [128, 128]); identb = C.sb("identb", [128, 128], BF16)
    S.dma("sp", lambda e: e.dma_start(out=ident[:], in_=ident_d[:, :]), writes=[ident])
    S.op("dve", lambda e: e.tensor_copy(out=identb[:], in_=ident[:]), reads=[ident], writes=[identb])

    if stop_after == "F_short":
        ynascr = C.dram("ynascr", [T, 512]); yrwscr = C.dram("yrwscr", [T, 512])
        build_phase_def(nc, C, S, xin, zscr, modscr, ynascr, yrwscr, wD, identb, ident, out_d, stop_after)
        C.barrier()
        print("ninst", S.ninst, "nwaits", S.nwaits)
        return nc
    m0 = C.mark()
    scol = C.sb("scol", [128, 2, 8]); modrow = C.sb("modrow", [2, 6 * D]); brow = C.sb("brow", [2, 6 * D])
    S.dma("sp", lambda e: e.dma_start(out=scol[:], in_=ccin.t.rearrange("r (kc p) -> p r kc", p=128),
                                      allow_slow_non_contiguous=True), writes=[scol])
    S.dma("sp", lambda e: e.dma_start(out=brow[:], in_=b_mod.t.partition_broadcast(2)), writes=[brow])
    S.op("act", lambda e: e.activation(out=scol[:], in_=scol[:], func=AF.Silu), reads=[scol], writes=[scol])
    wm = [C.sb("wm", [128, 8, 512]) for _ in range(2)]
    pm = [C.ps("pm", [2, 512]) for _ in range(2)]
    wmv = w_mod.t.rearrange("(kc p) n -> p kc n", p=128)
    for cb in range(12):
        w, p = rr(wm, cb), rr(pm, cb)
        S.dma("sp", lambda e: e.dma_start(out=w[:], in_=wmv[:, :, cb * 512:(cb + 1) * 512]), writes=[w])
        for kc in range(8):
            S.op("pe", lambda e: e.matmul(p[:], lhsT=scol[:, :, kc], rhs=w[:, kc, :], start=(kc == 0), stop=(kc == 7)),
                 reads=[scol, w], writes=[p], pe_acc=(kc > 0))
        S.op("dve", lambda e: e.tensor_tensor(out=modrow[:, cb * 512:(cb + 1) * 512], in0=p[:], in1=brow[:, cb * 512:(cb + 1) * 512], op=ALU.add),
             reads=[p, brow], writes=[modrow])
    for sec in (1, 4):
        S.op("dve", lambda e: e.tensor_scalar_add(out=modrow[:, sec * D:(sec + 1) * D], in0=modrow[:, sec * D:(sec + 1) * D], scalar1=1.0),
             reads=[modrow], writes=[modrow])
    S.dma("sp", lambda e: e.dma_start(out=modscr[:, :], in_=modrow[:]), reads=[modrow], writes=[modscr])
    C.release(m0)

    def bcast(name, src_buf, src_ap, n=D, eng="sp"):
        b = C.sb(name, [128, n])
        S.dma(eng, lambda e: e.dma_start(out=b[:], in_=src_ap.partition_broadcast(128)), reads=[src_buf], writes=[b])
        return b

    def layer_norm_stats(xt, stats, mv, rstd, eps):
        for hf in range(2):
            S.op("dve", lambda e: e.bn_stats(out=stats[:, hf, :], in_=xt[:, hf * 512:(hf + 1) * 512]), reads=[xt], writes=[stats])
        S.op("dve", lambda e: e.bn_aggr(out=mv[:], in_=stats[:]), reads=[stats], writes=[mv])
        S.op("dve", lambda e: e.tensor_scalar_add(out=rstd[:], in0=mv[:, 1:2], scalar1=eps), reads=[mv], writes=[rstd])
        S.op("act", lambda e: e.activation(out=rstd[:], in_=rstd[:], func=AF.Ln), reads=[rstd], writes=[rstd])
        S.op("act", lambda e: e.activation(out=rstd[:], in_=rstd[:], func=AF.Exp, scale=-0.5), reads=[rstd], writes=[rstd])

    mA = C.mark()
    win_b = C.sb("win_b", [128, 8, P_IN], BF16)
    wst = [C.sb("wst", [128, 8, 512]) for _ in range(2)]
    winv = w_in.t.rearrange("(kc p) n -> p kc n", p=128)
    for cb in range(11):
        c0, c1 = cb * 512, min(P_IN, (cb + 1) * 512)
        w = rr(wst, cb)
        S.dma("sp", lambda e: e.dma_start(out=w[:, :, 0:c1 - c0], in_=winv[:, :, c0:c1]), writes=[w])
        S.op(rr(["dve", "pool"], cb), lambda e: e.tensor_copy(out=win_b[:, :, c0:c1], in_=w[:, :, 0:c1 - c0]), reads=[w], writes=[win_b])
    scA = [bcast("sc1", modscr, modscr[0, 1 * D:2 * D]), bcast("csc1", modscr, modscr[1, 1 * D:2 * D])]
    shA = [bcast("sh1", modscr, modscr[0, 0:D]), bcast("csh1", modscr, modscr[1, 0:D])]
    xt = [C.sb("xt", [128, D]) for _ in range(2)]
    ub = [C.sb("ub", [128, D], BF16) for _ in range(2)]
    uT = [C.sb("uT", [128, 8, 128], BF16) for _ in range(2)]
    zt = [C.sb("zt", [128, P_IN]) for _ in range(2)]
    stats = C.sb("stats", [128, 2, 6]); mv = C.sb("mv", [128, 2]); rstd = C.sb("rstd", [128, 1])
    rope = [C.sb("rope", [128, 2, 2, 16]) for _ in range(2)]
    rt = [C.sb("rt", [128, 16, 2, 16]) for _ in range(4)]
    ptp = [C.ps("ptp", [128, 8, 128], BF16) for _ in range(2)]
    pz = [C.ps("pz", [128, 512]) for _ in range(4)]
    tilesA = list(range(NTT))
    if stop_after == "A_short":
        tilesA = [0, 64]
    if stop_after == "B_short":
        tilesA = [0, 1, 2, 3, 4, 60, 61, 62, 63, 64, 65]
    if stop_after == "C_short":
        tilesA = [0, 1, 2, 61, 62, 63, 64, 65]
    if stop_after in ("C_feat", "C_one"):
        tilesA = [0, 1, 62, 63]
    for it, n in enumerate(tilesA):
        isctx = n >= NT
        x_, u_, uT_, z_, pt_ = rr(xt, it), rr(ub, it), rr(uT, it), rr(zt, it), rr(ptp, it)
        sc, sh = scA[isctx], shA[isctx]
        S.dma("sp", lambda e: e.dma_start(out=x_[:], in_=xin[n * 128:(n + 1) * 128, :]), reads=[xin], writes=[x_])
        layer_norm_stats(x_, stats, mv, rstd, LN_EPS)
        S.op("dve", lambda e: e.tensor_scalar(out=x_[:], in0=x_[:], scalar1=mv[:, 0:1], scalar2=rstd[:, 0:1], op0=ALU.subtract, op1=ALU.mult),
             reads=[x_, mv, rstd], writes=[x_])
        S.op("pool", lambda e: e.tensor_tensor(out=x_[:], in0=x_[:], in1=sc[:], op=ALU.mult), reads=[x_, sc], writes=[x_])
        S.op("pool", lambda e: e.tensor_tensor(out=u_[:], in0=x_[:], in1=sh[:], op=ALU.add), reads=[x_, sh], writes=[u_])
        for kc in range(8):
            S.op("pe", lambda e: e.transpose(out=pt_[:, kc, :], in_=u_[:, kc * 128:(kc + 1) * 128], identity=identb[:]),
                 reads=[u_, identb], writes=[pt_], pe_acc=True)
        S.op("act", lambda e: e.copy(out=uT_[:], in_=pt_[:]), reads=[pt_], writes=[uT_])
        for cb in range(11):
            c0, c1 = cb * 512, min(P_IN, (cb + 1) * 512)
            p = rr(pz, cb)
            for kc in range(8):
                S.op("pe", lambda e: e.matmul(p[:, 0:c1 - c0], lhsT=uT_[:, kc, :], rhs=win_b[:, kc, c0:c1], start=(kc == 0), stop=(kc == 7)),
                     reads=[uT_, win_b], writes=[p], pe_acc=(kc > 0))
            if cb % 2 == 0:
                S.op("act", lambda e: e.copy(out=z_[:, c0:c1], in_=p[:, 0:c1 - c0]), reads=[p], writes=[z_])
            else:
                S.op("dve", lambda e: e.tensor_copy(out=z_[:, c0:c1], in_=p[:, 0:c1 - c0]), reads=[p], writes=[z_])
        if not isctx:
            rp = rr(rope, it)
            S.dma("sp", lambda e: e.dma_start(out=rp[:], in_=rope_d[n * 128:(n + 1) * 128]), reads=[rope_d], writes=[rp])
            qk = z_.t[:, 0:1024].rearrange("p (h a b f) -> p h a b f", h=16, a=2, b=2)
            x1, x2 = qk[:, :, :, 0, :], qk[:, :, :, 1, :]
            cosb = rp.t[:, 0, :, :].unsqueeze(1).to_broadcast([128, 16, 2, 16])
            sinb = rp.t[:, 1, :, :].unsqueeze(1).to_broadcast([128, 16, 2, 16])
            t1, t2, t3, t4 = rt
            S.op("dve", lambda e: e.tensor_tensor(out=t1[:], in0=x1, in1=cosb, op=ALU.mult), reads=[z_, rp], writes=[t1])
            S.op("pool", lambda e: e.tensor_tensor(out=t2[:], in0=x2, in1=sinb, op=ALU.mult), reads=[z_, rp], writes=[t2])
            S.op("dve", lambda e: e.tensor_tensor(out=t3[:], in0=x2, in1=cosb, op=ALU.mult), reads=[z_, rp], writes=[t3])
            S.op("pool", lambda e: e.tensor_tensor(out=t4[:], in0=x1, in1=sinb, op=ALU.mult), reads=[z_, rp], writes=[t4])
            S.op("dve", lambda e: e.tensor_tensor(out=x1, in0=t1[:], in1=t2[:], op=ALU.subtract), reads=[t1, t2], writes=[z_])
            S.op("dve", lambda e: e.tensor_tensor(out=x2, in0=t3[:], in1=t4[:], op=ALU.add), reads=[t3, t4], writes=[z_])
        S.dma("sp", lambda e: e.dma_start(out=zscr[n * 128:(n + 1) * 128, :], in_=z_[:]), reads=[z_], writes=[zscr])
    C.release(mA)
    if stop_after in ("A", "A_short"):
        if dbg is not None:
            d = Buf("dbg", nc.dram_tensor("dbg", [256, P_IN], F32, kind="ExternalOutput").ap())
            S.dma("sp", lambda e: e.dma_start(out=d[0:128, :], in_=zscr[0:128, :]), reads=[zscr], writes=[d])
            S.dma("sp", lambda e: e.dma_start(out=d[128:256, :], in_=zscr[T:T + 128, :]), reads=[zscr], writes=[d])
        C.barrier()
        print("ninst", S.ninst, "nwaits", S.nwaits)
        return nc

    ynascr = C.dram("ynascr", [T, 512])
    if stop_after in ("C_short", "C_feat", "C_one"):
        return build_phase_c(nc, C, S, zscr, ident, tri_d, rwp, stop_after)
    mB = C.mark()
    KT = C.sb("KT", [128, 4, TT], BF16)
    mB0 = C.mark()
    kst = [C.sb("kst", [128, 512]) for _ in range(2)]
    kb = [C.sb("kb", [128, 512], BF16) for _ in range(2)]
    pkt = [C.ps("pkt", [128, 4, 128], BF16) for _ in range(2)]
    for it, n in enumerate(tilesA if stop_after == "B_short" else range(NTT)):
        ks_, kb_, pk_ = rr(kst, it), rr(kb, it), rr(pkt, it)
        S.dma("sp", lambda e: e.dma_start(out=ks_[:], in_=zscr[n * 128:(n + 1) * 128, 512:1024]), reads=[zscr], writes=[ks_])
        S.op("dve", lambda e: e.tensor_copy(out=kb_[:], in_=ks_[:]), reads=[ks_], writes=[kb_])
        for hp in range(4):
            S.op("pe", lambda e: e.transpose(out=pk_[:, hp, :], in_=kb_[:, hp * 128:(hp + 1) * 128], identity=identb[:]),
                 reads=[kb_, identb], writes=[pk_], pe_acc=True)
        S.op("act", lambda e: e.copy(out=KT[:, :, n * 128:(n + 1) * 128], in_=pk_[:]), reads=[pk_], writes=[KT])
    C.release(mB0)
    ebst = C.sb("ebst", [128, 8, 256])
    EBi = C.sb("EBi", [128, 8, 256], BF16); EBe = C.sb("EBe", [128, 8, 256], BF16)

    def load_eb(qi, dst):
        S.dma("sp", lambda e: e.dma_start(out=ebst[:], in_=nab_d[qi]), reads=[nab_d], writes=[ebst])
        S.op("act", lambda e: e.activation(out=dst[:], in_=ebst[:], func=AF.Exp), reads=[ebst], writes=[dst])
    load_eb(4, EBi)
    vcst = C.sb("vcst", [128, 2, 512]); VCa = C.sb("VCa", [128, 2, 8, 65], BF16)
    S.dma("sp", lambda e: e.dma_start(out=vcst[:], in_=zscr.t[T:TT, 1024:1536].rearrange("(j p) c -> p j c", p=128)), reads=[zscr], writes=[vcst])
    S.op("pool", lambda e: e.memset(VCa[:], 1.0), writes=[VCa])
    S.op("dve", lambda e: e.tensor_copy(out=VCa[:, :, :, 0:64], in_=vcst.t[:].rearrange("p j (h d) -> p j h d", h=8)), reads=[vcst], writes=[VCa])
    qst = [C.sb("qst", [64, 512]) for _ in range(2)]; qb = [C.sb("qb", [64, 512], BF16) for _ in range(2)]
    QTr = [C.sb("QTr", [128, 4, 64], BF16) for _ in range(2)]
    vst = [C.sb("vst", [128, 4, 512]) for _ in range(2)]
    Va = [C.sb("Va", [128, 4, 8, 65], BF16) for _ in range(2)]
    for v_ in Va:
        S.op("pool", lambda e: e.memset(v_[:], 1.0), writes=[v_])
    PT = [C.sb("PT", [128, 384], BF16) for _ in range(3)]
    yrow = [C.sb("yrow", [64, 8, 64]) for _ in range(2)]
    rec = [C.sb("rec", [64, 8, 1]) for _ in range(2)]
    pq = C.ps("pq", [128, 4, 64], BF16)
    pss = [C.ps("pss", [128, 384]) for _ in range(3)]
    po = [[C.ps("po", [64, 4, 65]) for _ in range(2)] for _ in range(2)]
    rowsB = list(range(128))
    if stop_after == "B_short":
        rowsB = [0, 1, 2, 3, 4, 5, 125, 127]
    cur_edge = None
    cnt = 0
    for ir, i in enumerate(rowsB):
        rs_ = min(max(i - 4, 0), 120); qi = i - rs_
        if qi == 4:
            EB = EBi
        else:
            if cur_edge != qi:
                load_eb(qi, EBe); cur_edge = qi
            EB = EBe
        qs_, qb_, qt_, vs_, va_, yr_, rc_, po_ = rr(qst, ir), rr(qb, ir), rr(QTr, ir), rr(vst, ir), rr(Va, ir), rr(yrow, ir), rr(rec, ir), rr(po, ir)
        S.dma("sp", lambda e: e.dma_start(out=qs_[:], in_=zscr[i * 64:(i + 1) * 64, 0:512]), reads=[zscr], writes=[qs_])
        S.dma("sp", lambda e: e.dma_start(out=vs_[:], in_=zscr.t[rs_ * 64:rs_ * 64 + 512, 1024:1536].rearrange("(j p) c -> p j c", p=128)),
              reads=[zscr], writes=[vs_])
        S.op("dve", lambda e: e.tensor_copy(out=qb_[:], in_=qs_[:]), reads=[qs_], writes=[qb_])
        for hp in range(4):
            S.op("pe", lambda e: e.transpose(out=pq[:, hp, :], in_=qb_[:, hp * 128:(hp + 1) * 128], identity=identb[0:64, 0:64]),
                 reads=[qb_, identb], writes=[pq], pe_acc=True)
        S.op("act", lambda e: e.copy(out=qt_[:], in_=pq[:]), reads=[pq], writes=[qt_])
        S.op("pool", lambda e: e.tensor_copy(out=va_[:, :, :, 0:64], in_=vs_.t[:].rearrange("p j (h d) -> p j h d", h=8)), reads=[vs_], writes=[va_])
        for h in range(8):
            hp, j2 = divmod(h, 2)
            pr = slice(j2 * 64, (j2 + 1) * 64)
            ps_, pt_ = rr(pss, cnt), rr(PT, cnt); cnt += 1
            for jt in range(4):
                k0 = rs_ * 64 + jt * 128
                S.op("pe", lambda e: e.matmul(ps_[:, jt * 64:(jt + 1) * 64], lhsT=KT[pr, hp, k0:k0 + 128], rhs=qt_[pr, hp, :], start=True, stop=True),
                     reads=[KT, qt_], writes=[ps_], pe_acc=(jt > 0))
            for jc in range(2):
                k0 = T + jc * 128
                S.op("pe", lambda e: e.matmul(ps_[:, 256 + jc * 64:256 + (jc + 1) * 64], lhsT=KT[pr, hp, k0:k0 + 128], rhs=qt_[pr, hp, :], start=True, stop=True),
                     reads=[KT, qt_], writes=[ps_], pe_acc=True)
            S.op("act", lambda e: e.activation(out=pt_[:], in_=ps_[:], func=AF.Exp, scale=0.125), reads=[ps_], writes=[pt_])
            S.op("dve", lambda e: e.tensor_tensor(out=pt_[:, 0:256], in0=pt_[:, 0:256], in1=EB[:, h, :], op=ALU.mult), reads=[pt_, EB], writes=[pt_])
            pot = po_[h // 4]
            for jt in range(4):
                S.op("pe", lambda e: e.matmul(pot[:, h % 4, :], lhsT=pt_[:, jt * 64:(jt + 1) * 64], rhs=va_[:, jt, h, :], start=(jt == 0), stop=False),
                     reads=[pt_, va_], writes=[pot], pe_acc=(jt > 0 or h % 4 > 0))
            for jc in range(2):
                S.op("pe", lambda e: e.matmul(pot[:, h % 4, :], lhsT=pt_[:, 256 + jc * 64:256 + (jc + 1) * 64], rhs=VCa[:, jc, h, :], start=False, stop=(jc == 1)),
                     reads=[pt_, VCa], writes=[pot], pe_acc=True)
        for hh in range(2):
            pot = po_[hh]
            S.op("dve", lambda e: e.reciprocal(out=rc_[:, hh * 4:(hh + 1) * 4, :], in_=pot[:, :, 64:65]), reads=[pot], writes=[rc_])
            S.op("dve", lambda e: e.tensor_tensor(out=yr_[:, hh * 4:(hh + 1) * 4, :], in0=pot[:, :, 0:64],
                                                  in1=rc_[:, hh * 4:(hh + 1) * 4, :].to_broadcast([64, 4, 64]), op=ALU.mult),
                 reads=[pot, rc_], writes=[yr_])
        S.dma("sp", lambda e: e.dma_start(out=ynascr[i * 64:(i + 1) * 64, :], in_=yr_.t[:].rearrange("p h d -> p (h d)")), reads=[yr_], writes=[ynascr])
    C.release(mB)
    if stop_after == "B_short":
        d = Buf("dbg", nc.dram_tensor("dbg", [8 * 64, 512], F32, kind="ExternalOutput").ap())
        for ir, i in enumerate(rowsB):
            S.dma("sp", lambda e: e.dma_start(out=d[ir * 64:(ir + 1) * 64, :], in_=ynascr[i * 64:(i + 1) * 64, :]), reads=[ynascr], writes=[d])
        C.barrier()
        print("ninst", S.ninst, "nwaits", S.nwaits)
        return nc
    yrwscr = build_phase_c(nc, C, S, zscr, ident, tri_d, rwp, stop_after)
    build_phase_def(nc, C, S, xin, zscr, modscr, ynascr, yrwscr, wD, identb, ident, out_d, stop_after)
    C.barrier()
    print("ninst", S.ninst, "nwaits", S.nwaits)
    return nc


def build_phase_c(nc, C, S, zscr, ident, tri_d, rwp, stop_after):
    yfscr = C.dram("yfscr", [T, 512]); yrwscr = C.dram("yrwscr", [T, 512])
    short = stop_after == "C_short"
    mC = C.mark()
    US, LS, UI, LI, SEL127, SEL0 = [C.sb(f"tri{i}", [128, 128]) for i in range(6)]
    for i, b in enumerate([US, LS, UI, LI, SEL127, SEL0]):
        S.dma("sp", lambda e: e.dma_start(out=b[:], in_=tri_d[i]), reads=[tri_d], writes=[b])

    def bc(key, ap, n):
        b = C.sb(key, [128, n])
        S.dma("sp", lambda e: e.dma_start(out=b[:], in_=ap.partition_broadcast(128)), reads=[rwp[key.split("#")[0]]], writes=[b])
        return b
    mu_p = bc("mu_prev", rwp["mu_prev"].t, 1920); mu_n = bc("mu_next", rwp["mu_next"].t, 1920)
    k_k = bc("k_k", rwp["k_k"].t, 512); k_a = bc("k_a", rwp["k_a"].t, 512); r_k = bc("r_k", rwp["r_k"].t, 512)
    gn_w = bc("gn_w", rwp["gn_w"].t, 512); gn_b = bc("gn_b", rwp["gn_b"].t, 512)
    w0 = [bc(f"w0#{d}", rwp["w0"][d], 512) for d in range(2)]; a0 = [bc(f"a0#{d}", rwp["a0"][d], 512) for d in range(2)]
    WUP = C.sb("WUP", [128, 512]); AUP = C.sb("AUP", [128, 512]); GUP = C.sb("GUP", [128, 512])
    for b, k in [(WUP, "w_up"), (AUP, "a_up"), (GUP, "g_up")]:
        S.dma("sp", lambda e: e.dma_start(out=b[:], in_=rwp[k][:, :]), reads=[rwp[k]], writes=[b])
    S0all = C.sb("S0all", [128, 64, 8])
    zc = C.sb("zc", [128, 1920]); zp = C.sb("zp", [128, 1920]); zn = C.sb("zn", [128, 1920]); zs = C.sb("zs", [128, 1920])
    lin = C.sb("lin", [128, 384]); LT = C.sb("LT", [128, 3, 128])
    f = {k: C.sb(k, [128, 512]) for k in ["kk", "logw", "a", "kmod", "g", "rrk", "tmp", "cum", "ep", "em", "eex",
                                          "At", "Rt", "Bt", "Kt", "Bh", "Kh", "pCb", "ysum", "yout"]}
    ss = C.sb("ss", [128, 8]); sd = C.sb("sd", [128, 8]); gmv = C.sb("gmv", [128, 8, 2]); gst = C.sb("gst", [128, 8, 6])
    TTt = C.sb("TTt", [128, 4, 4, 128])
    Dg = C.sb("Dg", [64, 8, 64])
    Nm = C.sb("Nm", [128, 128])
    NP = [C.sb("NP", [128, 2, 128]) for _ in range(2)]
    PRB = C.sb("PRB", [128, 2, 128]); AKRK = C.sb("AKRK", [128, 2, 128])
    X = [C.sb("X", [128, 128]) for _ in range(2)]
    McT = C.sb("McT", [64, 64]); NcS = C.sb("NcS", [64, 64]); QT = C.sb("QT", [64, 128])
    Hst = C.sb("Hst", [64, 8, 64])
    M2 = [C.sb("M2", [128, 2, 128]) for _ in range(2)]
    for d, (st, inc) in enumerate([(US, UI), (LS, LI)]):
        S.op("pool", lambda e: e.tensor_copy(out=M2[d][:, 0, :], in_=st[:]), reads=[st], writes=[M2[d]])
        S.op("pool", lambda e: e.tensor_copy(out=M2[d][:, 1, :], in_=inc[:]), reads=[inc], writes=[M2[d]])
    pbig = [C.ps("pbig", [128, 512]) for _ in range(2)]
    pT = C.ps("pT", [128, 4, 128])
    pA = C.ps("pA", [128, 2, 128]); pB = C.ps("pB", [128, 2, 128])
    pX = C.ps("pX", [128, 2, 128])
    pM = C.ps("pM", [64, 4, 64])
    pY = C.ps("pY", [128, 512])
    v3 = lambda b: b.t[:].rearrange("p (h d) -> p h d", h=8)
    NEG = -float(np.exp(-0.5))

    def features(n, d):
        t0 = n * 128
        first = n in (0, NT); lastt = n in (NT - 1, NTT - 1)
        S.dma("sp", lambda e: e.dma_start(out=zc[:], in_=zscr[t0:t0 + 128, RW0:RW0 + 1920]), reads=[zscr], writes=[zc])
        if first:
            S.op("pool", lambda e: e.memset(zp[:], 0.0), writes=[zp])
            S.dma("sp", lambda e: e.dma_start(out=zp[1:128, :], in_=zscr[t0:t0 + 127, RW0:RW0 + 1920]), reads=[zscr], writes=[zp])
        else:
            S.dma("sp", lambda e: e.dma_start(out=zp[:], in_=zscr[t0 - 1:t0 + 127, RW0:RW0 + 1920]), reads=[zscr], writes=[zp])
        if lastt:
            S.op("pool", lambda e: e.memset(zn[:], 0.0), writes=[zn])
            S.dma("sp", lambda e: e.dma_start(out=zn[0:127, :], in_=zscr[t0 + 1:t0 + 128, RW0:RW0 + 1920]), reads=[zscr], writes=[zn])
        else:
            S.dma("sp", lambda e: e.dma_start(out=zn[:], in_=zscr[t0 + 1:t0 + 129, RW0:RW0 + 1920]), reads=[zscr], writes=[zn])
        S.op("dve", lambda e: e.tensor_tensor(out=zp[:], in0=zp[:], in1=zc[:], op=ALU.subtract), reads=[zp, zc], writes=[zp])
        S.op("pool", lambda e: e.tensor_tensor(out=zn[:], in0=zn[:], in1=zc[:], op=ALU.subtract), reads=[zn, zc], writes=[zn])
        S.op("dve", lambda e: e.tensor_tensor(out=zp[:], in0=zp[:], in1=mu_p[:], op=ALU.mult), reads=[zp, mu_p], writes=[zp])
        S.op("pool", lambda e: e.tensor_tensor(out=zn[:], in0=zn[:], in1=mu_n[:], op=ALU.mult), reads=[zn, mu_n], writes=[zn])
        S.op("dve", lambda e: e.tensor_tensor(out=zs[:], in0=zc[:], in1=zp[:], op=ALU.add), reads=[zc, zp], writes=[zs])
        S.op("dve", lambda e: e.tensor_tensor(out=zs[:], in0=zs[:], in1=zn[:], op=ALU.add), reads=[zs, zn], writes=[zs])
        S.op("act", lambda e: e.activation(out=lin[:, 0:128], in_=zs[:, 1536:1664], func=AF.Tanh), reads=[zs], writes=[lin])
        S.op("act", lambda e: e.copy(out=lin[:, 128:256], in_=zs[:, 1664:1792]), reads=[zs], writes=[lin])
        S.op("act", lambda e: e.activation(out=lin[:, 256:384], in_=zs[:, 1792:1920], func=AF.Sigmoid), reads=[zs], writes=[lin])
        for j in range(3):
            S.op("pe", lambda e: e.transpose(out=pT[:, j, :], in_=lin[:, j * 128:(j + 1) * 128], identity=ident[:]), reads=[lin, ident], writes=[pT], pe_acc=True)
        S.op("act", lambda e: e.copy(out=LT[:], in_=pT[:, 0:3, :]), reads=[pT], writes=[LT])
        dp = slice(d * 64, (d + 1) * 64)
        S.op("pe", lambda e: e.matmul(pbig[0][:], lhsT=LT[dp, 0, :], rhs=WUP[dp, :], start=True, stop=True), reads=[LT, WUP], writes=[pbig[0]])
        S.op("dve", lambda e: e.tensor_tensor(out=f["logw"][:], in0=pbig[0][:], in1=w0[d][:], op=ALU.add), reads=[pbig[0], w0[d]], writes=[f["logw"]])
        S.op("act", lambda e: e.activation(out=f["logw"][:], in_=f["logw"][:], func=AF.Sigmoid), reads=[f["logw"]], writes=[f["logw"]])
        S.op("pool", lambda e: e.tensor_scalar_mul(out=f["logw"][:], in0=f["logw"][:], scalar1=NEG), reads=[f["logw"]], writes=[f["logw"]])
        S.op("pe", lambda e: e.matmul(pbig[1][:], lhsT=LT[dp, 1, :], rhs=AUP[dp, :], start=True, stop=True), reads=[LT, AUP], writes=[pbig[1]])
        S.op("dve", lambda e: e.tensor_tensor(out=f["a"][:], in0=pbig[1][:], in1=a0[d][:], op=ALU.add), reads=[pbig[1], a0[d]], writes=[f["a"]])
        S.op("act", lambda e: e.activation(out=f["a"][:], in_=f["a"][:], func=AF.Sigmoid), reads=[f["a"]], writes=[f["a"]])
        S.op("pe", lambda e: e.matmul(pbig[0][:], lhsT=LT[:, 2, :], rhs=GUP[:], start=True, stop=True), reads=[LT, GUP], writes=[pbig[0]])
        S.op("act", lambda e: e.copy(out=f["g"][:], in_=pbig[0][:]), reads=[pbig[0]], writes=[f["g"]])
        S.op("dve", lambda e: e.tensor_tensor(out=f["kk"][:], in0=zs[:, 512:1024], in1=k_k[:], op=ALU.mult), reads=[zs, k_k], writes=[f["kk"]])
        S.op("pool", lambda e: e.tensor_tensor(out=f["tmp"][:], in0=f["kk"][:], in1=f["kk"][:], op=ALU.mult), reads=[f["kk"]], writes=[f["tmp"]])
        S.op("dve", lambda e: e.tensor_reduce(out=ss[:], in_=v3(f["tmp"]), axis=AX.X, op=ALU.add), reads=[f["tmp"]], writes=[ss])
        S.op("dve", lambda e: e.tensor_scalar_add(out=ss[:], in0=ss[:], scalar1=1e-12), reads=[ss], writes=[ss])
        S.op("act", lambda e: e.activation(out=ss[:], in_=ss[:], func=AF.Ln), reads=[ss], writes=[ss])
        S.op("act", lambda e: e.activation(out=ss[:], in_=ss[:], func=AF.Exp, scale=-0.5), reads=[ss], writes=[ss])
        S.op("dve", lambda e: e.tensor_tensor(out=v3(f["kk"]), in0=v3(f["kk"]), in1=ss.t[:].unsqueeze(2).to_broadcast([128, 8, 64]), op=ALU.mult),
             reads=[f["kk"], ss], writes=[f["kk"]])
        S.op("dve", lambda e: e.scalar_tensor_tensor(out=f["kmod"][:], in0=f["a"][:], scalar=-1.0, in1=k_a[:], op0=ALU.add, op1=ALU.mult),
             reads=[f["a"], k_a], writes=[f["kmod"]])
        S.op("dve", lambda e: e.scalar_tensor_tensor(out=f["kmod"][:], in0=f["kmod"][:], scalar=1.0, in1=zs[:, 512:1024], op0=ALU.add, op1=ALU.mult),
             reads=[f["kmod"], zs], writes=[f["kmod"]])
        S.op("pool", lambda e: e.tensor_tensor(out=f["rrk"][:], in0=zs[:, 0:512], in1=r_k[:], op=ALU.mult), reads=[zs, r_k], writes=[f["rrk"]])
        S.op("pool", lambda e: e.tensor_tensor(out=f["tmp"][:], in0=f["rrk"][:], in1=f["kmod"][:], op=ALU.mult), reads=[f["rrk"], f["kmod"]], writes=[f["tmp"]])
        S.op("dve", lambda e: e.tensor_reduce(out=sd[:], in_=v3(f["tmp"]), axis=AX.X, op=ALU.add), reads=[f["tmp"]], writes=[sd])

    def chunk(n, d, emit, nheads=8, stage=6):
        Lm, maskN, sel = ((UI, LS, SEL127), (LI, US, SEL0))[d]
        m2 = M2[d]
        r_ = zs.t[:, 0:512]; v_ = zs.t[:, 1024:1536]
        S.op("pe", lambda e: e.matmul(pbig[1][:], lhsT=Lm[:], rhs=f["logw"][:], start=True, stop=True), reads=[Lm, f["logw"]], writes=[pbig[1]])
        S.op("act", lambda e: e.activation(out=f["ep"][:], in_=pbig[1][:], func=AF.Exp), reads=[pbig[1]], writes=[f["ep"]])
        S.op("act", lambda e: e.activation(out=f["em"][:], in_=pbig[1][:], func=AF.Exp, scale=-1.0), reads=[pbig[1]], writes=[f["em"]])
        S.op("dve", lambda e: e.tensor_tensor(out=f["cum"][:], in0=pbig[1][:], in1=f["logw"][:], op=ALU.subtract), reads=[pbig[1], f["logw"]], writes=[f["cum"]])
        S.op("act", lambda e: e.activation(out=f["eex"][:], in_=f["cum"][:], func=AF.Exp), reads=[f["cum"]], writes=[f["eex"]])
        S.op("dve", lambda e: e.scalar_tensor_tensor(out=f["At"][:], in0=f["kk"][:], scalar=-1.0, in1=f["eex"][:], op0=ALU.mult, op1=ALU.mult),
             reads=[f["kk"], f["eex"]], writes=[f["At"]])
        S.op("pool", lambda e: e.tensor_tensor(out=f["Rt"][:], in0=r_, in1=f["ep"][:], op=ALU.mult), reads=[zs, f["ep"]], writes=[f["Rt"]])
        S.op("dve", lambda e: e.tensor_tensor(out=f["Bt"][:], in0=f["kk"][:], in1=f["a"][:], op=ALU.mult), reads=[f["kk"], f["a"]], writes=[f["Bt"]])
        S.op("dve", lambda e: e.tensor_tensor(out=f["Bt"][:], in0=f["Bt"][:], in1=f["em"][:], op=ALU.mult), reads=[f["Bt"], f["em"]], writes=[f["Bt"]])
        S.op("pool", lambda e: e.tensor_tensor(out=f["Kt"][:], in0=f["kmod"][:], in1=f["em"][:], op=ALU.mult), reads=[f["kmod"], f["em"]], writes=[f["Kt"]])
        S.op("pe", lambda e: e.matmul(pbig[0][:], lhsT=sel[:], rhs=f["ep"][:], start=True, stop=True), reads=[sel, f["ep"]], writes=[pbig[0]])
        S.op("act", lambda e: e.copy(out=f["pCb"][:], in_=pbig[0][:]), reads=[pbig[0]], writes=[f["pCb"]])
        S.op("dve", lambda e: e.tensor_tensor(out=f["Bh"][:], in0=f["Bt"][:], in1=f["pCb"][:], op=ALU.mult), reads=[f["Bt"], f["pCb"]], writes=[f["Bh"]])
        S.op("pool", lambda e: e.tensor_tensor(out=f["Kh"][:], in0=f["Kt"][:], in1=f["pCb"][:], op=ALU.mult), reads=[f["Kt"], f["pCb"]], writes=[f["Kh"]])
        S.op("dve", lambda e: e.tensor_tensor(out=Dg[:], in0=f["pCb"].t[0:64, :].rearrange("p (h d) -> p h d", h=8),
                                              in1=ident.t[0:64, 0:64].unsqueeze(1).to_broadcast([64, 8, 64]), op=ALU.mult),
             reads=[f["pCb"], ident], writes=[Dg])
        for ai, key in enumerate(["At", "Rt", "Bt", "Kt"]):
            for hp in range(4):
                S.op("pe", lambda e: e.transpose(out=pT[:, hp, :], in_=f[key][:, hp * 128:(hp + 1) * 128], identity=ident[:]),
                     reads=[f[key], ident], writes=[pT], pe_acc=True)
            S.op(("act", "dve")[ai % 2], (lambda e: e.copy(out=TTt[:, :, ai, :], in_=pT[:])) if ai % 2 == 0 else
                 (lambda e: e.tensor_copy(out=TTt[:, :, ai, :], in_=pT[:])), reads=[pT], writes=[TTt])
        for h in range(nheads if stage >= 2 else 0):
            hp, j2 = divmod(h, 2)
            pr = slice(j2 * 64, (j2 + 1) * 64); hs = slice(h * 64, (h + 1) * 64)
            AR = TTt[pr, hp, 0:2, :]; AtT = TTt[pr, hp, 0, :]; BtT = TTt[pr, hp, 2, :]; KtT = TTt[pr, hp, 3, :]
            S.op("pe", lambda e: e.matmul(pA[:], lhsT=BtT, rhs=AR, start=True, stop=True), reads=[TTt], writes=[pA])
            S.op("pe", lambda e: e.matmul(pB[:], lhsT=KtT, rhs=AR, start=True, stop=True), reads=[TTt], writes=[pB])
            S.op("pe", lambda e: e.matmul(pX[:, 0, :], lhsT=AtT, rhs=BtT, start=True, stop=True), reads=[TTt], writes=[pX])
            S.op("dve", lambda e: e.tensor_tensor(out=PRB[:], in0=pA[:], in1=m2[:], op=ALU.mult), reads=[pA, m2], writes=[PRB])
            S.op("dve", lambda e: e.tensor_tensor(out=AKRK[:], in0=pB[:], in1=m2[:], op=ALU.mult), reads=[pB, m2], writes=[AKRK])
            S.op("dve", lambda e: e.tensor_tensor(out=NP[0][:, 0, :], in0=pX[:, 0, :], in1=maskN[:], op=ALU.mult), reads=[pX, maskN], writes=[NP[0]])
            S.op("pool", lambda e: e.tensor_copy(out=NP[0][:, 1, :], in_=PRB[:, 0, :]), reads=[PRB], writes=[NP[0]])
            S.op("pe", lambda e: e.matmul(pX[:, 1, 0:64], lhsT=AKRK[:, 0, :], rhs=v_[:, hs], start=True, stop=True), reads=[AKRK, zs], writes=[pX])
            S.op("pool", lambda e: e.tensor_copy(out=X[0][:, 0:64], in_=f["At"][:, hs]), reads=[f["At"]], writes=[X[0]])
            S.op("act", lambda e: e.copy(out=X[0][:, 64:128], in_=pX[:, 1, 0:64]), reads=[pX], writes=[X[0]])
            for i in range(7 if stage >= 3 else 0):
                npi, npn = NP[i % 2], NP[(i + 1) % 2]
                xi, xn = X[i % 2], X[(i + 1) % 2]
                S.op("pe", lambda e: e.matmul(pX[:, 0, :], lhsT=npi[:, 1, :], rhs=xi[:], start=True, stop=True), reads=[npi, xi], writes=[pX])
                S.op("dve", lambda e: e.tensor_tensor(out=xn[:], in0=pX[:, 0, :], in1=xi[:], op=ALU.add), reads=[pX, xi], writes=[xn])
                if i < 6:
                    S.op("pe", lambda e: e.matmul(pA[:, 0, :], lhsT=npi[:, 1, :], rhs=npi[:, 0, :], start=True, stop=True), reads=[npi], writes=[pA])
                    S.op("pe", lambda e: e.matmul(pA[:, 1, :], lhsT=npi[:, 0, :], rhs=npi[:, 1, :], start=True, stop=True), reads=[npi], writes=[pA], pe_acc=True)
                    S.op("act", lambda e: e.copy(out=npn[:], in_=pA[:]), reads=[pA], writes=[npn])
            Xf = X[1]
            if stage < 4:
                continue
            G = Xf[:, 0:64]; Ul = Xf[:, 64:128]
            sub = 0
            if sub in (0, 1):
                S.op("pe", lambda e: e.matmul(pM[:, 0, :], lhsT=G, rhs=f["Bh"][:, hs], start=True, stop=True), reads=[Xf, f["Bh"]], writes=[pM])
            if sub in (0, 2):
                S.op("pe", lambda e: e.matmul(pM[:, 1, :], lhsT=f["Bh"][:, hs], rhs=Ul, start=True, stop=True), reads=[Xf, f["Bh"]], writes=[pM], pe_acc=True)
                S.op("pe", lambda e: e.matmul(pM[:, 3, :], lhsT=f["Kh"][:, hs], rhs=v_[:, hs], start=True, stop=True), reads=[f["Kh"], zs], writes=[pM], pe_acc=True)
            if sub in (0, 1):
                S.op("dve", lambda e: e.tensor_tensor(out=McT[:], in0=pM[:, 0, :], in1=Dg[:, h, :], op=ALU.add), reads=[pM, Dg], writes=[McT])
            if sub in (0, 2):
                S.op("dve", lambda e: e.tensor_copy(out=NcS[:], in_=pM[:, 1, :]), reads=[pM], writes=[NcS])
                S.op("dve", lambda e: e.tensor_tensor(out=NcS[:], in0=pM[:, 3, :], in1=NcS[:], op=ALU.add), reads=[pM, NcS], writes=[NcS])
            if emit and stage >= 5:
                S.op("pe", lambda e: e.matmul(pB[0:64, 0, :], lhsT=G, rhs=PRB[:, 1, :], start=True, stop=True), reads=[Xf, PRB], writes=[pB])
                S.op("pe", lambda e: e.matmul(pB[0:64, 1, :], lhsT=f["Rt"][:, hs], rhs=ident[:], start=True, stop=True), reads=[f["Rt"], ident], writes=[pB], pe_acc=True)
                S.op("dve", lambda e: e.tensor_copy(out=QT[:], in_=pB[0:64, 0, :]), reads=[pB], writes=[QT])
                S.op("dve", lambda e: e.tensor_tensor(out=QT[:], in0=pB[0:64, 1, :], in1=QT[:], op=ALU.add), reads=[pB, QT], writes=[QT])
                S.op("pe", lambda e: e.matmul(pY[:, hs], lhsT=QT[:], rhs=Hst[:, h, :], start=True, stop=False), reads=[QT, Hst], writes=[pY], pe_acc=(h > 0))
                S.op("pe", lambda e: e.matmul(pY[:, hs], lhsT=PRB[:, 1, :], rhs=Ul, start=False, stop=False), reads=[PRB, Xf], writes=[pY], pe_acc=True)
                S.op("pe", lambda e: e.matmul(pY[:, hs], lhsT=AKRK[:, 1, :], rhs=v_[:, hs], start=False, stop=True), reads=[AKRK, zs], writes=[pY], pe_acc=True)
            if stage < 6:
                continue
            S.op("pe", lambda e: e.matmul(pM[:, 2, :], lhsT=McT[:], rhs=Hst[:, h, :], start=True, stop=True), reads=[McT, Hst], writes=[pM])
            S.op("dve", lambda e: e.tensor_tensor(out=Hst[:, h, :], in0=pM[:, 2, :], in1=NcS[:], op=ALU.add), reads=[pM, NcS], writes=[Hst])

    if stop_after == "C_feat":
        dd = Buf("dbg", nc.dram_tensor("dbg", [2, 5, 128, 512], F32, kind="ExternalOutput").ap())
        for ti, (n, d) in enumerate([(0, 0), (63, 1)]):
            features(n, d)
            for ki, key in enumerate(["logw", "a", "kmod", "kk", "g"]):
                S.dma("sp", lambda e: e.dma_start(out=dd[ti, ki], in_=f[key][:]), reads=[f[key]], writes=[dd])
        C.barrier()
        print("ninst", S.ninst, "nwaits", S.nwaits)
        return nc
    if stop_after == "C_one":
        nh = 8
        dd = Buf("dbg", nc.dram_tensor("dbg", [128, 512], F32, kind="ExternalOutput").ap())
        dh = Buf("dbgh", nc.dram_tensor("dbgh", [64, 512], F32, kind="ExternalOutput").ap())
        S.op("pool", lambda e: e.memset(Hst[:], 0.0), writes=[Hst])
        features(0, 0)
        stg = 6
        chunk(0, 0, True, nheads=nh, stage=stg)
        if stg >= 5:
            S.op("act", lambda e: e.copy(out=f["yout"][:, 0:nh * 64], in_=pY[:, 0:nh * 64]), reads=[pY], writes=[f["yout"]])
        else:
            src = {1: f["Bh"], 2: X[0], 3: X[1], 4: X[1]}[stg]
            S.op("act", lambda e: e.copy(out=f["yout"][:, 0:128], in_=src[:, 0:128]), reads=[src], writes=[f["yout"]])
        S.dma("sp", lambda e: e.dma_start(out=dd[:, 0:nh * 64], in_=f["yout"][:, 0:nh * 64]), reads=[f["yout"]], writes=[dd])
        S.dma("sp", lambda e: e.dma_start(out=dh[:, :], in_=Hst.t[:].rearrange("p h d -> p (h d)")), reads=[Hst], writes=[dh])
        C.barrier()
        print("ninst", S.ninst, "nwaits", S.nwaits)
        return nc
    dbg_rows = []
    for d in range(2):
        if d == 0:
            order = [NT, NT + 1] + (list(range(NT)) if not short else [0, 1])
        else:
            order = [NT + 1, NT] + (list(range(NT - 1, -1, -1)) if not short else [63, 62])
        S.op("pool", lambda e: e.memset(Hst[:], 0.0), writes=[Hst])
        for n in order:
            emit = n < NT
            features(n, d)
            chunk(n, d, emit)
            if not emit:
                continue
            rows = slice(n * 128, (n + 1) * 128)
            if d == 0:
                S.op("act", lambda e: e.copy(out=f["yout"][:], in_=pY[:]), reads=[pY], writes=[f["yout"]])
                S.op("pool", lambda e: e.tensor_copy(out=S0all[:, n, :], in_=sd[:]), reads=[sd], writes=[S0all])
                S.dma("sp", lambda e: e.dma_start(out=yfscr[rows, :], in_=f["yout"][:]), reads=[f["yout"]], writes=[yfscr])
                continue
            if short:
                S.op("act", lambda e: e.copy(out=f["yout"][:], in_=pY[:]), reads=[pY], writes=[f["yout"]])
                S.dma("sp", lambda e: e.dma_start(out=yrwscr[rows, :], in_=f["yout"][:]), reads=[f["yout"]], writes=[yrwscr])
                continue
            S.dma("sp", lambda e: e.dma_start(out=f["ysum"][:], in_=yfscr[rows, :]), reads=[yfscr], writes=[f["ysum"]])
            S.op("dve", lambda e: e.tensor_tensor(out=f["ysum"][:], in0=pY[:], in1=f["ysum"][:], op=ALU.add), reads=[pY, f["ysum"]], writes=[f["ysum"]])
            for h in range(8):
                S.op("dve", lambda e: e.bn_stats(out=gst[:, h, :], in_=f["ysum"][:, h * 64:(h + 1) * 64]), reads=[f["ysum"]], writes=[gst])
                S.op("dve", lambda e: e.bn_aggr(out=gmv[:, h, :], in_=gst[:, h, :]), reads=[gst], writes=[gmv])
            S.op("dve", lambda e: e.tensor_scalar_add(out=ss[:], in0=gmv[:, :, 1], scalar1=GN_EPS), reads=[gmv], writes=[ss])
            S.op("act", lambda e: e.activation(out=ss[:], in_=ss[:], func=AF.Ln), reads=[ss], writes=[ss])
            S.op("act", lambda e: e.activation(out=ss[:], in_=ss[:], func=AF.Exp, scale=-0.5), reads=[ss], writes=[ss])
            y3 = v3(f["ysum"])
            S.op("dve", lambda e: e.tensor_tensor(out=y3, in0=y3, in1=gmv.t[:, :, 0:1].to_broadcast([128, 8, 64]), op=ALU.subtract), reads=[f["ysum"], gmv], writes=[f["ysum"]])
            S.op("dve", lambda e: e.tensor_tensor(out=y3, in0=y3, in1=ss.t[:].unsqueeze(2).to_broadcast([128, 8, 64]), op=ALU.mult), reads=[f["ysum"], ss], writes=[f["ysum"]])
            S.op("pool", lambda e: e.tensor_tensor(out=f["ysum"][:], in0=f["ysum"][:], in1=gn_w[:], op=ALU.mult), reads=[f["ysum"], gn_w], writes=[f["ysum"]])
            S.op("pool", lambda e: e.tensor_tensor(out=f["ysum"][:], in0=f["ysum"][:], in1=gn_b[:], op=ALU.add), reads=[f["ysum"], gn_b], writes=[f["ysum"]])
            S.op("dve", lambda e: e.tensor_tensor(out=sd[:], in0=sd[:], in1=S0all[:, n, :], op=ALU.add), reads=[sd, S0all], writes=[sd])
            S.op("dve", lambda e: e.tensor_tensor(out=v3(f["tmp"]), in0=zs.t[:, 1024:1536].rearrange("p (h d) -> p h d", h=8),
                                                  in1=sd.t[:].unsqueeze(2).to_broadcast([128, 8, 64]), op=ALU.mult), reads=[zs, sd], writes=[f["tmp"]])
            S.op("dve", lambda e: e.tensor_tensor(out=f["ysum"][:], in0=f["ysum"][:], in1=f["tmp"][:], op=ALU.add), reads=[f["ysum"], f["tmp"]], writes=[f["ysum"]])
            S.op("pool", lambda e: e.tensor_tensor(out=f["yout"][:], in0=f["ysum"][:], in1=f["g"][:], op=ALU.mult), reads=[f["ysum"], f["g"]], writes=[f["yout"]])
            S.dma("sp", lambda e: e.dma_start(out=yrwscr[rows, :], in_=f["yout"][:]), reads=[f["yout"]], writes=[yrwscr])
    C.release(mC)
    if short:
        dd = Buf("dbg", nc.dram_tensor("dbg", [512, 512], F32, kind="ExternalOutput").ap())
        S.dma("sp", lambda e: e.dma_start(out=dd[0:256, :], in_=yfscr[0:256, :]), reads=[yfscr], writes=[dd])
        S.dma("sp", lambda e: e.dma_start(out=dd[256:512, :], in_=yrwscr[62 * 128:64 * 128, :]), reads=[yrwscr], writes=[dd])
        C.barrier()
        print("ninst", S.ninst, "nwaits", S.nwaits)
        return nc
    return yrwscr


def build_phase_def(nc, C, S, xin, zscr, modscr, ynascr, yrwscr, wD, identb, ident, out_d, stop_after):
    h1scr = C.dram("h1scr", [T, D]); u2Tscr = C.dram("u2Tscr", [8, 128, T], BF16)
    AFF = C.sb("AFF", [128, NT, NE]); GM = C.sb("GM", [128, NT, NE])

    def bcast(name, src_buf, src_ap, eng="sp"):
        b = C.sb(name, [128, D])
        S.dma(eng, lambda e: e.dma_start(out=b[:], in_=src_ap.partition_broadcast(128)), reads=[src_buf], writes=[b])
        return b

    def ln_rows(xt, stats, mv, rstd, eps):
        for hf in range(2):
            S.op("dve", lambda e: e.bn_stats(out=stats[:, hf, :], in_=xt[:, hf * 512:(hf + 1) * 512]), reads=[xt], writes=[stats])
        S.op("dve", lambda e: e.bn_aggr(out=mv[:], in_=stats[:]), reads=[stats], writes=[mv])
        S.op("dve", lambda e: e.tensor_scalar_add(out=rstd[:], in0=mv[:, 1:2], scalar1=eps), reads=[mv], writes=[rstd])
        S.op("act", lambda e: e.activation(out=rstd[:], in_=rstd[:], func=AF.Ln), reads=[rstd], writes=[rstd])
        S.op("act", lambda e: e.activation(out=rstd[:], in_=rstd[:], func=AF.Exp, scale=-0.5), reads=[rstd], writes=[rstd])
        S.op("dve", lambda e: e.tensor_scalar(out=xt[:], in0=xt[:], scalar1=mv[:, 0:1], scalar2=rstd[:, 0:1], op0=ALU.subtract, op1=ALU.mult),
             reads=[xt, mv, rstd], writes=[xt])

    mD = C.mark()
    wst = C.sb("wstD", [128, 8, D])
    wpa = C.sb("wpa", [128, 4, D], BF16); wpr = C.sb("wpr", [128, 4, D], BF16); wo = C.sb("wo", [128, 8, D], BF16)
    wr = C.sb("wr", [128, 8, NE], BF16); wrs = C.sb("wrs", [128, 8, NE])
    for dst, key, nk in [(wpa, "w_pa", 4), (wpr, "w_pr", 4), (wo, "w_o", 8)]:
        S.dma("sp", lambda e: e.dma_start(out=wst[:, 0:nk, :], in_=wD[key].t.rearrange("(kc p) n -> p kc n", p=128)), reads=[wD[key]], writes=[wst])
        S.op("dve", lambda e: e.tensor_copy(out=dst[:], in_=wst[:, 0:nk, :]), reads=[wst], writes=[dst])
    S.dma("sp", lambda e: e.dma_start(out=wrs[:], in_=wD["w_router"].t.rearrange("(kc p) n -> p kc n", p=128)), reads=[wD["w_router"]], writes=[wrs])
    S.op("dve", lambda e: e.tensor_copy(out=wr[:], in_=wrs[:]), reads=[wrs], writes=[wr])
    gt1 = bcast("gt1", modscr, modscr[0, 2 * D:3 * D]); sh2 = bcast("sh2", modscr, modscr[0, 3 * D:4 * D]); sc2 = bcast("sc2", modscr, modscr[0, 4 * D:5 * D])
    l1g = bcast("l1g", wD["ln1_g"], wD["ln1_g"].t); l1b = bcast("l1b", wD["ln1_b"], wD["ln1_b"].t)
    yna = C.sb("yna", [128, 1024]); ynb = C.sb("ynb", [128, 1024], BF16)
    zg = C.sb("zg", [128, 2048]); xt = C.sb("xtD", [128, D]); pre = C.sb("pre", [128, D]); t1 = C.sb("t1", [128, D])
    mb = C.sb("mb", [128, D], BF16); u2b = C.sb("u2b", [128, D], BF16)
    yT = C.sb("yT", [128, 8, 128], BF16); mT = C.sb("mT", [128, 8, 128], BF16); u2T = C.sb("u2T", [128, 8, 128], BF16)
    stats = C.sb("statsD", [128, 2, 6]); mv = C.sb("mvD", [128, 2]); rstd = C.sb("rstdD", [128, 1])
    ex = C.sb("ex", [128, NE]); sm = C.sb("sm", [128, 1])
    ptp = C.ps("ptpD", [128, 8, 128], BF16)
    pa = [C.ps("paD", [128, 512]) for _ in range(2)]; pr_ = [C.ps("prD", [128, 512]) for _ in range(2)]
    pl = C.ps("plD", [128, NE])
    tilesD = range(NT) if stop_after != "F_short" else range(0)
    for n in tilesD:
        rows = slice(n * 128, (n + 1) * 128)
        S.dma("sp", lambda e: e.dma_start(out=yna[:, 0:512], in_=ynascr[rows, :]), reads=[ynascr], writes=[yna])
        S.dma("sp", lambda e: e.dma_start(out=yna[:, 512:1024], in_=yrwscr[rows, :]), reads=[yrwscr], writes=[yna])
        S.dma("sp", lambda e: e.dma_start(out=zg[:], in_=zscr[rows, G0:G0 + 2048]), reads=[zscr], writes=[zg])
        S.dma("sp", lambda e: e.dma_start(out=xt[:], in_=xin[rows, :]), reads=[xin], writes=[xt])
        S.op("pool", lambda e: e.tensor_copy(out=ynb[:], in_=yna[:]), reads=[yna], writes=[ynb])
        for c in range(8):
            S.op("pe", lambda e: e.transpose(out=ptp[:, c, :], in_=ynb[:, c * 128:(c + 1) * 128], identity=identb[:]), reads=[ynb, identb], writes=[ptp], pe_acc=True)
        S.op("act", lambda e: e.copy(out=yT[:], in_=ptp[:]), reads=[ptp], writes=[yT])
        S.op("act", lambda e: e.activation(out=zg[:], in_=zg[:], func=AF.Sigmoid), reads=[zg], writes=[zg])
        for hf in range(2):
            cs = slice(hf * 512, (hf + 1) * 512)
            for c in range(4):
                S.op("pe", lambda e: e.matmul(pa[hf][:], lhsT=yT[:, c, :], rhs=wpa[:, c, cs], start=(c == 0), stop=(c == 3)), reads=[yT, wpa], writes=[pa[hf]], pe_acc=(c > 0))
            for c in range(4):
                S.op("pe", lambda e: e.matmul(pr_[hf][:], lhsT=yT[:, 4 + c, :], rhs=wpr[:, c, cs], start=(c == 0), stop=(c == 3)), reads=[yT, wpr], writes=[pr_[hf]], pe_acc=(c > 0))
            S.op("dve", lambda e: e.tensor_tensor(out=pre[:, cs], in0=pa[hf][:], in1=zg[:, hf * 512:(hf + 1) * 512], op=ALU.mult), reads=[pa[hf], zg], writes=[pre])
            S.op("dve", lambda e: e.tensor_tensor(out=t1[:, cs], in0=pr_[hf][:], in1=zg[:, 1024 + hf * 512:1024 + (hf + 1) * 512], op=ALU.mult), reads=[pr_[hf], zg], writes=[t1])
        S.op("pool", lambda e: e.tensor_tensor(out=mb[:], in0=pre[:], in1=t1[:], op=ALU.add), reads=[pre, t1], writes=[mb])
        for c in range(8):
            S.op("pe", lambda e: e.transpose(out=ptp[:, c, :], in_=mb[:, c * 128:(c + 1) * 128], identity=identb[:]), reads=[mb, identb], writes=[ptp], pe_acc=(c > 0))
        S.op("act", lambda e: e.copy(out=mT[:], in_=ptp[:]), reads=[ptp], writes=[mT])
        for hf in range(2):
            cs = slice(hf * 512, (hf + 1) * 512)
            for c in range(8):
                S.op("pe", lambda e: e.matmul(pa[hf][:], lhsT=mT[:, c, :], rhs=wo[:, c, cs], start=(c == 0), stop=(c == 7)), reads=[mT, wo], writes=[pa[hf]], pe_acc=(c > 0))
            S.op("dve", lambda e: e.tensor_tensor(out=pre[:, cs], in0=pa[hf][:], in1=gt1[:, cs], op=ALU.mult), reads=[pa[hf], gt1], writes=[pre])
        S.op("dve", lambda e: e.scalar_tensor_tensor(out=pre[:], in0=xt[:], scalar=float(ALPHA), in1=pre[:], op0=ALU.mult, op1=ALU.add), reads=[xt, pre], writes=[pre])
        ln_rows(pre, stats, mv, rstd, LN_EPS)
        S.op("pool", lambda e: e.tensor_tensor(out=pre[:], in0=pre[:], in1=l1g[:], op=ALU.mult), reads=[pre, l1g], writes=[pre])
        S.op("pool", lambda e: e.tensor_tensor(out=pre[:], in0=pre[:], in1=l1b[:], op=ALU.add), reads=[pre, l1b], writes=[pre])
        S.dma("sp", lambda e: e.dma_start(out=h1scr[rows, :], in_=pre[:]), reads=[pre], writes=[h1scr])
        S.op("dve", lambda e: e.tensor_copy(out=t1[:], in_=pre[:]), reads=[pre], writes=[t1])
        ln_rows(t1, stats, mv, rstd, LN_EPS)
        S.op("pool", lambda e: e.tensor_tensor(out=t1[:], in0=t1[:], in1=sc2[:], op=ALU.mult), reads=[t1, sc2], writes=[t1])
        S.op("pool", lambda e: e.tensor_tensor(out=u2b[:], in0=t1[:], in1=sh2[:], op=ALU.add), reads=[t1, sh2], writes=[u2b])
        for c in range(8):
            S.op("pe", lambda e: e.transpose(out=ptp[:, c, :], in_=u2b[:, c * 128:(c + 1) * 128], identity=identb[:]), reads=[u2b, identb], writes=[ptp], pe_acc=(c > 0))
        S.op("act", lambda e: e.copy(out=u2T[:], in_=ptp[:]), reads=[ptp], writes=[u2T])
        S.dma("sp", lambda e: e.dma_start(out=u2Tscr.t[:, :, rows].rearrange("k p t -> p k t"), in_=u2T[:]), reads=[u2T], writes=[u2Tscr])
        for c in range(8):
            S.op("pe", lambda e: e.matmul(pl[:], lhsT=u2T[:, c, :], rhs=wr[:, c, :], start=(c == 0), stop=(c == 7)), reads=[u2T, wr], writes=[pl], pe_acc=(c > 0))
        S.op("act", lambda e: e.activation(out=ex[:], in_=pl[:], func=AF.Exp), reads=[pl], writes=[ex])
        S.op("dve", lambda e: e.tensor_reduce(out=sm[:], in_=ex[:], axis=AX.X, op=ALU.add), reads=[ex], writes=[sm])
        S.op("dve", lambda e: e.reciprocal(out=sm[:], in_=sm[:]), reads=[sm], writes=[sm])
        S.op("dve", lambda e: e.tensor_scalar(out=AFF[:, n, :], in0=ex[:], scalar1=sm[:, 0:1], scalar2=None, op0=ALU.mult), reads=[ex, sm], writes=[AFF])
    C.release(mD)

    mE = C.mark()
    ones = C.sb("ones", [128, 128]); lo = C.sb("lo", [128, NE]); hi = C.sb("hi", [128, NE]); mid = C.sb("mid", [128, NE])
    cmpb = C.sb("cmpb", [128, NT, NE]); cp = C.sb("cp", [128, NE]); ge = C.sb("ge", [128, NE]); dl = C.sb("dl", [128, NE])
    pc = C.ps("pcE", [128, NE])
    S.op("pool", lambda e: e.memset(ones[:], 1.0), writes=[ones])
    S.op("pool", lambda e: e.memset(lo[:], 0.0), writes=[lo])
    S.op("pool", lambda e: e.memset(hi[:], 1.0), writes=[hi])
    for it in range(34 if stop_after != "F_short" else 1):
        S.op("dve", lambda e: e.tensor_tensor(out=mid[:], in0=lo[:], in1=hi[:], op=ALU.add), reads=[lo, hi], writes=[mid])
        S.op("dve", lambda e: e.tensor_scalar_mul(out=mid[:], in0=mid[:], scalar1=0.5), reads=[mid], writes=[mid])
        S.op("dve", lambda e: e.tensor_tensor(out=cmpb[:], in0=AFF[:], in1=mid.t[:].unsqueeze(1).to_broadcast([128, NT, NE]), op=ALU.is_ge), reads=[AFF, mid], writes=[cmpb])
        S.op("dve", lambda e: e.tensor_reduce(out=cp[:], in_=cmpb.t[:].rearrange("p n e -> p e n"), axis=AX.X, op=ALU.add), reads=[cmpb], writes=[cp])
        S.op("pe", lambda e: e.matmul(pc[:], lhsT=ones[:], rhs=cp[:], start=True, stop=True), reads=[ones, cp], writes=[pc])
        S.op("dve", lambda e: e.tensor_single_scalar(out=ge[:], in_=pc[:], scalar=float(CAP) - 0.5, op=ALU.is_ge), reads=[pc], writes=[ge])
        S.op("dve", lambda e: e.tensor_tensor(out=dl[:], in0=mid[:], in1=lo[:], op=ALU.subtract), reads=[mid, lo], writes=[dl])
        S.op("dve", lambda e: e.tensor_tensor(out=dl[:], in0=dl[:], in1=ge[:], op=ALU.mult), reads=[dl, ge], writes=[dl])
        S.op("dve", lambda e: e.tensor_tensor(out=lo[:], in0=lo[:], in1=dl[:], op=ALU.add), reads=[lo, dl], writes=[lo])
        S.op("dve", lambda e: e.tensor_tensor(out=dl[:], in0=hi[:], in1=mid[:], op=ALU.subtract), reads=[hi, mid], writes=[dl])
        S.op("dve", lambda e: e.tensor_tensor(out=dl[:], in0=dl[:], in1=ge[:], op=ALU.mult), reads=[dl, ge], writes=[dl])
        S.op("dve", lambda e: e.tensor_tensor(out=hi[:], in0=mid[:], in1=dl[:], op=ALU.add), reads=[mid, dl], writes=[hi])
    S.op("dve", lambda e: e.tensor_tensor(out=cmpb[:], in0=AFF[:], in1=lo.t[:].unsqueeze(1).to_broadcast([128, NT, NE]), op=ALU.is_ge), reads=[AFF, lo], writes=[cmpb])
    S.op("dve", lambda e: e.tensor_tensor(out=GM[:], in0=AFF[:], in1=cmpb[:], op=ALU.mult), reads=[AFF, cmpb], writes=[GM])
    C.release(mE)

    mF = C.mark()
    gt2 = bcast("gt2", modscr, modscr[0, 5 * D:6 * D]); l2g = bcast("l2g", wD["ln2_g"], wD["ln2_g"].t); l2b = bcast("l2b", wD["ln2_b"], wD["ln2_b"].t)
    TB = 512
    u2blk = C.sb("u2blk", [128, 8, TB], BF16)
    hT = C.sb("hT", [128, 22, TB], BF16); W2b = C.sb("W2b", [128, 22, D], BF16)
    acc = C.sb("acc", [128, 4, D])
    w1s = [C.sb("w1s", [128, 8, 256]) for _ in range(2)]; w3s = [C.sb("w3s", [128, 8, 256]) for _ in range(2)]; w2s = [C.sb("w2s", [128, D]) for _ in range(2)]
    w1b = [C.sb("w1b", [128, 8, 256], BF16) for _ in range(2)]; w3b = [C.sb("w3b", [128, 8, 256], BF16) for _ in range(2)]
    sg = [C.sb("sg", [128, TB]) for _ in range(2)]
    h1t = C.sb("h1t", [128, D]); stats = C.sb("statsF", [128, 2, 6]); mv = C.sb("mvF", [128, 2]); rstd = C.sb("rstdF", [128, 1])
    ph1 = [C.ps("ph1", [128, TB]) for _ in range(2)]; ph3 = [C.ps("ph3", [128, TB]) for _ in range(2)]
    py = [C.ps("py", [128, 512]) for _ in range(2)]
    w1v = wD["w_e1"].t.rearrange("e (kc p) f -> e p kc f", p=128); w3v = wD["w_e3"].t.rearrange("e (kc p) f -> e p kc f", p=128)
    cnt = 0
    for bk in range(T // TB if stop_after != "F_short" else 1):
        t0 = bk * TB
        S.dma("sp", lambda e: e.dma_start(out=u2blk[:], in_=u2Tscr.t[:, :, t0:t0 + TB].rearrange("k p t -> p k t")), reads=[u2Tscr], writes=[u2blk])
        S.op("pool", lambda e: e.memset(acc[:], 0.0), writes=[acc])
        for ex_ in range(NE if stop_after != "F_short" else 2):
            for jp in range(11):
                a1, a3, b1, b3 = rr(w1s, jp), rr(w3s, jp), rr(w1b, jp), rr(w3b, jp)
                fs2 = slice(jp * 256, (jp + 1) * 256)
                S.dma("sp", lambda e: e.dma_start(out=a1[:], in_=w1v[ex_, :, :, fs2]), reads=[wD["w_e1"]], writes=[a1])
                S.dma("sp", lambda e: e.dma_start(out=a3[:], in_=w3v[ex_, :, :, fs2]), reads=[wD["w_e3"]], writes=[a3])
                for jj in range(2):
                    j = 2 * jp + jj
                    a2 = w2s[jj]
                    S.dma("sp", lambda e: e.dma_start(out=a2[:], in_=wD["w_e2"][ex_, j * 128:(j + 1) * 128, :]), reads=[wD["w_e2"]], writes=[a2])
                S.op("pool", lambda e: e.tensor_copy(out=b1[:], in_=a1[:]), reads=[a1], writes=[b1])
                S.op("pool", lambda e: e.tensor_copy(out=b3[:], in_=a3[:]), reads=[a3], writes=[b3])
                for jj in range(2):
                    j = 2 * jp + jj
                    a2 = w2s[jj]
                    cs_ = slice(jj * 128, (jj + 1) * 128)
                    p1, p3, sg_ = rr(ph1, cnt), rr(ph3, cnt), rr(sg, cnt); cnt += 1
                    S.op("act", lambda e: e.copy(out=W2b[:, j, :], in_=a2[:]), reads=[a2], writes=[W2b])
                    for kc in range(8):
                        S.op("pe", lambda e: e.matmul(p1[:], lhsT=b1[:, kc, cs_], rhs=u2blk[:, kc, :], start=(kc == 0), stop=(kc == 7)), reads=[b1, u2blk], writes=[p1], pe_acc=(kc > 0))
                    for kc in range(8):
                        S.op("pe", lambda e: e.matmul(p3[:], lhsT=b3[:, kc, cs_], rhs=u2blk[:, kc, :], start=(kc == 0), stop=(kc == 7)), reads=[b3, u2blk], writes=[p3], pe_acc=(kc > 0))
                    S.op("act", lambda e: e.activation(out=sg_[:], in_=p1[:], func=AF.Silu), reads=[p1], writes=[sg_])
                    S.op("dve", lambda e: e.tensor_tensor(out=hT[:, j, :], in0=p3[:], in1=sg_[:], op=ALU.mult), reads=[p3, sg_], writes=[hT])
            for tt in range(4):
                n = bk * 4 + tt
                for hf in range(2):
                    p = rr(py, tt * 2 + hf)
                    for j in range(22):
                        S.op("pe", lambda e: e.matmul(p[:], lhsT=hT[:, j, tt * 128:(tt + 1) * 128], rhs=W2b[:, j, hf * 512:(hf + 1) * 512], start=(j == 0), stop=(j == 21)),
                             reads=[hT, W2b], writes=[p], pe_acc=(j > 0))
                    S.op("dve", lambda e: e.scalar_tensor_tensor(out=acc[:, tt, hf * 512:(hf + 1) * 512], in0=p[:], scalar=GM[:, n, ex_:ex_ + 1],
                                                                 in1=acc[:, tt, hf * 512:(hf + 1) * 512], op0=ALU.mult, op1=ALU.add), reads=[p, GM, acc], writes=[acc])
        for tt in range(4):
            rows = slice(t0 + tt * 128, t0 + (tt + 1) * 128)
            S.dma("sp", lambda e: e.dma_start(out=h1t[:], in_=h1scr[rows, :]), reads=[h1scr], writes=[h1t])
            S.op("pool", lambda e: e.tensor_tensor(out=acc[:, tt, :], in0=acc[:, tt, :], in1=gt2[:], op=ALU.mult), reads=[acc, gt2], writes=[acc])
            S.op("dve", lambda e: e.scalar_tensor_tensor(out=h1t[:], in0=h1t[:], scalar=float(ALPHA), in1=acc[:, tt, :], op0=ALU.mult, op1=ALU.add), reads=[h1t, acc], writes=[h1t])
            ln_rows(h1t, stats, mv, rstd, LN_EPS)
            S.op("pool", lambda e: e.tensor_tensor(out=h1t[:], in0=h1t[:], in1=l2g[:], op=ALU.mult), reads=[h1t, l2g], writes=[h1t])
            S.op("pool", lambda e: e.tensor_tensor(out=h1t[:], in0=h1t[:], in1=l2b[:], op=ALU.add), reads=[h1t, l2b], writes=[h1t])
            S.dma("sp", lambda e: e.dma_start(out=out_d[rows, :], in_=h1t[:]), reads=[h1t], writes=[out_d])
    C.release(mF)


def host_consts():
    ident = np.eye(128, dtype=np.float32)
    t = np.arange(T)
    pos = np.stack([t // 64, t % 64], -1).astype(np.float32)
    nf = 16
    inv = np.power(np.float32(10000.0), -np.arange(nf, dtype=np.float32) / nf).astype(np.float32)
    ang = pos[:, :, None] * inv
    rope = np.stack([np.cos(ang), np.sin(ang)], 1).astype(np.float32)
    i = np.arange(128)
    US = (i[:, None] < i[None, :]); LS = (i[:, None] > i[None, :]); UI = (i[:, None] <= i[None, :]); LI = (i[:, None] >= i[None, :])
    SEL127 = np.broadcast_to((i == 127)[:, None], (128, 128)); SEL0 = np.broadcast_to((i == 0)[:, None], (128, 128))
    tri = np.stack([US, LS, UI, LI, SEL127, SEL0]).astype(np.float32)
    return {"ident": ident, "rope": rope, "tri": tri}


def na_bias_table(rpb):
    qi = np.arange(8)[:, None, None, None]
    p = np.arange(128)[None, :, None, None]
    jt = np.arange(4)[None, None, :, None]
    j = np.arange(64)[None, None, None, :]
    kr = 2 * jt + p // 64
    c = p % 64
    cs = np.clip(j - 8, 0, 48)
    valid = (c >= cs) & (c < cs + 16)
    ro = np.broadcast_to(kr - qi + 7, (8, 128, 4, 64))
    co = np.clip(np.broadcast_to(c - j + 15, (8, 128, 4, 64)), 0, 30)
    g = rpb[:, ro, co]
    g = np.where(np.broadcast_to(valid, g.shape), g, np.float32(-30000.0))
    return np.ascontiguousarray(g.transpose(1, 2, 0, 3, 4).reshape(8, 128, 8, 256)).astype(np.float32)


def make_in_maps(inputs, consts):
    maps = []
    nab = na_bias_table(inputs["rpb"][0])
    for b in range(4):
        m = dict(consts)
        m["x"] = np.ascontiguousarray(np.concatenate([inputs["x"][b], inputs["ctx"][b]], 0))
        m["cc"] = np.ascontiguousarray(np.stack([inputs["c"][b], inputs["c_ctx"]], 0))
        m["w_mod"] = inputs["w_mod"][0]; m["b_mod"] = inputs["b_mod"][0]
        m["w_in"] = inputs["w_in"][0]
        m["nab"] = nab
        for k in ("mu_prev", "mu_next", "k_k", "k_a", "gn_w", "gn_b", "w0", "a0", "g_up"):
            m[k] = np.ascontiguousarray(inputs[k][0])
        m["r_k"] = np.ascontiguousarray(inputs["r_k"][0].reshape(512))
        for k in ("w_pa", "w_pr", "w_o", "w_router", "ln1_g", "ln1_b", "ln2_g", "ln2_b", "w_e1", "w_e3", "w_e2"):
            m[k] = inputs[k][0]
        m["w_up"] = np.ascontiguousarray(inputs["w_up"][0].reshape(128, 512))
        m["a_up"] = np.ascontiguousarray(inputs["a_up"][0].reshape(128, 512))
        maps.append(m)
    return maps


def kernel(**inputs):
    inputs = {k: np.asarray(v) for k, v in inputs.items()}
    nc = build_program()
    maps = make_in_maps(inputs, host_consts())
    res = run_bass_kernel_spmd(nc, maps, core_ids=list(range(4)))
    return np.stack([r["out"] for r in res.results], 0).astype(np.float32)
```

```python
import numpy as np
import ml_dtypes
import concourse.bass as bass
import concourse.mybir as mybir
from concourse.bass_utils import run_bass_kernel_spmd

F32 = mybir.dt.float32
BF16 = mybir.dt.bfloat16
AF = mybir.ActivationFunctionType
ALU = mybir.AluOpType
AX = mybir.AxisListType

D = 1024
T = 8192
L = 256
TT = T + L
NT = T // 128
NTT = TT // 128
P_IN = 5504
RW0 = 1536
G0 = 3456
NE = 16
DE = 2816
CAP = 1024
ALPHA = 2.0 ** 0.25
LN_EPS = 1e-6
GN_EPS = 64e-5


class Buf:
    __slots__ = ("name", "t", "lw", "rs")

    def __init__(self, name, t):
        self.name, self.t, self.lw, self.rs = name, t, None, {}

    def __getitem__(self, idx):
        return self.t[idx]


class Sched:
    NDMA = 24

    def __init__(self, nc):
        self.nc = nc
        self.eng = {"pe": nc.tensor, "dve": nc.vector, "act": nc.scalar, "pool": nc.gpsimd, "sp": nc.sync}
        self.sem = {k: nc.alloc_semaphore(name=f"s_{k}") for k in self.eng}
        self.cnt = {k: 0 for k in self.eng}
        self.dsem = [nc.alloc_semaphore(name=f"d_{i}") for i in range(self.NDMA)]
        self.dcnt = [0] * self.NDMA
        self.dnext = 0
        self.semobj = dict(self.sem)
        for i, s in enumerate(self.dsem):
            self.semobj[("d", i)] = s
        self.seen = {k: {} for k in self.eng}
        self.ninst = 0
        self.nwaits = 0

    def _wait(self, e, key, val, same_ok=False):
        if key == e and same_ok:
            return
        if self.seen[e].get(key, 0) >= val:
            return
        self.eng[e].wait_ge(self.semobj[key], val)
        self.seen[e][key] = val
        self.nwaits += 1

    def _deps(self, e, reads, writes, pe_acc=False):
        for r in reads:
            if r.lw is not None:
                self._wait(e, r.lw[0], r.lw[1])
        for w in writes:
            if w.lw is not None:
                self._wait(e, w.lw[0], w.lw[1], same_ok=(pe_acc and e == "pe"))
            for k, v in w.rs.items():
                self._wait(e, k, v, same_ok=(k == e and e == "pe"))

    def _mark(self, key, val, reads, writes):
        for r in reads:
            r.rs[key] = val
        for w in writes:
            w.lw = (key, val)
            w.rs = {}

    def op(self, e, fn, reads=(), writes=(), pe_acc=False):
        self._deps(e, reads, writes, pe_acc)
        ins = fn(self.eng[e])
        self.cnt[e] += 1
        ins.then_inc(self.sem[e], 1)
        self._mark(e, self.cnt[e], reads, writes)
        self.ninst += 1
        return ins

    def dma(self, e, fn, reads=(), writes=()):
        i = self.dnext
        self.dnext = (self.dnext + 1) % self.NDMA
        key = ("d", i)
        if self.dcnt[i] > 0:
            self._wait(e, key, self.dcnt[i])
        self._deps(e, reads, writes)
        ins = fn(self.eng[e])
        self.dcnt[i] += 16
        ins.then_inc(self.dsem[i], 16)
        self._mark(key, self.dcnt[i], reads, writes)
        self.ninst += 1
        return ins

    def finish(self, e="sp"):
        for i in range(self.NDMA):
            if self.dcnt[i] > 0:
                self._wait(e, ("d", i), self.dcnt[i])
        for k in self.eng:
            if k != e and self.cnt[k] > 0:
                self._wait(e, k, self.cnt[k])


class Ctx:
    def __init__(self, nc):
        self.nc = nc
        self.S = Sched(nc)
        self.stack = []
        self.uid = 0

    def sb(self, name, shape, dt=F32):
        self.uid += 1
        cm = self.nc.sbuf_tensor(f"{name}_{self.uid}", list(shape), dt)
        t = cm.__enter__()
        self.stack.append(cm)
        return Buf(name, t)

    def ps(self, name, shape, dt=F32):
        self.uid += 1
        cm = self.nc.psum_tensor(f"{name}_{self.uid}", list(shape), dt)
        t = cm.__enter__()
        self.stack.append(cm)
        return Buf(name, t)

    def dram(self, name, shape, dt=F32):
        return Buf(name, self.nc.dram_tensor(name, list(shape), dt).ap())

    def mark(self):
        return len(self.stack)

    def release(self, m):
        self.barrier()
        while len(self.stack) > m:
            self.stack.pop().__exit__(None, None, None)

    def barrier(self):
        S = self.S
        for e in ("sp", "pe", "dve", "act", "pool"):
            S.finish(e)


def rr(lst, i):
    return lst[i % len(lst)]


def build_program(stop_after=None, dbg=None):
    nc = bass.Bass("TRN2", target_bir_lowering=False)
    C = Ctx(nc)
    S = C.S
    inp = lambda n, s, dt=F32: Buf(n, nc.dram_tensor(n, list(s), dt, kind="ExternalInput").ap())
    xin = inp("x", [TT, D])
    ccin = inp("cc", [2, D])
    w_mod = inp("w_mod", [D, 6 * D]); b_mod = inp("b_mod", [6 * D])
    w_in = inp("w_in", [D, P_IN])
    ident_d = inp("ident", [128, 128])
    rope_d = inp("rope", [T, 2, 2, 16])
    tri_d = inp("tri", [6, 128, 128])
    rwp = {k: inp(k, shp) for k, shp in [("mu_prev", [1920]), ("mu_next", [1920]), ("k_k", [512]), ("k_a", [512]), ("r_k", [512]),
                                          ("gn_w", [512]), ("gn_b", [512]), ("w0", [2, 512]), ("a0", [2, 512]),
                                          ("w_up", [128, 512]), ("a_up", [128, 512]), ("g_up", [128, 512])]}
    wD = None if stop_after not in (None, "F_short") else {k: inp(k, shp) for k, shp in [("w_pa", [512, D]), ("w_pr", [512, D]), ("w_o", [D, D]), ("w_router", [D, NE]),
                                        ("ln1_g", [D]), ("ln1_b", [D]), ("ln2_g", [D]), ("ln2_b", [D]),
                                        ("w_e1", [NE, D, DE]), ("w_e3", [NE, D, DE]), ("w_e2", [NE, DE, D])]}
    nab_d = inp("nab", [8, 128, 8, 256])
    out_d = Buf("out", nc.dram_tensor("out", [T, D], F32, kind="ExternalOutput").ap())
    zscr = C.dram("zscr", [TT, P_IN])
    modscr = C.dram("modscr", [2, 6 * D])

    ident = C.sb("ident", [128, 128]); identb = C.sb("identb", [128, 128], BF16)
    S.dma("sp", lambda e: e.dma_start(out=ident[:], in_=ident_d[:, :]), writes=[ident])
    S.op("dve", lambda e: e.tensor_copy(out=identb[:], in_=ident[:]), reads=[ident], writes=[identb])

    if stop_after == "F_short":
        ynascr = C.dram("ynascr", [T, 512]); yrwscr = C.dram("yrwscr", [T, 512])
        build_phase_def(nc, C, S, xin, zscr, modscr, ynascr, yrwscr, wD, identb, ident, out_d, stop_after)
        C.barrier()
        print("ninst", S.ninst, "nwaits", S.nwaits)
        return nc
    m0 = C.mark()
    scol = C.sb("scol", [128, 2, 8]); modrow = C.sb("modrow", [2, 6 * D]); brow = C.sb("brow", [2, 6 * D])
    S.dma("sp", lambda e: e.dma_start(out=scol[:], in_=ccin.t.rearrange("r (kc p) -> p r kc", p=128),
                                      allow_slow_non_contiguous=True), writes=[scol])
    S.dma("sp", lambda e: e.dma_start(out=brow[:], in_=b_mod.t.partition_broadcast(2)), writes=[brow])
    S.op("act", lambda e: e.activation(out=scol[:], in_=scol[:], func=AF.Silu), reads=[scol], writes=[scol])
    wm = [C.sb("wm", [128, 8, 512]) for _ in range(2)]
    pm = [C.ps("pm", [2, 512]) for _ in range(2)]
    wmv = w_mod.t.rearrange("(kc p) n -> p kc n", p=128)
    for cb in range(12):
        w, p = rr(wm, cb), rr(pm, cb)
        S.dma("sp", lambda e: e.dma_start(out=w[:], in_=wmv[:, :, cb * 512:(cb + 1) * 512]), writes=[w])
        for kc in range(8):
            S.op("pe", lambda e: e.matmul(p[:], lhsT=scol[:, :, kc], rhs=w[:, kc, :], start=(kc == 0), stop=(kc == 7)),
                 reads=[scol, w], writes=[p], pe_acc=(kc > 0))
        S.op("dve", lambda e: e.tensor_tensor(out=modrow[:, cb * 512:(cb + 1) * 512], in0=p[:], in1=brow[:, cb * 512:(cb + 1) * 512], op=ALU.add),
             reads=[p, brow], writes=[modrow])
    for sec in (1, 4):
        S.op("dve", lambda e: e.tensor_scalar_add(out=modrow[:, sec * D:(sec + 1) * D], in0=modrow[:, sec * D:(sec + 1) * D], scalar1=1.0),
             reads=[modrow], writes=[modrow])
    S.dma("sp", lambda e: e.dma_start(out=modscr[:, :], in_=modrow[:]), reads=[modrow], writes=[modscr])
    C.release(m0)

    def bcast(name, src_buf, src_ap, n=D, eng="sp"):
        b = C.sb(name, [128, n])
        S.dma(eng, lambda e: e.dma_start(out=b[:], in_=src_ap.partition_broadcast(128)), reads=[src_buf], writes=[b])
        return b

    def layer_norm_stats(xt, stats, mv, rstd, eps):
        for hf in range(2):
            S.op("dve", lambda e: e.bn_stats(out=stats[:, hf, :], in_=xt[:, hf * 512:(hf + 1) * 512]), reads=[xt], writes=[stats])
        S.op("dve", lambda e: e.bn_aggr(out=mv[:], in_=stats[:]), reads=[stats], writes=[mv])
        S.op("dve", lambda e: e.tensor_scalar_add(out=rstd[:], in0=mv[:, 1:2], scalar1=eps), reads=[mv], writes=[rstd])
        S.op("act", lambda e: e.activation(out=rstd[:], in_=rstd[:], func=AF.Ln), reads=[rstd], writes=[rstd])
        S.op("act", lambda e: e.activation(out=rstd[:], in_=rstd[:], func=AF.Exp, scale=-0.5), reads=[rstd], writes=[rstd])

    mA = C.mark()
    win_b = C.sb("win_b", [128, 8, P_IN], BF16)
    wst = [C.sb("wst", [128, 8, 512]) for _ in range(2)]
    winv = w_in.t.rearrange("(kc p) n -> p kc n", p=128)
    for cb in range(11):
        c0, c1 = cb * 512, min(P_IN, (cb + 1) * 512)
        w = rr(wst, cb)
        S.dma("sp", lambda e: e.dma_start(out=w[:, :, 0:c1 - c0], in_=winv[:, :, c0:c1]), writes=[w])
        S.op(rr(["dve", "pool"], cb), lambda e: e.tensor_copy(out=win_b[:, :, c0:c1], in_=w[:, :, 0:c1 - c0]), reads=[w], writes=[win_b])
    scA = [bcast("sc1", modscr, modscr[0, 1 * D:2 * D]), bcast("csc1", modscr, modscr[1, 1 * D:2 * D])]
    shA = [bcast("sh1", modscr, modscr[0, 0:D]), bcast("csh1", modscr, modscr[1, 0:D])]
    xt = [C.sb("xt", [128, D]) for _ in range(2)]
    ub = [C.sb("ub", [128, D], BF16) for _ in range(2)]
    uT = [C.sb("uT", [128, 8, 128], BF16) for _ in range(2)]
    zt = [C.sb("zt", [128, P_IN]) for _ in range(2)]
    stats = C.sb("stats", [128, 2, 6]); mv = C.sb("mv", [128, 2]); rstd = C.sb("rstd", [128, 1])
    rope = [C.sb("rope", [128, 2, 2, 16]) for _ in range(2)]
    rt = [C.sb("rt", [128, 16, 2, 16]) for _ in range(4)]
    ptp = [C.ps("ptp", [128, 8, 128], BF16) for _ in range(2)]
    pz = [C.ps("pz", [128, 512]) for _ in range(4)]
    tilesA = list(range(NTT))
    if stop_after == "A_short":
        tilesA = [0, 64]
    if stop_after == "B_short":
        tilesA = [0, 1, 2, 3, 4, 60, 61, 62, 63, 64, 65]
    if stop_after == "C_short":
        tilesA = [0, 1, 2, 61, 62, 63, 64, 65]
    if stop_after in ("C_feat", "C_one"):
        tilesA = [0, 1, 62, 63]
    for it, n in enumerate(tilesA):
        isctx = n >= NT
        x_, u_, uT_, z_, pt_ = rr(xt, it), rr(ub, it), rr(uT, it), rr(zt, it), rr(ptp, it)
        sc, sh = scA[isctx], shA[isctx]
        S.dma("sp", lambda e: e.dma_start(out=x_[:], in_=xin[n * 128:(n + 1) * 128, :]), reads=[xin], writes=[x_])
        layer_norm_stats(x_, stats, mv, rstd, LN_EPS)
        S.op("dve", lambda e: e.tensor_scalar(out=x_[:], in0=x_[:], scalar1=mv[:, 0:1], scalar2=rstd[:, 0:1], op0=ALU.subtract, op1=ALU.mult),
             reads=[x_, mv, rstd], writes=[x_])
        S.op("pool", lambda e: e.tensor_tensor(out=x_[:], in0=x_[:], in1=sc[:], op=ALU.mult), reads=[x_, sc], writes=[x_])
        S.op("pool", lambda e: e.tensor_tensor(out=u_[:], in0=x_[:], in1=sh[:], op=ALU.add), reads=[x_, sh], writes=[u_])
        for kc in range(8):
            S.op("pe", lambda e: e.transpose(out=pt_[:, kc, :], in_=u_[:, kc * 128:(kc + 1) * 128], identity=identb[:]),
                 reads=[u_, identb], writes=[pt_], pe_acc=True)
        S.op("act", lambda e: e.copy(out=uT_[:], in_=pt_[:]), reads=[pt_], writes=[uT_])
        for cb in range(11):
            c0, c1 = cb * 512, min(P_IN, (cb + 1) * 512)
            p = rr(pz, cb)
            for kc in range(8):
                S.op("pe", lambda e: e.matmul(p[:, 0:c1 - c0], lhsT=uT_[:, kc, :], rhs=win_b[:, kc, c0:c1], start=(kc == 0), stop=(kc == 7)),
                     reads=[uT_, win_b], writes=[p], pe_acc=(kc > 0))
            if cb % 2 == 0:
                S.op("act", lambda e: e.copy(out=z_[:, c0:c1], in_=p[:, 0:c1 - c0]), reads=[p], writes=[z_])
            else:
                S.op("dve", lambda e: e.tensor_copy(out=z_[:, c0:c1], in_=p[:, 0:c1 - c0]), reads=[p], writes=[z_])
        if not isctx:
            rp = rr(rope, it)
            S.dma("sp", lambda e: e.dma_start(out=rp[:], in_=rope_d[n * 128:(n + 1) * 128]), reads=[rope_d], writes=[rp])
            qk = z_.t[:, 0:1024].rearrange("p (h a b f) -> p h a b f", h=16, a=2, b=2)
            x1, x2 = qk[:, :, :, 0, :], qk[:, :, :, 1, :]
            cosb = rp.t[:, 0, :, :].unsqueeze(1).to_broadcast([128, 16, 2, 16])
            sinb = rp.t[:, 1, :, :].unsqueeze(1).to_broadcast([128, 16, 2, 16])
            t1, t2, t3, t4 = rt
            S.op("dve", lambda e: e.tensor_tensor(out=t1[:], in0=x1, in1=cosb, op=ALU.mult), reads=[z_, rp], writes=[t1])
            S.op("pool", lambda e: e.tensor_tensor(out=t2[:], in0=x2, in1=sinb, op=ALU.mult), reads=[z_, rp], writes=[t2])
            S.op("dve", lambda e: e.tensor_tensor(out=t3[:], in0=x2, in1=cosb, op=ALU.mult), reads=[z_, rp], writes=[t3])
            S.op("pool", lambda e: e.tensor_tensor(out=t4[:], in0=x1, in1=sinb, op=ALU.mult), reads=[z_, rp], writes=[t4])
            S.op("dve", lambda e: e.tensor_tensor(out=x1, in0=t1[:], in1=t2[:], op=ALU.subtract), reads=[t1, t2], writes=[z_])
            S.op("dve", lambda e: e.tensor_tensor(out=x2, in0=t3[:], in1=t4[:], op=ALU.add), reads=[t3, t4], writes=[z_])
        S.dma("sp", lambda e: e.dma_start(out=zscr[n * 128:(n + 1) * 128, :], in_=z_[:]), reads=[z_], writes=[zscr])
    C.release(mA)
    if stop_after in ("A", "A_short"):
        if dbg is not None:
            d = Buf("dbg", nc.dram_tensor("dbg", [256, P_IN], F32, kind="ExternalOutput").ap())
            S.dma("sp", lambda e: e.dma_start(out=d[0:128, :], in_=zscr[0:128, :]), reads=[zscr], writes=[d])
            S.dma("sp", lambda e: e.dma_start(out=d[128:256, :], in_=zscr[T:T + 128, :]), reads=[zscr], writes=[d])
        C.barrier()
        print("ninst", S.ninst, "nwaits", S.nwaits)
        return nc

    ynascr = C.dram("ynascr", [T, 512])
    if stop_after in ("C_short", "C_feat", "C_one"):
        return build_phase_c(nc, C, S, zscr, ident, tri_d, rwp, stop_after)
    mB = C.mark()
    KT = C.sb("KT", [128, 4, TT], BF16)
    mB0 = C.mark()
    kst = [C.sb("kst", [128, 512]) for _ in range(2)]
    kb = [C.sb("kb", [128, 512], BF16) for _ in range(2)]
    pkt = [C.ps("pkt", [128, 4, 128], BF16) for _ in range(2)]
    for it, n in enumerate(tilesA if stop_after == "B_short" else range(NTT)):
        ks_, kb_, pk_ = rr(kst, it), rr(kb, it), rr(pkt, it)
        S.dma("sp", lambda e: e.dma_start(out=ks_[:], in_=zscr[n * 128:(n + 1) * 128, 512:1024]), reads=[zscr], writes=[ks_])
        S.op("dve", lambda e: e.tensor_copy(out=kb_[:], in_=ks_[:]), reads=[ks_], writes=[kb_])
        for hp in range(4):
            S.op("pe", lambda e: e.transpose(out=pk_[:, hp, :], in_=kb_[:, hp * 128:(hp + 1) * 128], identity=identb[:]),
                 reads=[kb_, identb], writes=[pk_], pe_acc=True)
        S.op("act", lambda e: e.copy(out=KT[:, :, n * 128:(n + 1) * 128], in_=pk_[:]), reads=[pk_], writes=[KT])
    C.release(mB0)
    ebst = C.sb("ebst", [128, 8, 256])
    EBi = C.sb("EBi", [128, 8, 256], BF16); EBe = C.sb("EBe", [128, 8, 256], BF16)

    def load_eb(qi, dst):
        S.dma("sp", lambda e: e.dma_start(out=ebst[:], in_=nab_d[qi]), reads=[nab_d], writes=[ebst])
        S.op("act", lambda e: e.activation(out=dst[:], in_=ebst[:], func=AF.Exp), reads=[ebst], writes=[dst])
    load_eb(4, EBi)
    vcst = C.sb("vcst", [128, 2, 512]); VCa = C.sb("VCa", [128, 2, 8, 65], BF16)
    S.dma("sp", lambda e: e.dma_start(out=vcst[:], in_=zscr.t[T:TT, 1024:1536].rearrange("(j p) c -> p j c", p=128)), reads=[zscr], writes=[vcst])
    S.op("pool", lambda e: e.memset(VCa[:], 1.0), writes=[VCa])
    S.op("dve", lambda e: e.tensor_copy(out=VCa[:, :, :, 0:64], in_=vcst.t[:].rearrange("p j (h d) -> p j h d", h=8)), reads=[vcst], writes=[VCa])
    qst = [C.sb("qst", [64, 512]) for _ in range(2)]; qb = [C.sb("qb", [64, 512], BF16) for _ in range(2)]
    QTr = [C.sb("QTr", [128, 4, 64], BF16) for _ in range(2)]
    vst = [C.sb("vst", [128, 4, 512]) for _ in range(2)]
    Va = [C.sb("Va", [128, 4, 8, 65], BF16) for _ in range(2)]
    for v_ in Va:
        S.op("pool", lambda e: e.memset(v_[:], 1.0), writes=[v_])
    PT = [C.sb("PT", [128, 384], BF16) for _ in range(3)]
    yrow = [C.sb("yrow", [64, 8, 64]) for _ in range(2)]
    rec = [C.sb("rec", [64, 8, 1]) for _ in range(2)]
    pq = C.ps("pq", [128, 4, 64], BF16)
    pss = [C.ps("pss", [128, 384]) for _ in range(3)]
    po = [[C.ps("po", [64, 4, 65]) for _ in range(2)] for _ in range(2)]
    rowsB = list(range(128))
    if stop_after == "B_short":
        rowsB = [0, 1, 2, 3, 4, 5, 125, 127]
    cur_edge = None
    cnt = 0
    for ir, i in enumerate(rowsB):
        rs_ = min(max(i - 4, 0), 120); qi = i - rs_
        if qi == 4:
            EB = EBi
        else:
            if cur_edge != qi:
                load_eb(qi, EBe); cur_edge = qi
            EB = EBe
        qs_, qb_, qt_, vs_, va_, yr_, rc_, po_ = rr(qst, ir), rr(qb, ir), rr(QTr, ir), rr(vst, ir), rr(Va, ir), rr(yrow, ir), rr(rec, ir), rr(po, ir)
        S.dma("sp", lambda e: e.dma_start(out=qs_[:], in_=zscr[i * 64:(i + 1) * 64, 0:512]), reads=[zscr], writes=[qs_])
        S.dma("sp", lambda e: e.dma_start(out=vs_[:], in_=zscr.t[rs_ * 64:rs_ * 64 + 512, 1024:1536].rearrange("(j p) c -> p j c", p=128)),
              reads=[zscr], writes=[vs_])
        S.op("dve", lambda e: e.tensor_copy(out=qb_[:], in_=qs_[:]), reads=[qs_], writes=[qb_])
        for hp in range(4):
            S.op("pe", lambda e: e.transpose(out=pq[:, hp, :], in_=qb_[:, hp * 128:(hp + 1) * 128], identity=identb[0:64, 0:64]),
                 reads=[qb_, identb], writes=[pq], pe_acc=True)
        S.op("act", lambda e: e.copy(out=qt_[:], in_=pq[:]), reads=[pq], writes=[qt_])
        S.op("pool", lambda e: e.tensor_copy(out=va_[:, :, :, 0:64], in_=vs_.t[:].rearrange("p j (h d) -> p j h d", h=8)), reads=[vs_], writes=[va_])
        for h in range(8):
            hp, j2 = divmod(h, 2)
            pr = slice(j2 * 64, (j2 + 1) * 64)
            ps_, pt_ = rr(pss, cnt), rr(PT, cnt); cnt += 1
            for jt in range(4):
                k0 = rs_ * 64 + jt * 128
                S.op("pe", lambda e: e.matmul(ps_[:, jt * 64:(jt + 1) * 64], lhsT=KT[pr, hp, k0:k0 + 128], rhs=qt_[pr, hp, :], start=True, stop=True),
                     reads=[KT, qt_], writes=[ps_], pe_acc=(jt > 0))
            for jc in range(2):
                k0 = T + jc * 128
                S.op("pe", lambda e: e.matmul(ps_[:, 256 + jc * 64:256 + (jc + 1) * 64], lhsT=KT[pr, hp, k0:k0 + 128], rhs=qt_[pr, hp, :], start=True, stop=True),
                     reads=[KT, qt_], writes=[ps_], pe_acc=True)
            S.op("act", lambda e: e.activation(out=pt_[:], in_=ps_[:], func=AF.Exp, scale=0.125), reads=[ps_], writes=[pt_])
            S.op("dve", lambda e: e.tensor_tensor(out=pt_[:, 0:256], in0=pt_[:, 0:256], in1=EB[:, h, :], op=ALU.mult), reads=[pt_, EB], writes=[pt_])
            pot = po_[h // 4]
            for jt in range(4):
                S.op("pe", lambda e: e.matmul(pot[:, h % 4, :], lhsT=pt_[:, jt * 64:(jt + 1) * 64], rhs=va_[:, jt, h, :], start=(jt == 0), stop=False),
                     reads=[pt_, va_], writes=[pot], pe_acc=(jt > 0 or h % 4 > 0))
            for jc in range(2):
                S.op("pe", lambda e: e.matmul(pot[:, h % 4, :], lhsT=pt_[:, 256 + jc * 64:256 + (jc + 1) * 64], rhs=VCa[:, jc, h, :], start=False, stop=(jc == 1)),
                     reads=[pt_, VCa], writes=[pot], pe_acc=True)
        for hh in range(2):
            pot = po_[hh]
            S.op("dve", lambda e: e.reciprocal(out=rc_[:, hh * 4:(hh + 1) * 4, :], in_=pot[:, :, 64:65]), reads=[pot], writes=[rc_])
            S.op("dve", lambda e: e.tensor_tensor(out=yr_[:, hh * 4:(hh + 1) * 4, :], in0=pot[:, :, 0:64],
                                                  in1=rc_[:, hh * 4:(hh + 1) * 4, :].to_broadcast([64, 4, 64]), op=ALU.mult),
                 reads=[pot, rc_], writes=[yr_])
        S.dma("sp", lambda e: e.dma_start(out=ynascr[i * 64:(i + 1) * 64, :], in_=yr_.t[:].rearrange("p h d -> p (h d)")), reads=[yr_], writes=[ynascr])
    C.release(mB)
    if stop_after == "B_short":
        d = Buf("dbg", nc.dram_tensor("dbg", [8 * 64, 512], F32, kind="ExternalOutput").ap())
        for ir, i in enumerate(rowsB):
            S.dma("sp", lambda e: e.dma_start(out=d[ir * 64:(ir + 1) * 64, :], in_=ynascr[i * 64:(i + 1) * 64, :]), reads=[ynascr], writes=[d])
        C.barrier()
        print("ninst", S.ninst, "nwaits", S.nwaits)
        return nc
    yrwscr = build_phase_c(nc, C, S, zscr, ident, tri_d, rwp, stop_after)
    build_phase_def(nc, C, S, xin, zscr, modscr, ynascr, yrwscr, wD, identb, ident, out_d, stop_after)
    C.barrier()
    print("ninst", S.ninst, "nwaits", S.nwaits)
    return nc


def build_phase_c(nc, C, S, zscr, ident, tri_d, rwp, stop_after):
    yfscr = C.dram("yfscr", [T, 512]); yrwscr = C.dram("yrwscr", [T, 512])
    short = stop_after == "C_short"
    mC = C.mark()
    US, LS, UI, LI, SEL127, SEL0 = [C.sb(f"tri{i}", [128, 128]) for i in range(6)]
    for i, b in enumerate([US, LS, UI, LI, SEL127, SEL0]):
        S.dma("sp", lambda e: e.dma_start(out=b[:], in_=tri_d[i]), reads=[tri_d], writes=[b])

    def bc(key, ap, n):
        b = C.sb(key, [128, n])
        S.dma("sp", lambda e: e.dma_start(out=b[:], in_=ap.partition_broadcast(128)), reads=[rwp[key.split("#")[0]]], writes=[b])
        return b
    mu_p = bc("mu_prev", rwp["mu_prev"].t, 1920); mu_n = bc("mu_next", rwp["mu_next"].t, 1920)
    k_k = bc("k_k", rwp["k_k"].t, 512); k_a = bc("k_a", rwp["k_a"].t, 512); r_k = bc("r_k", rwp["r_k"].t, 512)
    gn_w = bc("gn_w", rwp["gn_w"].t, 512); gn_b = bc("gn_b", rwp["gn_b"].t, 512)
    w0 = [bc(f"w0#{d}", rwp["w0"][d], 512) for d in range(2)]; a0 = [bc(f"a0#{d}", rwp["a0"][d], 512) for d in range(2)]
    WUP = C.sb("WUP", [128, 512]); AUP = C.sb("AUP", [128, 512]); GUP = C.sb("GUP", [128, 512])
    for b, k in [(WUP, "w_up"), (AUP, "a_up"), (GUP, "g_up")]:
        S.dma("sp", lambda e: e.dma_start(out=b[:], in_=rwp[k][:, :]), reads=[rwp[k]], writes=[b])
    S0all = C.sb("S0all", [128, 64, 8])
    zc = C.sb("zc", [128, 1920]); zp = C.sb("zp", [128, 1920]); zn = C.sb("zn", [128, 1920]); zs = C.sb("zs", [128, 1920])
    lin = C.sb("lin", [128, 384]); LT = C.sb("LT", [128, 3, 128])
    f = {k: C.sb(k, [128, 512]) for k in ["kk", "logw", "a", "kmod", "g", "rrk", "tmp", "cum", "ep", "em", "eex",
                                          "At", "Rt", "Bt", "Kt", "Bh", "Kh", "pCb", "ysum", "yout"]}
    ss = C.sb("ss", [128, 8]); sd = C.sb("sd", [128, 8]); gmv = C.sb("gmv", [128, 8, 2]); gst = C.sb("gst", [128, 8, 6])
    TTt = C.sb("TTt", [128, 4, 4, 128])
    Dg = C.sb("Dg", [64, 8, 64])
    Nm = C.sb("Nm", [128, 128])
    NP = [C.sb("NP", [128, 2, 128]) for _ in range(2)]
    PRB = C.sb("PRB", [128, 2, 128]); AKRK = C.sb("AKRK", [128, 2, 128])
    X = [C.sb("X", [128, 128]) for _ in range(2)]
    McT = C.sb("McT", [64, 64]); NcS = C.sb("NcS", [64, 64]); QT = C.sb("QT", [64, 128])
    Hst = C.sb("Hst", [64, 8, 64])
    M2 = [C.sb("M2", [128, 2, 128]) for _ in range(2)]
    for d, (st, inc) in enumerate([(US, UI), (LS, LI)]):
        S.op("pool", lambda e: e.tensor_copy(out=M2[d][:, 0, :], in_=st[:]), reads=[st], writes=[M2[d]])
        S.op("pool", lambda e: e.tensor_copy(out=M2[d][:, 1, :], in_=inc[:]), reads=[inc], writes=[M2[d]])
    pbig = [C.ps("pbig", [128, 512]) for _ in range(2)]
    pT = C.ps("pT", [128, 4, 128])
    pA = C.ps("pA", [128, 2, 128]); pB = C.ps("pB", [128, 2, 128])
    pX = C.ps("pX", [128, 2, 128])
    pM = C.ps("pM", [64, 4, 64])
    pY = C.ps("pY", [128, 512])
    v3 = lambda b: b.t[:].rearrange("p (h d) -> p h d", h=8)
    NEG = -float(np.exp(-0.5))

    def features(n, d):
        t0 = n * 128
        first = n in (0, NT); lastt = n in (NT - 1, NTT - 1)
        S.dma("sp", lambda e: e.dma_start(out=zc[:], in_=zscr[t0:t0 + 128, RW0:RW0 + 1920]), reads=[zscr], writes=[zc])
        if first:
            S.op("pool", lambda e: e.memset(zp[:], 0.0), writes=[zp])
            S.dma("sp", lambda e: e.dma_start(out=zp[1:128, :], in_=zscr[t0:t0 + 127, RW0:RW0 + 1920]), reads=[zscr], writes=[zp])
        else:
            S.dma("sp", lambda e: e.dma_start(out=zp[:], in_=zscr[t0 - 1:t0 + 127, RW0:RW0 + 1920]), reads=[zscr], writes=[zp])
        if lastt:
            S.op("pool", lambda e: e.memset(zn[:], 0.0), writes=[zn])
            S.dma("sp", lambda e: e.dma_start(out=zn[0:127, :], in_=zscr[t0 + 1:t0 + 128, RW0:RW0 + 1920]), reads=[zscr], writes=[zn])
        else:
            S.dma("sp", lambda e: e.dma_start(out=zn[:], in_=zscr[t0 + 1:t0 + 129, RW0:RW0 + 1920]), reads=[zscr], writes=[zn])
        S.op("dve", lambda e: e.tensor_tensor(out=zp[:], in0=zp[:], in1=zc[:], op=ALU.subtract), reads=[zp, zc], writes=[zp])
        S.op("pool", lambda e: e.tensor_tensor(out=zn[:], in0=zn[:], in1=zc[:], op=ALU.subtract), reads=[zn, zc], writes=[zn])
        S.op("dve", lambda e: e.tensor_tensor(out=zp[:], in0=zp[:], in1=mu_p[:], op=ALU.mult), reads=[zp, mu_p], writes=[zp])
        S.op("pool", lambda e: e.tensor_tensor(out=zn[:], in0=zn[:], in1=mu_n[:], op=ALU.mult), reads=[zn, mu_n], writes=[zn])
        S.op("dve", lambda e: e.tensor_tensor(out=zs[:], in0=zc[:], in1=zp[:], op=ALU.add), reads=[zc, zp], writes=[zs])
        S.op("dve", lambda e: e.tensor_tensor(out=zs[:], in0=zs[:], in1=zn[:], op=ALU.add), reads=[zs, zn], writes=[zs])
        S.op("act", lambda e: e.activation(out=lin[:, 0:128], in_=zs[:, 1536:1664], func=AF.Tanh), reads=[zs], writes=[lin])
        S.op("act", lambda e: e.copy(out=lin[:, 128:256], in_=zs[:, 1664:1792]), reads=[zs], writes=[lin])
        S.op("act", lambda e: e.activation(out=lin[:, 256:384], in_=zs[:, 1792:1920], func=AF.Sigmoid), reads=[zs], writes=[lin])
        for j in range(3):
            S.op("pe", lambda e: e.transpose(out=pT[:, j, :], in_=lin[:, j * 128:(j + 1) * 128], identity=ident[:]), reads=[lin, ident], writes=[pT], pe_acc=True)
        S.op("act", lambda e: e.copy(out=LT[:], in_=pT[:, 0:3, :]), reads=[pT], writes=[LT])
        dp = slice(d * 64, (d + 1) * 64)
        S.op("pe", lambda e: e.matmul(pbig[0][:], lhsT=LT[dp, 0, :], rhs=WUP[dp, :], start=True, stop=True), reads=[LT, WUP], writes=[pbig[0]])
        S.op("dve", lambda e: e.tensor_tensor(out=f["logw"][:], in0=pbig[0][:], in1=w0[d][:], op=ALU.add), reads=[pbig[0], w0[d]], writes=[f["logw"]])
        S.op("act", lambda e: e.activation(out=f["logw"][:], in_=f["logw"][:], func=AF.Sigmoid), reads=[f["logw"]], writes=[f["logw"]])
        S.op("pool", lambda e: e.tensor_scalar_mul(out=f["logw"][:], in0=f["logw"][:], scalar1=NEG), reads=[f["logw"]], writes=[f["logw"]])
        S.op("pe", lambda e: e.matmul(pbig[1][:], lhsT=LT[dp, 1, :], rhs=AUP[dp, :], start=True, stop=True), reads=[LT, AUP], writes=[pbig[1]])
        S.op("dve", lambda e: e.tensor_tensor(out=f["a"][:], in0=pbig[1][:], in1=a0[d][:], op=ALU.add), reads=[pbig[1], a0[d]], writes=[f["a"]])
        S.op("act", lambda e: e.activation(out=f["a"][:], in_=f["a"][:], func=AF.Sigmoid), reads=[f["a"]], writes=[f["a"]])
        S.op("pe", lambda e: e.matmul(pbig[0][:], lhsT=LT[:, 2, :], rhs=GUP[:], start=True, stop=True), reads=[LT, GUP], writes=[pbig[0]])
        S.op("act", lambda e: e.copy(out=f["g"][:], in_=pbig[0][:]), reads=[pbig[0]], writes=[f["g"]])
        S.op("dve", lambda e: e.tensor_tensor(out=f["kk"][:], in0=zs[:, 512:1024], in1=k_k[:], op=ALU.mult), reads=[zs, k_k], writes=[f["kk"]])
        S.op("pool", lambda e: e.tensor_tensor(out=f["tmp"][:], in0=f["kk"][:], in1=f["kk"][:], op=ALU.mult), reads=[f["kk"]], writes=[f["tmp"]])
        S.op("dve", lambda e: e.tensor_reduce(out=ss[:], in_=v3(f["tmp"]), axis=AX.X, op=ALU.add), reads=[f["tmp"]], writes=[ss])
        S.op("dve", lambda e: e.tensor_scalar_add(out=ss[:], in0=ss[:], scalar1=1e-12), reads=[ss], writes=[ss])
        S.op("act", lambda e: e.activation(out=ss[:], in_=ss[:], func=AF.Ln), reads=[ss], writes=[ss])
        S.op("act", lambda e: e.activation(out=ss[:], in_=ss[:], func=AF.Exp, scale=-0.5), reads=[ss], writes=[ss])
        S.op("dve", lambda e: e.tensor_tensor(out=v3(f["kk"]), in0=v3(f["kk"]), in1=ss.t[:].unsqueeze(2).to_broadcast([128, 8, 64]), op=ALU.mult),
             reads=[f["kk"], ss], writes=[f["kk"]])
        S.op("dve", lambda e: e.scalar_tensor_tensor(out=f["kmod"][:], in0=f["a"][:], scalar=-1.0, in1=k_a[:], op0=ALU.add, op1=ALU.mult),
             reads=[f["a"], k_a], writes=[f["kmod"]])
        S.op("dve", lambda e: e.scalar_tensor_tensor(out=f["kmod"][:], in0=f["kmod"][:], scalar=1.0, in1=zs[:, 512:1024], op0=ALU.add, op1=ALU.mult),
             reads=[f["kmod"], zs], writes=[f["kmod"]])
        S.op("pool", lambda e: e.tensor_tensor(out=f["rrk"][:], in0=zs[:, 0:512], in1=r_k[:], op=ALU.mult), reads=[zs, r_k], writes=[f["rrk"]])
        S.op("pool", lambda e: e.tensor_tensor(out=f["tmp"][:], in0=f["rrk"][:], in1=f["kmod"][:], op=ALU.mult), reads=[f["rrk"], f["kmod"]], writes=[f["tmp"]])
        S.op("dve", lambda e: e.tensor_reduce(out=sd[:], in_=v3(f["tmp"]), axis=AX.X, op=ALU.add), reads=[f["tmp"]], writes=[sd])

    def chunk(n, d, emit, nheads=8, stage=6):
        Lm, maskN, sel = ((UI, LS, SEL127), (LI, US, SEL0))[d]
        m2 = M2[d]
        r_ = zs.t[:, 0:512]; v_ = zs.t[:, 1024:1536]
        S.op("pe", lambda e: e.matmul(pbig[1][:], lhsT=Lm[:], rhs=f["logw"][:], start=True, stop=True), reads=[Lm, f["logw"]], writes=[pbig[1]])
        S.op("act", lambda e: e.activation(out=f["ep"][:], in_=pbig[1][:], func=AF.Exp), reads=[pbig[1]], writes=[f["ep"]])
        S.op("act", lambda e: e.activation(out=f["em"][:], in_=pbig[1][:], func=AF.Exp, scale=-1.0), reads=[pbig[1]], writes=[f["em"]])
        S.op("dve", lambda e: e.tensor_tensor(out=f["cum"][:], in0=pbig[1][:], in1=f["logw"][:], op=ALU.subtract), reads=[pbig[1], f["logw"]], writes=[f["cum"]])
        S.op("act", lambda e: e.activation(out=f["eex"][:], in_=f["cum"][:], func=AF.Exp), reads=[f["cum"]], writes=[f["eex"]])
        S.op("dve", lambda e: e.scalar_tensor_tensor(out=f["At"][:], in0=f["kk"][:], scalar=-1.0, in1=f["eex"][:], op0=ALU.mult, op1=ALU.mult),
             reads=[f["kk"], f["eex"]], writes=[f["At"]])
        S.op("pool", lambda e: e.tensor_tensor(out=f["Rt"][:], in0=r_, in1=f["ep"][:], op=ALU.mult), reads=[zs, f["ep"]], writes=[f["Rt"]])
        S.op("dve", lambda e: e.tensor_tensor(out=f["Bt"][:], in0=f["kk"][:], in1=f["a"][:], op=ALU.mult), reads=[f["kk"], f["a"]], writes=[f["Bt"]])
        S.op("dve", lambda e: e.tensor_tensor(out=f["Bt"][:], in0=f["Bt"][:], in1=f["em"][:], op=ALU.mult), reads=[f["Bt"], f["em"]], writes=[f["Bt"]])
        S.op("pool", lambda e: e.tensor_tensor(out=f["Kt"][:], in0=f["kmod"][:], in1=f["em"][:], op=ALU.mult), reads=[f["kmod"], f["em"]], writes=[f["Kt"]])
        S.op("pe", lambda e: e.matmul(pbig[0][:], lhsT=sel[:], rhs=f["ep"][:], start=True, stop=True), reads=[sel, f["ep"]], writes=[pbig[0]])
        S.op("act", lambda e: e.copy(out=f["pCb"][:], in_=pbig[0][:]), reads=[pbig[0]], writes=[f["pCb"]])
        S.op("dve", lambda e: e.tensor_tensor(out=f["Bh"][:], in0=f["Bt"][:], in1=f["pCb"][:], op=ALU.mult), reads=[f["Bt"], f["pCb"]], writes=[f["Bh"]])
        S.op("pool", lambda e: e.tensor_tensor(out=f["Kh"][:], in0=f["Kt"][:], in1=f["pCb"][:], op=ALU.mult), reads=[f["Kt"], f["pCb"]], writes=[f["Kh"]])
        S.op("dve", lambda e: e.tensor_tensor(out=Dg[:], in0=f["pCb"].t[0:64, :].rearrange("p (h d) -> p h d", h=8),
                                              in1=ident.t[0:64, 0:64].unsqueeze(1).to_broadcast([64, 8, 64]), op=ALU.mult),
             reads=[f["pCb"], ident], writes=[Dg])
        for ai, key in enumerate(["At", "Rt", "Bt", "Kt"]):
            for hp in range(4):
                S.op("pe", lambda e: e.transpose(out=pT[:, hp, :], in_=f[key][:, hp * 128:(hp + 1) * 128], identity=ident[:]),
                     reads=[f[key], ident], writes=[pT], pe_acc=True)
            S.op(("act", "dve")[ai % 2], (lambda e: e.copy(out=TTt[:, :, ai, :], in_=pT[:])) if ai % 2 == 0 else
                 (lambda e: e.tensor_copy(out=TTt[:, :, ai, :], in_=pT[:])), reads=[pT], writes=[TTt])
        for h in range(nheads if stage >= 2 else 0):
            hp, j2 = divmod(h, 2)
            pr = slice(j2 * 64, (j2 + 1) * 64); hs = slice(h * 64, (h + 1) * 64)
            AR = TTt[pr, hp, 0:2, :]; AtT = TTt[pr, hp, 0, :]; BtT = TTt[pr, hp, 2, :]; KtT = TTt[pr, hp, 3, :]
            S.op("pe", lambda e: e.matmul(pA[:], lhsT=BtT, rhs=AR, start=True, stop=True), reads=[TTt], writes=[pA])
            S.op("pe", lambda e: e.matmul(pB[:], lhsT=KtT, rhs=AR, start=True, stop=True), reads=[TTt], writes=[pB])
            S.op("pe", lambda e: e.matmul(pX[:, 0, :], lhsT=AtT, rhs=BtT, start=True, stop=True), reads=[TTt], writes=[pX])
            S.op("dve", lambda e: e.tensor_tensor(out=PRB[:], in0=pA[:], in1=m2[:], op=ALU.mult), reads=[pA, m2], writes=[PRB])
            S.op("dve", lambda e: e.tensor_tensor(out=AKRK[:], in0=pB[:], in1=m2[:], op=ALU.mult), reads=[pB, m2], writes=[AKRK])
            S.op("dve", lambda e: e.tensor_tensor(out=NP[0][:, 0, :], in0=pX[:, 0, :], in1=maskN[:], op=ALU.mult), reads=[pX, maskN], writes=[NP[0]])
            S.op("dve", lambda e: e.tensor_copy(out=NP[0][:, 1, :], in_=PRB[:, 0, :]), reads=[PRB], writes=[NP[0]])
            S.op("pe", lambda e: e.matmul(pX[:, 1, 0:64], lhsT=AKRK[:, 0, :], rhs=v_[:, hs], start=True, stop=True), reads=[AKRK, zs], writes=[pX])
            S.op("dve", lambda e: e.tensor_copy(out=X[0][:, 0:64], in_=f["At"][:, hs]), reads=[f["At"]], writes=[X[0]])
            S.op("act", lambda e: e.copy(out=X[0][:, 64:128], in_=pX[:, 1, 0:64]), reads=[pX], writes=[X[0]])
            for i in range(7 if stage >= 3 else 0):
                npi, npn = NP[i % 2], NP[(i + 1) % 2]
                xi, xn = X[i % 2], X[(i + 1) % 2]
                S.op("pe", lambda e: e.matmul(pX[:, 0, :], lhsT=npi[:, 1, :], rhs=xi[:], start=True, stop=True), reads=[npi, xi], writes=[pX])
                S.op("dve", lambda e: e.tensor_tensor(out=xn[:], in0=pX[:, 0, :], in1=xi[:], op=ALU.add), reads=[pX, xi], writes=[xn])
                if i < 6:
                    S.op("pe", lambda e: e.matmul(pA[:, 0, :], lhsT=npi[:, 1, :], rhs=npi[:, 0, :], start=True, stop=True), reads=[npi], writes=[pA])
                    S.op("pe", lambda e: e.matmul(pA[:, 1, :], lhsT=npi[:, 0, :], rhs=npi[:, 1, :], start=True, stop=True), reads=[npi], writes=[pA], pe_acc=True)
                    S.op("act", lambda e: e.copy(out=npn[:], in_=pA[:]), reads=[pA], writes=[npn])
            Xf = X[1]
            if stage < 4:
                continue
            G = Xf[:, 0:64]; Ul = Xf[:, 64:128]
            sub = 0
            if sub in (0, 1):
                S.op("pe", lambda e: e.matmul(pM[:, 0, :], lhsT=G, rhs=f["Bh"][:, hs], start=True, stop=True), reads=[Xf, f["Bh"]], writes=[pM])
            if sub in (0, 2):
                S.op("pe", lambda e: e.matmul(pM[:, 1, :], lhsT=f["Bh"][:, hs], rhs=Ul, start=True, stop=True), reads=[Xf, f["Bh"]], writes=[pM], pe_acc=True)
                S.op("pe", lambda e: e.matmul(pM[:, 3, :], lhsT=f["Kh"][:, hs], rhs=v_[:, hs], start=True, stop=True), reads=[f["Kh"], zs], writes=[pM], pe_acc=True)
            if sub in (0, 1):
                S.op("dve", lambda e: e.tensor_tensor(out=McT[:], in0=pM[:, 0, :], in1=Dg[:, h, :], op=ALU.add), reads=[pM, Dg], writes=[McT])
            if sub in (0, 2):
                S.op("dve", lambda e: e.tensor_copy(out=NcS[:], in_=pM[:, 1, :]), reads=[pM], writes=[NcS])
                S.op("dve", lambda e: e.tensor_tensor(out=NcS[:], in0=pM[:, 3, :], in1=NcS[:], op=ALU.add), reads=[pM, NcS], writes=[NcS])
            if emit and stage >= 5:
                S.op("pe", lambda e: e.matmul(pB[0:64, 0, :], lhsT=G, rhs=PRB[:, 1, :], start=True, stop=True), reads=[Xf, PRB], writes=[pB])
                S.op("pe", lambda e: e.matmul(pB[0:64, 1, :], lhsT=f["Rt"][:, hs], rhs=ident[:], start=True, stop=True), reads=[f["Rt"], ident], writes=[pB], pe_acc=True)
                S.op("dve", lambda e: e.tensor_copy(out=QT[:], in_=pB[0:64, 0, :]), reads=[pB], writes=[QT])
                S.op("dve", lambda e: e.tensor_tensor(out=QT[:], in0=pB[0:64, 1, :], in1=QT[:], op=ALU.add), reads=[pB, QT], writes=[QT])
                S.op("pe", lambda e: e.matmul(pY[:, hs], lhsT=QT[:], rhs=Hst[:, h, :], start=True, stop=False), reads=[QT, Hst], writes=[pY], pe_acc=(h > 0))
                S.op("pe", lambda e: e.matmul(pY[:, hs], lhsT=PRB[:, 1, :], rhs=Ul, start=False, stop=False), reads=[PRB, Xf], writes=[pY], pe_acc=True)
                S.op("pe", lambda e: e.matmul(pY[:, hs], lhsT=AKRK[:, 1, :], rhs=v_[:, hs], start=False, stop=True), reads=[AKRK, zs], writes=[pY], pe_acc=True)
            if stage < 6:
                continue
            S.op("pe", lambda e: e.matmul(pM[:, 2, :], lhsT=McT[:], rhs=Hst[:, h, :], start=True, stop=True), reads=[McT, Hst], writes=[pM])
            S.op("dve", lambda e: e.tensor_tensor(out=Hst[:, h, :], in0=pM[:, 2, :], in1=NcS[:], op=ALU.add), reads=[pM, NcS], writes=[Hst])

    if stop_after == "C_feat":
        dd = Buf("dbg", nc.dram_tensor("dbg", [2, 5, 128, 512], F32, kind="ExternalOutput").ap())
        for ti, (n, d) in enumerate([(0, 0), (63, 1)]):
            features(n, d)
            for ki, key in enumerate(["logw", "a", "kmod", "kk", "g"]):
                S.dma("sp", lambda e: e.dma_start(out=dd[ti, ki], in_=f[key][:]), reads=[f[key]], writes=[dd])
        C.barrier()
        print("ninst", S.ninst, "nwaits", S.nwaits)
        return nc
    if stop_after == "C_one":
        nh = 8
        dd = Buf("dbg", nc.dram_tensor("dbg", [128, 512], F32, kind="ExternalOutput").ap())
        dh = Buf("dbgh", nc.dram_tensor("dbgh", [64, 512], F32, kind="ExternalOutput").ap())
        S.op("pool", lambda e: e.memset(Hst[:], 0.0), writes=[Hst])
        features(0, 0)
        stg = 6
        chunk(0, 0, True, nheads=nh, stage=stg)
        if stg >= 5:
            S.op("act", lambda e: e.copy(out=f["yout"][:, 0:nh * 64], in_=pY[:, 0:nh * 64]), reads=[pY], writes=[f["yout"]])
        else:
            src = {1: f["Bh"], 2: X[0], 3: X[1], 4: X[1]}[stg]
            S.op("act", lambda e: e.copy(out=f["yout"][:, 0:128], in_=src[:, 0:128]), reads=[src], writes=[f["yout"]])
        S.dma("sp", lambda e: e.dma_start(out=dd[:, 0:nh * 64], in_=f["yout"][:, 0:nh * 64]), reads=[f["yout"]], writes=[dd])
        S.dma("sp", lambda e: e.dma_start(out=dh[:, :], in_=Hst.t[:].rearrange("p h d -> p (h d)")), reads=[Hst], writes=[dh])
        C.barrier()
        print("ninst", S.ninst, "nwaits", S.nwaits)
        return nc
    dbg_rows = []
    for d in range(2):
        if d == 0:
            order = [NT, NT + 1] + (list(range(NT)) if not short else [0, 1])
        else:
            order = [NT + 1, NT] + (list(range(NT - 1, -1, -1)) if not short else [63, 62])
        S.op("pool", lambda e: e.memset(Hst[:], 0.0), writes=[Hst])
        for n in order:
            emit = n < NT
            features(n, d)
            chunk(n, d, emit)
            if not emit:
                continue
            rows = slice(n * 128, (n + 1) * 128)
            if d == 0:
                S.op("act", lambda e: e.copy(out=f["yout"][:], in_=pY[:]), reads=[pY], writes=[f["yout"]])
                S.op("pool", lambda e: e.tensor_copy(out=S0all[:, n, :], in_=sd[:]), reads=[sd], writes=[S0all])
                S.dma("sp", lambda e: e.dma_start(out=yfscr[rows, :], in_=f["yout"][:]), reads=[f["yout"]], writes=[yfscr])
                continue
            if short:
                S.op("act", lambda e: e.copy(out=f["yout"][:], in_=pY[:]), reads=[pY], writes=[f["yout"]])
                S.dma("sp", lambda e: e.dma_start(out=yrwscr[rows, :], in_=f["yout"][:]), reads=[f["yout"]], writes=[yrwscr])
                continue
            S.dma("sp", lambda e: e.dma_start(out=f["ysum"][:], in_=yfscr[rows, :]), reads=[yfscr], writes=[f["ysum"]])
            S.op("dve", lambda e: e.tensor_tensor(out=f["ysum"][:], in0=pY[:], in1=f["ysum"][:], op=ALU.add), reads=[pY, f["ysum"]], writes=[f["ysum"]])
            for h in range(8):
                S.op("dve", lambda e: e.bn_stats(out=gst[:, h, :], in_=f["ysum"][:, h * 64:(h + 1) * 64]), reads=[f["ysum"]], writes=[gst])
                S.op("dve", lambda e: e.bn_aggr(out=gmv[:, h, :], in_=gst[:, h, :]), reads=[gst], writes=[gmv])
            S.op("dve", lambda e: e.tensor_scalar_add(out=ss[:], in0=gmv[:, :, 1], scalar1=GN_EPS), reads=[gmv], writes=[ss])
            S.op("act", lambda e: e.activation(out=ss[:], in_=ss[:], func=AF.Ln), reads=[ss], writes=[ss])
            S.op("act", lambda e: e.activation(out=ss[:], in_=ss[:], func=AF.Exp, scale=-0.5), reads=[ss], writes=[ss])
            y3 = v3(f["ysum"])
            S.op("dve", lambda e: e.tensor_tensor(out=y3, in0=y3, in1=gmv.t[:, :, 0:1].to_broadcast([128, 8, 64]), op=ALU.subtract), reads=[f["ysum"], gmv], writes=[f["ysum"]])
            S.op("dve", lambda e: e.tensor_tensor(out=y3, in0=y3, in1=ss.t[:].unsqueeze(2).to_broadcast([128, 8, 64]), op=ALU.mult), reads=[f["ysum"], ss], writes=[f["ysum"]])
            S.op("pool", lambda e: e.tensor_tensor(out=f["ysum"][:], in0=f["ysum"][:], in1=gn_w[:], op=ALU.mult), reads=[f["ysum"], gn_w], writes=[f["ysum"]])
            S.op("pool", lambda e: e.tensor_tensor(out=f["ysum"][:], in0=f["ysum"][:], in1=gn_b[:], op=ALU.add), reads=[f["ysum"], gn_b], writes=[f["ysum"]])
            S.op("dve", lambda e: e.tensor_tensor(out=sd[:], in0=sd[:], in1=S0all[:, n, :], op=ALU.add), reads=[sd, S0all], writes=[sd])
            S.op("dve", lambda e: e.tensor_tensor(out=v3(f["tmp"]), in0=zs.t[:, 1024:1536].rearrange("p (h d) -> p h d", h=8),
                                                  in1=sd.t[:].unsqueeze(2).to_broadcast([128, 8, 64]), op=ALU.mult), reads=[zs, sd], writes=[f["tmp"]])
            S.op("dve", lambda e: e.tensor_tensor(out=f["ysum"][:], in0=f["ysum"][:], in1=f["tmp"][:], op=ALU.add), reads=[f["ysum"], f["tmp"]], writes=[f["ysum"]])
            S.op("pool", lambda e: e.tensor_tensor(out=f["yout"][:], in0=f["ysum"][:], in1=f["g"][:], op=ALU.mult), reads=[f["ysum"], f["g"]], writes=[f["yout"]])
            S.dma("sp", lambda e: e.dma_start(out=yrwscr[rows, :], in_=f["yout"][:]), reads=[f["yout"]], writes=[yrwscr])
    C.release(mC)
    if short:
        dd = Buf("dbg", nc.dram_tensor("dbg", [512, 512], F32, kind="ExternalOutput").ap())
        S.dma("sp", lambda e: e.dma_start(out=dd[0:256, :], in_=yfscr[0:256, :]), reads=[yfscr], writes=[dd])
        S.dma("sp", lambda e: e.dma_start(out=dd[256:512, :], in_=yrwscr[62 * 128:64 * 128, :]), reads=[yrwscr], writes=[dd])
        C.barrier()
        print("ninst", S.ninst, "nwaits", S.nwaits)
        return nc
    return yrwscr


def build_phase_def(nc, C, S, xin, zscr, modscr, ynascr, yrwscr, wD, identb, ident, out_d, stop_after):
    h1scr = C.dram("h1scr", [T, D]); u2Tscr = C.dram("u2Tscr", [8, 128, T], BF16)
    AFF = C.sb("AFF", [128, NT, NE]); GM = C.sb("GM", [128, NT, NE])

    def bcast(name, src_buf, src_ap, eng="sp"):
        b = C.sb(name, [128, D])
        S.dma(eng, lambda e: e.dma_start(out=b[:], in_=src_ap.partition_broadcast(128)), reads=[src_buf], writes=[b])
        return b

    def ln_rows(xt, stats, mv, rstd, eps):
        for hf in range(2):
            S.op("dve", lambda e: e.bn_stats(out=stats[:, hf, :], in_=xt[:, hf * 512:(hf + 1) * 512]), reads=[xt], writes=[stats])
        S.op("dve", lambda e: e.bn_aggr(out=mv[:], in_=stats[:]), reads=[stats], writes=[mv])
        S.op("dve", lambda e: e.tensor_scalar_add(out=rstd[:], in0=mv[:, 1:2], scalar1=eps), reads=[mv], writes=[rstd])
        S.op("act", lambda e: e.activation(out=rstd[:], in_=rstd[:], func=AF.Ln), reads=[rstd], writes=[rstd])
        S.op("act", lambda e: e.activation(out=rstd[:], in_=rstd[:], func=AF.Exp, scale=-0.5), reads=[rstd], writes=[rstd])
        S.op("dve", lambda e: e.tensor_scalar(out=xt[:], in0=xt[:], scalar1=mv[:, 0:1], scalar2=rstd[:, 0:1], op0=ALU.subtract, op1=ALU.mult),
             reads=[xt, mv, rstd], writes=[xt])

    mD = C.mark()
    wst = C.sb("wstD", [128, 8, D])
    wpa = C.sb("wpa", [128, 4, D], BF16); wpr = C.sb("wpr", [128, 4, D], BF16); wo = C.sb("wo", [128, 8, D], BF16)
    wr = C.sb("wr", [128, 8, NE], BF16); wrs = C.sb("wrs", [128, 8, NE])
    for dst, key, nk in [(wpa, "w_pa", 4), (wpr, "w_pr", 4), (wo, "w_o", 8)]:
        S.dma("sp", lambda e: e.dma_start(out=wst[:, 0:nk, :], in_=wD[key].t.rearrange("(kc p) n -> p kc n", p=128)), reads=[wD[key]], writes=[wst])
        S.op("dve", lambda e: e.tensor_copy(out=dst[:], in_=wst[:, 0:nk, :]), reads=[wst], writes=[dst])
    S.dma("sp", lambda e: e.dma_start(out=wrs[:], in_=wD["w_router"].t.rearrange("(kc p) n -> p kc n", p=128)), reads=[wD["w_router"]], writes=[wrs])
    S.op("dve", lambda e: e.tensor_copy(out=wr[:], in_=wrs[:]), reads=[wrs], writes=[wr])
    gt1 = bcast("gt1", modscr, modscr[0, 2 * D:3 * D]); sh2 = bcast("sh2", modscr, modscr[0, 3 * D:4 * D]); sc2 = bcast("sc2", modscr, modscr[0, 4 * D:5 * D])
    l1g = bcast("l1g", wD["ln1_g"], wD["ln1_g"].t); l1b = bcast("l1b", wD["ln1_b"], wD["ln1_b"].t)
    yna = C.sb("yna", [128, 1024]); ynb = C.sb("ynb", [128, 1024], BF16)
    zg = C.sb("zg", [128, 2048]); xt = C.sb("xtD", [128, D]); pre = C.sb("pre", [128, D]); t1 = C.sb("t1", [128, D])
    mb = C.sb("mb", [128, D], BF16); u2b = C.sb("u2b", [128, D], BF16)
    yT = C.sb("yT", [128, 8, 128], BF16); mT = C.sb("mT", [128, 8, 128], BF16); u2T = C.sb("u2T", [128, 8, 128], BF16)
    stats = C.sb("statsD", [128, 2, 6]); mv = C.sb("mvD", [128, 2]); rstd = C.sb("rstdD", [128, 1])
    ex = C.sb("ex", [128, NE]); sm = C.sb("sm", [128, 1])
    ptp = C.ps("ptpD", [128, 8, 128], BF16)
    pa = [C.ps("paD", [128, 512]) for _ in range(2)]; pr_ = [C.ps("prD", [128, 512]) for _ in range(2)]
    pl = C.ps("plD", [128, NE])
    tilesD = range(NT) if stop_after != "F_short" else range(0)
    for n in tilesD:
        rows = slice(n * 128, (n + 1) * 128)
        S.dma("sp", lambda e: e.dma_start(out=yna[:, 0:512], in_=ynascr[rows, :]), reads=[ynascr], writes=[yna])
        S.dma("sp", lambda e: e.dma_start(out=yna[:, 512:1024], in_=yrwscr[rows, :]), reads=[yrwscr], writes=[yna])
        S.dma("sp", lambda e: e.dma_start(out=zg[:], in_=zscr[rows, G0:G0 + 2048]), reads=[zscr], writes=[zg])
        S.dma("sp", lambda e: e.dma_start(out=xt[:], in_=xin[rows, :]), reads=[xin], writes=[xt])
        S.op("pool", lambda e: e.tensor_copy(out=ynb[:], in_=yna[:]), reads=[yna], writes=[ynb])
        for c in range(8):
            S.op("pe", lambda e: e.transpose(out=ptp[:, c, :], in_=ynb[:, c * 128:(c + 1) * 128], identity=identb[:]), reads=[ynb, identb], writes=[ptp], pe_acc=True)
        S.op("act", lambda e: e.copy(out=yT[:], in_=ptp[:]), reads=[ptp], writes=[yT])
        S.op("act", lambda e: e.activation(out=zg[:], in_=zg[:], func=AF.Sigmoid), reads=[zg], writes=[zg])
        for hf in range(2):
            cs = slice(hf * 512, (hf + 1) * 512)
            for c in range(4):
                S.op("pe", lambda e: e.matmul(pa[hf][:], lhsT=yT[:, c, :], rhs=wpa[:, c, cs], start=(c == 0), stop=(c == 3)), reads=[yT, wpa], writes=[pa[hf]], pe_acc=(c > 0))
            for c in range(4):
                S.op("pe", lambda e: e.matmul(pr_[hf][:], lhsT=yT[:, 4 + c, :], rhs=wpr[:, c, cs], start=(c == 0), stop=(c == 3)), reads=[yT, wpr], writes=[pr_[hf]], pe_acc=(c > 0))
            S.op("dve", lambda e: e.tensor_tensor(out=pre[:, cs], in0=pa[hf][:], in1=zg[:, hf * 512:(hf + 1) * 512], op=ALU.mult), reads=[pa[hf], zg], writes=[pre])
            S.op("dve", lambda e: e.tensor_tensor(out=t1[:, cs], in0=pr_[hf][:], in1=zg[:, 1024 + hf * 512:1024 + (hf + 1) * 512], op=ALU.mult), reads=[pr_[hf], zg], writes=[t1])
        S.op("pool", lambda e: e.tensor_tensor(out=mb[:], in0=pre[:], in1=t1[:], op=ALU.add), reads=[pre, t1], writes=[mb])
        for c in range(8):
            S.op("pe", lambda e: e.transpose(out=ptp[:, c, :], in_=mb[:, c * 128:(c + 1) * 128], identity=identb[:]), reads=[mb, identb], writes=[ptp], pe_acc=(c > 0))
        S.op("act", lambda e: e.copy(out=mT[:], in_=ptp[:]), reads=[ptp], writes=[mT])
        for hf in range(2):
            cs = slice(hf * 512, (hf + 1) * 512)
            for c in range(8):
                S.op("pe", lambda e: e.matmul(pa[hf][:], lhsT=mT[:, c, :], rhs=wo[:, c, cs], start=(c == 0), stop=(c == 7)), reads=[mT, wo], writes=[pa[hf]], pe_acc=(c > 0))
            S.op("dve", lambda e: e.tensor_tensor(out=pre[:, cs], in0=pa[hf][:], in1=gt1[:, cs], op=ALU.mult), reads=[pa[hf], gt1], writes=[pre])
        S.op("dve", lambda e: e.scalar_tensor_tensor(out=pre[:], in0=xt[:], scalar=float(ALPHA), in1=pre[:], op0=ALU.mult, op1=ALU.add), reads=[xt, pre], writes=[pre])
        ln_rows(pre, stats, mv, rstd, LN_EPS)
        S.op("pool", lambda e: e.tensor_tensor(out=pre[:], in0=pre[:], in1=l1g[:], op=ALU.mult), reads=[pre, l1g], writes=[pre])
        S.op("pool", lambda e: e.tensor_tensor(out=pre[:], in0=pre[:], in1=l1b[:], op=ALU.add), reads=[pre, l1b], writes=[pre])
        S.dma("sp", lambda e: e.dma_start(out=h1scr[rows, :], in_=pre[:]), reads=[pre], writes=[h1scr])
        S.op("dve", lambda e: e.tensor_copy(out=t1[:], in_=pre[:]), reads=[pre], writes=[t1])
        ln_rows(t1, stats, mv, rstd, LN_EPS)
        S.op("pool", lambda e: e.tensor_tensor(out=t1[:], in0=t1[:], in1=sc2[:], op=ALU.mult), reads=[t1, sc2], writes=[t1])
        S.op("pool", lambda e: e.tensor_tensor(out=u2b[:], in0=t1[:], in1=sh2[:], op=ALU.add), reads=[t1, sh2], writes=[u2b])
        for c in range(8):
            S.op("pe", lambda e: e.transpose(out=ptp[:, c, :], in_=u2b[:, c * 128:(c + 1) * 128], identity=identb[:]), reads=[u2b, identb], writes=[ptp], pe_acc=(c > 0))
        S.op("act", lambda e: e.copy(out=u2T[:], in_=ptp[:]), reads=[ptp], writes=[u2T])
        S.dma("sp", lambda e: e.dma_start(out=u2Tscr.t[:, :, rows].rearrange("k p t -> p k t"), in_=u2T[:]), reads=[u2T], writes=[u2Tscr])
        for c in range(8):
            S.op("pe", lambda e: e.matmul(pl[:], lhsT=u2T[:, c, :], rhs=wr[:, c, :], start=(c == 0), stop=(c == 7)), reads=[u2T, wr], writes=[pl], pe_acc=(c > 0))
        S.op("act", lambda e: e.activation(out=ex[:], in_=pl[:], func=AF.Exp), reads=[pl], writes=[ex])
        S.op("dve", lambda e: e.tensor_reduce(out=sm[:], in_=ex[:], axis=AX.X, op=ALU.add), reads=[ex], writes=[sm])
        S.op("dve", lambda e: e.reciprocal(out=sm[:], in_=sm[:]), reads=[sm], writes=[sm])
        S.op("dve", lambda e: e.tensor_scalar(out=AFF[:, n, :], in0=ex[:], scalar1=sm[:, 0:1], scalar2=None, op0=ALU.mult), reads=[ex, sm], writes=[AFF])
    C.release(mD)

    mE = C.mark()
    ones = C.sb("ones", [128, 128]); lo = C.sb("lo", [128, NE]); hi = C.sb("hi", [128, NE]); mid = C.sb("mid", [128, NE])
    cmpb = C.sb("cmpb", [128, NT, NE]); cp = C.sb("cp", [128, NE]); ge = C.sb("ge", [128, NE]); dl = C.sb("dl", [128, NE])
    pc = C.ps("pcE", [128, NE])
    S.op("pool", lambda e: e.memset(ones[:], 1.0), writes=[ones])
    S.op("pool", lambda e: e.memset(lo[:], 0.0), writes=[lo])
    S.op("pool", lambda e: e.memset(hi[:], 1.0), writes=[hi])
    for it in range(34 if stop_after != "F_short" else 1):
        S.op("dve", lambda e: e.tensor_tensor(out=mid[:], in0=lo[:], in1=hi[:], op=ALU.add), reads=[lo, hi], writes=[mid])
        S.op("dve", lambda e: e.tensor_scalar_mul(out=mid[:], in0=mid[:], scalar1=0.5), reads=[mid], writes=[mid])
        S.op("dve", lambda e: e.tensor_tensor(out=cmpb[:], in0=AFF[:], in1=mid.t[:].unsqueeze(1).to_broadcast([128, NT, NE]), op=ALU.is_ge), reads=[AFF, mid], writes=[cmpb])
        S.op("dve", lambda e: e.tensor_reduce(out=cp[:], in_=cmpb.t[:].rearrange("p n e -> p e n"), axis=AX.X, op=ALU.add), reads=[cmpb], writes=[cp])
        S.op("pe", lambda e: e.matmul(pc[:], lhsT=ones[:], rhs=cp[:], start=True, stop=True), reads=[ones, cp], writes=[pc])
        S.op("dve", lambda e: e.tensor_single_scalar(out=ge[:], in_=pc[:], scalar=float(CAP) - 0.5, op=ALU.is_ge), reads=[pc], writes=[ge])
        S.op("dve", lambda e: e.tensor_tensor(out=dl[:], in0=mid[:], in1=lo[:], op=ALU.subtract), reads=[mid, lo], writes=[dl])
        S.op("dve", lambda e: e.tensor_tensor(out=dl[:], in0=dl[:], in1=ge[:], op=ALU.mult), reads=[dl, ge], writes=[dl])
        S.op("dve", lambda e: e.tensor_tensor(out=lo[:], in0=lo[:], in1=dl[:], op=ALU.add), reads=[lo, dl], writes=[lo])
        S.op("dve", lambda e: e.tensor_tensor(out=dl[:], in0=hi[:], in1=mid[:], op=ALU.subtract), reads=[hi, mid], writes=[dl])
        S.op("dve", lambda e: e.tensor_tensor(out=dl[:], in0=dl[:], in1=ge[:], op=ALU.mult), reads=[dl, ge], writes=[dl])
        S.op("dve", lambda e: e.tensor_tensor(out=hi[:], in0=mid[:], in1=dl[:], op=ALU.add), reads=[mid, dl], writes=[hi])
    S.op("dve", lambda e: e.tensor_tensor(out=cmpb[:], in0=AFF[:], in1=lo.t[:].unsqueeze(1).to_broadcast([128, NT, NE]), op=ALU.is_ge), reads=[AFF, lo], writes=[cmpb])
    S.op("dve", lambda e: e.tensor_tensor(out=GM[:], in0=AFF[:], in1=cmpb[:], op=ALU.mult), reads=[AFF, cmpb], writes=[GM])
    C.release(mE)

    mF = C.mark()
    gt2 = bcast("gt2", modscr, modscr[0, 5 * D:6 * D]); l2g = bcast("l2g", wD["ln2_g"], wD["ln2_g"].t); l2b = bcast("l2b", wD["ln2_b"], wD["ln2_b"].t)
    TB = 512
    u2blk = C.sb("u2blk", [128, 8, TB], BF16)
    hT = C.sb("hT", [128, 22, TB], BF16); W2b = C.sb("W2b", [128, 22, D], BF16)
    acc = C.sb("acc", [128, 4, D])
    w1s = [C.sb("w1s", [128, 8, 128]) for _ in range(2)]; w3s = [C.sb("w3s", [128, 8, 128]) for _ in range(2)]; w2s = [C.sb("w2s", [128, D]) for _ in range(2)]
    w1b = [C.sb("w1b", [128, 8, 128], BF16) for _ in range(2)]; w3b = [C.sb("w3b", [128, 8, 128], BF16) for _ in range(2)]
    sg = [C.sb("sg", [128, TB]) for _ in range(2)]
    h1t = C.sb("h1t", [128, D]); stats = C.sb("statsF", [128, 2, 6]); mv = C.sb("mvF", [128, 2]); rstd = C.sb("rstdF", [128, 1])
    ph1 = [C.ps("ph1", [128, TB]) for _ in range(2)]; ph3 = [C.ps("ph3", [128, TB]) for _ in range(2)]
    py = [C.ps("py", [128, 512]) for _ in range(2)]
    w1v = wD["w_e1"].t.rearrange("e (kc p) f -> e p kc f", p=128); w3v = wD["w_e3"].t.rearrange("e (kc p) f -> e p kc f", p=128)
    cnt = 0
    for bk in range(T // TB if stop_after != "F_short" else 1):
        t0 = bk * TB
        S.dma("sp", lambda e: e.dma_start(out=u2blk[:], in_=u2Tscr.t[:, :, t0:t0 + TB].rearrange("k p t -> p k t")), reads=[u2Tscr], writes=[u2blk])
        S.op("pool", lambda e: e.memset(acc[:], 0.0), writes=[acc])
        for ex_ in range(NE if stop_after != "F_short" else 2):
            for j in range(22):
                a1, a3, a2, b1, b3 = rr(w1s, cnt), rr(w3s, cnt), rr(w2s, cnt), rr(w1b, cnt), rr(w3b, cnt)
                p1, p3, sg_ = rr(ph1, cnt), rr(ph3, cnt), rr(sg, cnt); cnt += 1
                fs = slice(j * 128, (j + 1) * 128)
                S.dma("sp", lambda e: e.dma_start(out=a1[:], in_=w1v[ex_, :, :, fs]), reads=[wD["w_e1"]], writes=[a1])
                S.dma("sp", lambda e: e.dma_start(out=a3[:], in_=w3v[ex_, :, :, fs]), reads=[wD["w_e3"]], writes=[a3])
                S.dma("sp", lambda e: e.dma_start(out=a2[:], in_=wD["w_e2"][ex_, fs, :]), reads=[wD["w_e2"]], writes=[a2])
                S.op("pool", lambda e: e.tensor_copy(out=b1[:], in_=a1[:]), reads=[a1], writes=[b1])
                S.op("dve", lambda e: e.tensor_copy(out=b3[:], in_=a3[:]), reads=[a3], writes=[b3])
                S.op("act", lambda e: e.copy(out=W2b[:, j, :], in_=a2[:]), reads=[a2], writes=[W2b])
                for kc in range(8):
                    S.op("pe", lambda e: e.matmul(p1[:], lhsT=b1[:, kc, :], rhs=u2blk[:, kc, :], start=(kc == 0), stop=(kc == 7)), reads=[b1, u2blk], writes=[p1], pe_acc=(kc > 0))
                for kc in range(8):
                    S.op("pe", lambda e: e.matmul(p3[:], lhsT=b3[:, kc, :], rhs=u2blk[:, kc, :], start=(kc == 0), stop=(kc == 7)), reads=[b3, u2blk], writes=[p3], pe_acc=(kc > 0))
                S.op("act", lambda e: e.activation(out=sg_[:], in_=p1[:], func=AF.Silu), reads=[p1], writes=[sg_])
                S.op("dve", lambda e: e.tensor_tensor(out=hT[:, j, :], in0=p3[:], in1=sg_[:], op=ALU.mult), reads=[p3, sg_], writes=[hT])
            for tt in range(4):
                n = bk * 4 + tt
                for hf in range(2):
                    p = rr(py, tt * 2 + hf)
                    for j in range(22):
                        S.op("pe", lambda e: e.matmul(p[:], lhsT=hT[:, j, tt * 128:(tt + 1) * 128], rhs=W2b[:, j, hf * 512:(hf + 1) * 512], start=(j == 0), stop=(j == 21)),
                             reads=[hT, W2b], writes=[p], pe_acc=(j > 0))
                    S.op("dve", lambda e: e.scalar_tensor_tensor(out=acc[:, tt, hf * 512:(hf + 1) * 512], in0=p[:], scalar=GM[:, n, ex_:ex_ + 1],
                                                                 in1=acc[:, tt, hf * 512:(hf + 1) * 512], op0=ALU.mult, op1=ALU.add), reads=[p, GM, acc], writes=[acc])
        for tt in range(4):
            rows = slice(t0 + tt * 128, t0 + (tt + 1) * 128)
            S.dma("sp", lambda e: e.dma_start(out=h1t[:], in_=h1scr[rows, :]), reads=[h1scr], writes=[h1t])
            S.op("pool", lambda e: e.tensor_tensor(out=acc[:, tt, :], in0=acc[:, tt, :], in1=gt2[:], op=ALU.mult), reads=[acc, gt2], writes=[acc])
            S.op("dve", lambda e: e.scalar_tensor_tensor(out=h1t[:], in0=h1t[:], scalar=float(ALPHA), in1=acc[:, tt, :], op0=ALU.mult, op1=ALU.add), reads=[h1t, acc], writes=[h1t])
            ln_rows(h1t, stats, mv, rstd, LN_EPS)
            S.op("pool", lambda e: e.tensor_tensor(out=h1t[:], in0=h1t[:], in1=l2g[:], op=ALU.mult), reads=[h1t, l2g], writes=[h1t])
            S.op("pool", lambda e: e.tensor_tensor(out=h1t[:], in0=h1t[:], in1=l2b[:], op=ALU.add), reads=[h1t, l2b], writes=[h1t])
            S.dma("sp", lambda e: e.dma_start(out=out_d[rows, :], in_=h1t[:]), reads=[h1t], writes=[out_d])
    C.release(mF)


def host_consts():
    ident = np.eye(128, dtype=np.float32)
    t = np.arange(T)
    pos = np.stack([t // 64, t % 64], -1).astype(np.float32)
    nf = 16
    inv = np.power(np.float32(10000.0), -np.arange(nf, dtype=np.float32) / nf).astype(np.float32)
    ang = pos[:, :, None] * inv
    rope = np.stack([np.cos(ang), np.sin(ang)], 1).astype(np.float32)
    i = np.arange(128)
    US = (i[:, None] < i[None, :]); LS = (i[:, None] > i[None, :]); UI = (i[:, None] <= i[None, :]); LI = (i[:, None] >= i[None, :])
    SEL127 = np.broadcast_to((i == 127)[:, None], (128, 128)); SEL0 = np.broadcast_to((i == 0)[:, None], (128, 128))
    tri = np.stack([US, LS, UI, LI, SEL127, SEL0]).astype(np.float32)
    return {"ident": ident, "rope": rope, "tri": tri}


def na_bias_table(rpb):
    qi = np.arange(8)[:, None, None, None]
    p = np.arange(128)[None, :, None, None]
    jt = np.arange(4)[None, None, :, None]
    j = np.arange(64)[None, None, None, :]
    kr = 2 * jt + p // 64
    c = p % 64
    cs = np.clip(j - 8, 0, 48)
    valid = (c >= cs) & (c < cs + 16)
    ro = np.broadcast_to(kr - qi + 7, (8, 128, 4, 64))
    co = np.clip(np.broadcast_to(c - j + 15, (8, 128, 4, 64)), 0, 30)
    g = rpb[:, ro, co]
    g = np.where(np.broadcast_to(valid, g.shape), g, np.float32(-30000.0))
    return np.ascontiguousarray(g.transpose(1, 2, 0, 3, 4).reshape(8, 128, 8, 256)).astype(np.float32)


def make_in_maps(inputs, consts):
    maps = []
    nab = na_bias_table(inputs["rpb"][0])
    for b in range(4):
        m = dict(consts)
        m["x"] = np.ascontiguousarray(np.concatenate([inputs["x"][b], inputs["ctx"][b]], 0))
        m["cc"] = np.ascontiguousarray(np.stack([inputs["c"][b], inputs["c_ctx"]], 0))
        m["w_mod"] = inputs["w_mod"][0]; m["b_mod"] = inputs["b_mod"][0]
        m["w_in"] = inputs["w_in"][0]
        m["nab"] = nab
        for k in ("mu_prev", "mu_next", "k_k", "k_a", "gn_w", "gn_b", "w0", "a0", "g_up"):
            m[k] = np.ascontiguousarray(inputs[k][0])
        m["r_k"] = np.ascontiguousarray(inputs["r_k"][0].reshape(512))
        for k in ("w_pa", "w_pr", "w_o", "w_router", "ln1_g", "ln1_b", "ln2_g", "ln2_b", "w_e1", "w_e3", "w_e2"):
            m[k] = inputs[k][0]
        m["w_up"] = np.ascontiguousarray(inputs["w_up"][0].reshape(128, 512))
        m["a_up"] = np.ascontiguousarray(inputs["a_up"][0].reshape(128, 512))
        maps.append(m)
    return maps


def kernel(**inputs):
    inputs = {k: np.asarray(v) for k, v in inputs.items()}
    nc = build_program()
    maps = make_in_maps(inputs, host_consts())
    res = run_bass_kernel_spmd(nc, maps, core_ids=list(range(4)))
    return np.stack([r["out"] for r in res.results], 0).astype(np.float32)
```

```python
import numpy as np
import ml_dtypes
import concourse.bass as bass
import concourse.mybir as mybir
from concourse.bass_utils import run_bass_kernel_spmd

F32 = mybir.dt.float32
BF16 = mybir.dt.bfloat16
AF = mybir.ActivationFunctionType
ALU = mybir.AluOpType
AX = mybir.AxisListType

D = 1024
T = 8192
L = 256
TT = T + L
NT = T // 128
NTT = TT // 128
P_IN = 5504
RW0 = 1536
G0 = 3456
NE = 16
DE = 2816
CAP = 1024
ALPHA = 2.0 ** 0.25
LN_EPS = 1e-6
GN_EPS = 64e-5


class Buf:
    __slots__ = ("name", "t", "lw", "rs")

    def __init__(self, name, t):
        self.name, self.t, self.lw, self.rs = name, t, None, {}

    def __getitem__(self, idx):
        return self.t[idx]


class Sched:
    NDMA = 24

    def __init__(self, nc):
        self.nc = nc
        self.eng = {"pe": nc.tensor, "dve": nc.vector, "act": nc.scalar, "pool": nc.gpsimd, "sp": nc.sync}
        self.sem = {k: nc.alloc_semaphore(name=f"s_{k}") for k in self.eng}
        self.cnt = {k: 0 for k in self.eng}
        self.dsem = [nc.alloc_semaphore(name=f"d_{i}") for i in range(self.NDMA)]
        self.dcnt = [0] * self.NDMA
        self.dnext = 0
        self.semobj = dict(self.sem)
        for i, s in enumerate(self.dsem):
            self.semobj[("d", i)] = s
        self.seen = {k: {} for k in self.eng}
        self.ninst = 0
        self.nwaits = 0

    def _wait(self, e, key, val, same_ok=False):
        if key == e and same_ok:
            return
        if self.seen[e].get(key, 0) >= val:
            return
        self.eng[e].wait_ge(self.semobj[key], val)
        self.seen[e][key] = val
        self.nwaits += 1

    def _deps(self, e, reads, writes, pe_acc=False):
        for r in reads:
            if r.lw is not None:
                self._wait(e, r.lw[0], r.lw[1])
        for w in writes:
            if w.lw is not None:
                self._wait(e, w.lw[0], w.lw[1], same_ok=(pe_acc and e == "pe"))
            for k, v in w.rs.items():
                self._wait(e, k, v, same_ok=(k == e and e == "pe"))

    def _mark(self, key, val, reads, writes):
        for r in reads:
            r.rs[key] = val
        for w in writes:
            w.lw = (key, val)
            w.rs = {}

    def op(self, e, fn, reads=(), writes=(), pe_acc=False):
        self._deps(e, reads, writes, pe_acc)
        ins = fn(self.eng[e])
        self.cnt[e] += 1
        ins.then_inc(self.sem[e], 1)
        self._mark(e, self.cnt[e], reads, writes)
        self.ninst += 1
        return ins

    def dma(self, e, fn, reads=(), writes=()):
        i = self.dnext
        self.dnext = (self.dnext + 1) % self.NDMA
        key = ("d", i)
        if self.dcnt[i] > 0:
            self._wait(e, key, self.dcnt[i])
        self._deps(e, reads, writes)
        ins = fn(self.eng[e])
        self.dcnt[i] += 16
        ins.then_inc(self.dsem[i], 16)
        self._mark(key, self.dcnt[i], reads, writes)
        self.ninst += 1
        return ins

    def finish(self, e="sp"):
        for i in range(self.NDMA):
            if self.dcnt[i] > 0:
                self._wait(e, ("d", i), self.dcnt[i])
        for k in self.eng:
            if k != e and self.cnt[k] > 0:
                self._wait(e, k, self.cnt[k])


class Ctx:
    def __init__(self, nc):
        self.nc = nc
        self.S = Sched(nc)
        self.stack = []
        self.uid = 0

    def sb(self, name, shape, dt=F32):
        self.uid += 1
        cm = self.nc.sbuf_tensor(f"{name}_{self.uid}", list(shape), dt)
        t = cm.__enter__()
        self.stack.append(cm)
        return Buf(name, t)

    def ps(self, name, shape, dt=F32):
        self.uid += 1
        cm = self.nc.psum_tensor(f"{name}_{self.uid}", list(shape), dt)
        t = cm.__enter__()
        self.stack.append(cm)
        return Buf(name, t)

    def dram(self, name, shape, dt=F32):
        return Buf(name, self.nc.dram_tensor(name, list(shape), dt).ap())

    def mark(self):
        return len(self.stack)

    def release(self, m):
        self.barrier()
        while len(self.stack) > m:
            self.stack.pop().__exit__(None, None, None)

    def barrier(self):
        S = self.S
        for e in ("sp", "pe", "dve", "act", "pool"):
            S.finish(e)


def rr(lst, i):
    return lst[i % len(lst)]


def build_program(stop_after=None, dbg=None):
    nc = bass.Bass("TRN2", target_bir_lowering=False)
    C = Ctx(nc)
    S = C.S
    inp = lambda n, s, dt=F32: Buf(n, nc.dram_tensor(n, list(s), dt, kind="ExternalInput").ap())
    xin = inp("x", [TT, D])
    ccin = inp("cc", [2, D])
    w_mod = inp("w_mod", [D, 6 * D]); b_mod = inp("b_mod", [6 * D])
    w_in = inp("w_in", [D, P_IN])
    ident_d = inp("ident", [128, 128])
    rope_d = inp("rope", [T, 2, 2, 16])
    tri_d = inp("tri", [6, 128, 128])
    rwp = {k: inp(k, shp) for k, shp in [("mu_prev", [1920]), ("mu_next", [1920]), ("k_k", [512]), ("k_a", [512]), ("r_k", [512]),
                                          ("gn_w", [512]), ("gn_b", [512]), ("w0", [2, 512]), ("a0", [2, 512]),
                                          ("w_up", [128, 512]), ("a_up", [128, 512]), ("g_up", [128, 512])]}
    wD = None if stop_after not in (None, "F_short") else {k: inp(k, shp) for k, shp in [("w_pa", [512, D]), ("w_pr", [512, D]), ("w_o", [D, D]), ("w_router", [D, NE]),
                                        ("ln1_g", [D]), ("ln1_b", [D]), ("ln2_g", [D]), ("ln2_b", [D]),
                                        ("w_e1", [NE, D, DE]), ("w_e3", [NE, D, DE]), ("w_e2", [NE, DE, D])]}
    nab_d = inp("nab", [8, 128, 8, 256])
    out_d = Buf("out", nc.dram_tensor("out", [T, D], F32, kind="ExternalOutput").ap())
    zscr = C.dram("zscr", [TT, P_IN])
    modscr = C.dram("modscr", [2, 6 * D])

    ident = C.sb("ident", [128, 128]); identb = C.sb("identb", [128, 128], BF16)
    S.dma("sp", lambda e: e.dma_start(out=ident[:], in_=ident_d[:, :]), writes=[ident])
    S.op("dve", lambda e: e.tensor_copy(out=identb[:], in_=ident[:]), reads=[ident], writes=[identb])

    if stop_after == "F_short":
        ynascr = C.dram("ynascr", [T, 512]); yrwscr = C.dram("yrwscr", [T, 512])
        build_phase_def(nc, C, S, xin, zscr, modscr, ynascr, yrwscr, wD, identb, ident, out_d, stop_after)
        C.barrier()
        print("ninst", S.ninst, "nwaits", S.nwaits)
        return nc
    m0 = C.mark()
    scol = C.sb("scol", [128, 2, 8]); modrow = C.sb("modrow", [2, 6 * D]); brow = C.sb("brow", [2, 6 * D])
    S.dma("sp", lambda e: e.dma_start(out=scol[:], in_=ccin.t.rearrange("r (kc p) -> p r kc", p=128),
                                      allow_slow_non_contiguous=True), writes=[scol])
    S.dma("sp", lambda e: e.dma_start(out=brow[:], in_=b_mod.t.partition_broadcast(2)), writes=[brow])
    S.op("act", lambda e: e.activation(out=scol[:], in_=scol[:], func=AF.Silu), reads=[scol], writes=[scol])
    wm = [C.sb("wm", [128, 8, 512]) for _ in range(2)]
    pm = [C.ps("pm", [2, 512]) for _ in range(2)]
    wmv = w_mod.t.rearrange("(kc p) n -> p kc n", p=128)
    for cb in range(12):
        w, p = rr(wm, cb), rr(pm, cb)
        S.dma("sp", lambda e: e.dma_start(out=w[:], in_=wmv[:, :, cb * 512:(cb + 1) * 512]), writes=[w])
        for kc in range(8):
            S.op("pe", lambda e: e.matmul(p[:], lhsT=scol[:, :, kc], rhs=w[:, kc, :], start=(kc == 0), stop=(kc == 7)),
                 reads=[scol, w], writes=[p], pe_acc=(kc > 0))
        S.op("dve", lambda e: e.tensor_tensor(out=modrow[:, cb * 512:(cb + 1) * 512], in0=p[:], in1=brow[:, cb * 512:(cb + 1) * 512], op=ALU.add),
             reads=[p, brow], writes=[modrow])
    for sec in (1, 4):
        S.op("dve", lambda e: e.tensor_scalar_add(out=modrow[:, sec * D:(sec + 1) * D], in0=modrow[:, sec * D:(sec + 1) * D], scalar1=1.0),
             reads=[modrow], writes=[modrow])
    S.dma("sp", lambda e: e.dma_start(out=modscr[:, :], in_=modrow[:]), reads=[modrow], writes=[modscr])
    C.release(m0)

    def bcast(name, src_buf, src_ap, n=D, eng="sp"):
        b = C.sb(name, [128, n])
        S.dma(eng, lambda e: e.dma_start(out=b[:], in_=src_ap.partition_broadcast(128)), reads=[src_buf], writes=[b])
        return b

    def layer_norm_stats(xt, stats, mv, rstd, eps):
        for hf in range(2):
            S.op("dve", lambda e: e.bn_stats(out=stats[:, hf, :], in_=xt[:, hf * 512:(hf + 1) * 512]), reads=[xt], writes=[stats])
        S.op("dve", lambda e: e.bn_aggr(out=mv[:], in_=stats[:]), reads=[stats], writes=[mv])
        S.op("dve", lambda e: e.tensor_scalar_add(out=rstd[:], in0=mv[:, 1:2], scalar1=eps), reads=[mv], writes=[rstd])
        S.op("act", lambda e: e.activation(out=rstd[:], in_=rstd[:], func=AF.Ln), reads=[rstd], writes=[rstd])
        S.op("act", lambda e: e.activation(out=rstd[:], in_=rstd[:], func=AF.Exp, scale=-0.5), reads=[rstd], writes=[rstd])

    mA = C.mark()
    win_b = C.sb("win_b", [128, 8, P_IN], BF16)
    wst = [C.sb("wst", [128, 8, 512]) for _ in range(2)]
    winv = w_in.t.rearrange("(kc p) n -> p kc n", p=128)
    for cb in range(11):
        c0, c1 = cb * 512, min(P_IN, (cb + 1) * 512)
        w = rr(wst, cb)
        S.dma("sp", lambda e: e.dma_start(out=w[:, :, 0:c1 - c0], in_=winv[:, :, c0:c1]), writes=[w])
        S.op(rr(["dve", "pool"], cb), lambda e: e.tensor_copy(out=win_b[:, :, c0:c1], in_=w[:, :, 0:c1 - c0]), reads=[w], writes=[win_b])
    scA = [bcast("sc1", modscr, modscr[0, 1 * D:2 * D]), bcast("csc1", modscr, modscr[1, 1 * D:2 * D])]
    shA = [bcast("sh1", modscr, modscr[0, 0:D]), bcast("csh1", modscr, modscr[1, 0:D])]
    xt = [C.sb("xt", [128, D]) for _ in range(2)]
    ub = [C.sb("ub", [128, D], BF16) for _ in range(2)]
    uT = [C.sb("uT", [128, 8, 128], BF16) for _ in range(2)]
    zt = [C.sb("zt", [128, P_IN]) for _ in range(2)]
    stats = C.sb("stats", [128, 2, 6]); mv = C.sb("mv", [128, 2]); rstd = C.sb("rstd", [128, 1])
    rope = [C.sb("rope", [128, 2, 2, 16]) for _ in range(2)]
    rt = [C.sb("rt", [128, 16, 2, 16]) for _ in range(4)]
    ptp = [C.ps("ptp", [128, 8, 128], BF16) for _ in range(2)]
    pz = [C.ps("pz", [128, 512]) for _ in range(4)]
    tilesA = list(range(NTT))
    if stop_after == "A_short":
        tilesA = [0, 64]
    if stop_after == "B_short":
        tilesA = [0, 1, 2, 3, 4, 60, 61, 62, 63, 64, 65]
    if stop_after == "C_short":
        tilesA = [0, 1, 2, 61, 62, 63, 64, 65]
    if stop_after in ("C_feat", "C_one"):
        tilesA = [0, 1, 62, 63]
    for it, n in enumerate(tilesA):
        isctx = n >= NT
        x_, u_, uT_, z_, pt_ = rr(xt, it), rr(ub, it), rr(uT, it), rr(zt, it), rr(ptp, it)
        sc, sh = scA[isctx], shA[isctx]
        S.dma("sp", lambda e: e.dma_start(out=x_[:], in_=xin[n * 128:(n + 1) * 128, :]), reads=[xin], writes=[x_])
        layer_norm_stats(x_, stats, mv, rstd, LN_EPS)
        S.op("dve", lambda e: e.tensor_scalar(out=x_[:], in0=x_[:], scalar1=mv[:, 0:1], scalar2=rstd[:, 0:1], op0=ALU.subtract, op1=ALU.mult),
             reads=[x_, mv, rstd], writes=[x_])
        S.op("pool", lambda e: e.tensor_tensor(out=x_[:], in0=x_[:], in1=sc[:], op=ALU.mult), reads=[x_, sc], writes=[x_])
        S.op("pool", lambda e: e.tensor_tensor(out=u_[:], in0=x_[:], in1=sh[:], op=ALU.add), reads=[x_, sh], writes=[u_])
        for kc in range(8):
            S.op("pe", lambda e: e.transpose(out=pt_[:, kc, :], in_=u_[:, kc * 128:(kc + 1) * 128], identity=identb[:]),
                 reads=[u_, identb], writes=[pt_], pe_acc=True)
        S.op("act", lambda e: e.copy(out=uT_[:], in_=pt_[:]), reads=[pt_], writes=[uT_])
        for cb in range(11):
            c0, c1 = cb * 512, min(P_IN, (cb + 1) * 512)
            p = rr(pz, cb)
            for kc in range(8):
                S.op("pe", lambda e: e.matmul(p[:, 0:c1 - c0], lhsT=uT_[:, kc, :], rhs=win_b[:, kc, c0:c1], start=(kc == 0), stop=(kc == 7)),
                     reads=[uT_, win_b], writes=[p], pe_acc=(kc > 0))
            if cb % 2 == 0:
                S.op("act", lambda e: e.copy(out=z_[:, c0:c1], in_=p[:, 0:c1 - c0]), reads=[p], writes=[z_])
            else:
                S.op("dve", lambda e: e.tensor_copy(out=z_[:, c0:c1], in_=p[:, 0:c1 - c0]), reads=[p], writes=[z_])
        if not isctx:
            rp = rr(rope, it)
            S.dma("sp", lambda e: e.dma_start(out=rp[:], in_=rope_d[n * 128:(n + 1) * 128]), reads=[rope_d], writes=[rp])
            qk = z_.t[:, 0:1024].rearrange("p (h a b f) -> p h a b f", h=16, a=2, b=2)
            x1, x2 = qk[:, :, :, 0, :], qk[:, :, :, 1, :]
            cosb = rp.t[:, 0, :, :].unsqueeze(1).to_broadcast([128, 16, 2, 16])
            sinb = rp.t[:, 1, :, :].unsqueeze(1).to_broadcast([128, 16, 2, 16])
            t1, t2, t3, t4 = rt
            S.op("dve", lambda e: e.tensor_tensor(out=t1[:], in0=x1, in1=cosb, op=ALU.mult), reads=[z_, rp], writes=[t1])
            S.op("pool", lambda e: e.tensor_tensor(out=t2[:], in0=x2, in1=sinb, op=ALU.mult), reads=[z_, rp], writes=[t2])
            S.op("dve", lambda e: e.tensor_tensor(out=t3[:], in0=x2, in1=cosb, op=ALU.mult), reads=[z_, rp], writes=[t3])
            S.op("pool", lambda e: e.tensor_tensor(out=t4[:], in0=x1, in1=sinb, op=ALU.mult), reads=[z_, rp], writes=[t4])
            S.op("dve", lambda e: e.tensor_tensor(out=x1, in0=t1[:], in1=t2[:], op=ALU.subtract), reads=[t1, t2], writes=[z_])
            S.op("dve", lambda e: e.tensor_tensor(out=x2, in0=t3[:], in1=t4[:], op=ALU.add), reads=[t3, t4], writes=[z_])
        S.dma("sp", lambda e: e.dma_start(out=zscr[n * 128:(n + 1) * 128, :], in_=z_[:]), reads=[z_], writes=[zscr])
    C.release(mA)
    if stop_after in ("A", "A_short"):
        if dbg is not None:
            d = Buf("dbg", nc.dram_tensor("dbg", [256, P_IN], F32, kind="ExternalOutput").ap())
            S.dma("sp", lambda e: e.dma_start(out=d[0:128, :], in_=zscr[0:128, :]), reads=[zscr], writes=[d])
            S.dma("sp", lambda e: e.dma_start(out=d[128:256, :], in_=zscr[T:T + 128, :]), reads=[zscr], writes=[d])
        C.barrier()
        print("ninst", S.ninst, "nwaits", S.nwaits)
        return nc

    ynascr = C.dram("ynascr", [T, 512])
    if stop_after in ("C_short", "C_feat", "C_one"):
        return build_phase_c(nc, C, S, zscr, ident, tri_d, rwp, stop_after)
    mB = C.mark()
    KT = C.sb("KT", [128, 4, TT], BF16)
    mB0 = C.mark()
    kst = [C.sb("kst", [128, 512]) for _ in range(2)]
    kb = [C.sb("kb", [128, 512], BF16) for _ in range(2)]
    pkt = [C.ps("pkt", [128, 4, 128], BF16) for _ in range(2)]
    for it, n in enumerate(tilesA if stop_after == "B_short" else range(NTT)):
        ks_, kb_, pk_ = rr(kst, it), rr(kb, it), rr(pkt, it)
        S.dma("sp", lambda e: e.dma_start(out=ks_[:], in_=zscr[n * 128:(n + 1) * 128, 512:1024]), reads=[zscr], writes=[ks_])
        S.op("dve", lambda e: e.tensor_copy(out=kb_[:], in_=ks_[:]), reads=[ks_], writes=[kb_])
        for hp in range(4):
            S.op("pe", lambda e: e.transpose(out=pk_[:, hp, :], in_=kb_[:, hp * 128:(hp + 1) * 128], identity=identb[:]),
                 reads=[kb_, identb], writes=[pk_], pe_acc=True)
        S.op("act", lambda e: e.copy(out=KT[:, :, n * 128:(n + 1) * 128], in_=pk_[:]), reads=[pk_], writes=[KT])
    C.release(mB0)
    ebst = C.sb("ebst", [128, 8, 256])
    EBi = C.sb("EBi", [128, 8, 256], BF16); EBe = C.sb("EBe", [128, 8, 256], BF16)

    def load_eb(qi, dst):
        S.dma("sp", lambda e: e.dma_start(out=ebst[:], in_=nab_d[qi]), reads=[nab_d], writes=[ebst])
        S.op("act", lambda e: e.activation(out=dst[:], in_=ebst[:], func=AF.Exp), reads=[ebst], writes=[dst])
    load_eb(4, EBi)
    vcst = C.sb("vcst", [128, 2, 512]); VCa = C.sb("VCa", [128, 2, 8, 65], BF16)
    S.dma("sp", lambda e: e.dma_start(out=vcst[:], in_=zscr.t[T:TT, 1024:1536].rearrange("(j p) c -> p j c", p=128)), reads=[zscr], writes=[vcst])
    S.op("pool", lambda e: e.memset(VCa[:], 1.0), writes=[VCa])
    S.op("dve", lambda e: e.tensor_copy(out=VCa[:, :, :, 0:64], in_=vcst.t[:].rearrange("p j (h d) -> p j h d", h=8)), reads=[vcst], writes=[VCa])
    qst = [C.sb("qst", [64, 512]) for _ in range(2)]; qb = [C.sb("qb", [64, 512], BF16) for _ in range(2)]
    QTr = [C.sb("QTr", [128, 4, 64], BF16) for _ in range(2)]
    vst = [C.sb("vst", [128, 4, 512]) for _ in range(2)]
    Va = [C.sb("Va", [128, 4, 8, 65], BF16) for _ in range(2)]
    for v_ in Va:
        S.op("pool", lambda e: e.memset(v_[:], 1.0), writes=[v_])
    PT = [C.sb("PT", [128, 384], BF16) for _ in range(3)]
    yrow = [C.sb("yrow", [64, 8, 64]) for _ in range(2)]
    rec = [C.sb("rec", [64, 8, 1]) for _ in range(2)]
    pq = C.ps("pq", [128, 4, 64], BF16)
    pss = [C.ps("pss", [128, 384]) for _ in range(3)]
    po = [[C.ps("po", [64, 4, 65]) for _ in range(2)] for _ in range(2)]
    rowsB = list(range(128))
    if stop_after == "B_short":
        rowsB = [0, 1, 2, 3, 4, 5, 125, 127]
    cur_edge = None
    cnt = 0
    for ir, i in enumerate(rowsB):
        rs_ = min(max(i - 4, 0), 120); qi = i - rs_
        if qi == 4:
            EB = EBi
        else:
            if cur_edge != qi:
                load_eb(qi, EBe); cur_edge = qi
            EB = EBe
        qs_, qb_, qt_, vs_, va_, yr_, rc_, po_ = rr(qst, ir), rr(qb, ir), rr(QTr, ir), rr(vst, ir), rr(Va, ir), rr(yrow, ir), rr(rec, ir), rr(po, ir)
        S.dma("sp", lambda e: e.dma_start(out=qs_[:], in_=zscr[i * 64:(i + 1) * 64, 0:512]), reads=[zscr], writes=[qs_])
        S.dma("sp", lambda e: e.dma_start(out=vs_[:], in_=zscr.t[rs_ * 64:rs_ * 64 + 512, 1024:1536].rearrange("(j p) c -> p j c", p=128)),
              reads=[zscr], writes=[vs_])
        S.op("dve", lambda e: e.tensor_copy(out=qb_[:], in_=qs_[:]), reads=[qs_], writes=[qb_])
        for hp in range(4):
            S.op("pe", lambda e: e.transpose(out=pq[:, hp, :], in_=qb_[:, hp * 128:(hp + 1) * 128], identity=identb[0:64, 0:64]),
                 reads=[qb_, identb], writes=[pq], pe_acc=True)
        S.op("act", lambda e: e.copy(out=qt_[:], in_=pq[:]), reads=[pq], writes=[qt_])
        S.op("pool", lambda e: e.tensor_copy(out=va_[:, :, :, 0:64], in_=vs_.t[:].rearrange("p j (h d) -> p j h d", h=8)), reads=[vs_], writes=[va_])
        for h in range(8):
            hp, j2 = divmod(h, 2)
            pr = slice(j2 * 64, (j2 + 1) * 64)
            ps_, pt_ = rr(pss, cnt), rr(PT, cnt); cnt += 1
            for jt in range(4):
                k0 = rs_ * 64 + jt * 128
                S.op("pe", lambda e: e.matmul(ps_[:, jt * 64:(jt + 1) * 64], lhsT=KT[pr, hp, k0:k0 + 128], rhs=qt_[pr, hp, :], start=True, stop=True),
                     reads=[KT, qt_], writes=[ps_], pe_acc=(jt > 0))
            for jc in range(2):
                k0 = T + jc * 128
                S.op("pe", lambda e: e.matmul(ps_[:, 256 + jc * 64:256 + (jc + 1) * 64], lhsT=KT[pr, hp, k0:k0 + 128], rhs=qt_[pr, hp, :], start=True, stop=True),
                     reads=[KT, qt_], writes=[ps_], pe_acc=True)
            S.op("act", lambda e: e.activation(out=pt_[:], in_=ps_[:], func=AF.Exp, scale=0.125), reads=[ps_], writes=[pt_])
            S.op("dve", lambda e: e.tensor_tensor(out=pt_[:, 0:256], in0=pt_[:, 0:256], in1=EB[:, h, :], op=ALU.mult), reads=[pt_, EB], writes=[pt_])
            pot = po_[h // 4]
            for jt in range(4):
                S.op("pe", lambda e: e.matmul(pot[:, h % 4, :], lhsT=pt_[:, jt * 64:(jt + 1) * 64], rhs=va_[:, jt, h, :], start=(jt == 0), stop=False),
                     reads=[pt_, va_], writes=[pot], pe_acc=(jt > 0 or h % 4 > 0))
            for jc in range(2):
                S.op("pe", lambda e: e.matmul(pot[:, h % 4, :], lhsT=pt_[:, 256 + jc * 64:256 + (jc + 1) * 64], rhs=VCa[:, jc, h, :], start=False, stop=(jc == 1)),
                     reads=[pt_, VCa], writes=[pot], pe_acc=True)
        for hh in range(2):
            pot = po_[hh]
            S.op("dve", lambda e: e.reciprocal(out=rc_[:, hh * 4:(hh + 1) * 4, :], in_=pot[:, :, 64:65]), reads=[pot], writes=[rc_])
            S.op("dve", lambda e: e.tensor_tensor(out=yr_[:, hh * 4:(hh + 1) * 4, :], in0=pot[:, :, 0:64],
                                                  in1=rc_[:, hh * 4:(hh + 1) * 4, :].to_broadcast([64, 4, 64]), op=ALU.mult),
                 reads=[pot, rc_], writes=[yr_])
        S.dma("sp", lambda e: e.dma_start(out=ynascr[i * 64:(i + 1) * 64, :], in_=yr_.t[:].rearrange("p h d -> p (h d)")), reads=[yr_], writes=[ynascr])
    C.release(mB)
    if stop_after == "B_short":
        d = Buf("dbg", nc.dram_tensor("dbg", [8 * 64, 512], F32, kind="ExternalOutput").ap())
        for ir, i in enumerate(rowsB):
            S.dma("sp", lambda e: e.dma_start(out=d[ir * 64:(ir + 1) * 64, :], in_=ynascr[i * 64:(i + 1) * 64, :]), reads=[ynascr], writes=[d])
        C.barrier()
        print("ninst", S.ninst, "nwaits", S.nwaits)
        return nc
    yrwscr = build_phase_c(nc, C, S, zscr, ident, tri_d, rwp, stop_after)
    build_phase_def(nc, C, S, xin, zscr, modscr, ynascr, yrwscr, wD, identb, ident, out_d, stop_after)
    C.barrier()
    print("ninst", S.ninst, "nwaits", S.nwaits)
    return nc


def build_phase_c(nc, C, S, zscr, ident, tri_d, rwp, stop_after):
    yfscr = C.dram("yfscr", [T, 512]); yrwscr = C.dram("yrwscr", [T, 512])
    short = stop_after == "C_short"
    mC = C.mark()
    US, LS, UI, LI, SEL127, SEL0 = [C.sb(f"tri{i}", [128, 128]) for i in range(6)]
    for i, b in enumerate([US, LS, UI, LI, SEL127, SEL0]):
        S.dma("sp", lambda e: e.dma_start(out=b[:], in_=tri_d[i]), reads=[tri_d], writes=[b])

    def bc(key, ap, n):
        b = C.sb(key, [128, n])
        S.dma("sp", lambda e: e.dma_start(out=b[:], in_=ap.partition_broadcast(128)), reads=[rwp[key.split("#")[0]]], writes=[b])
        return b
    mu_p = bc("mu_prev", rwp["mu_prev"].t, 1920); mu_n = bc("mu_next", rwp["mu_next"].t, 1920)
    k_k = bc("k_k", rwp["k_k"].t, 512); k_a = bc("k_a", rwp["k_a"].t, 512); r_k = bc("r_k", rwp["r_k"].t, 512)
    gn_w = bc("gn_w", rwp["gn_w"].t, 512); gn_b = bc("gn_b", rwp["gn_b"].t, 512)
    w0 = [bc(f"w0#{d}", rwp["w0"][d], 512) for d in range(2)]; a0 = [bc(f"a0#{d}", rwp["a0"][d], 512) for d in range(2)]
    WUP = C.sb("WUP", [128, 512]); AUP = C.sb("AUP", [128, 512]); GUP = C.sb("GUP", [128, 512])
    for b, k in [(WUP, "w_up"), (AUP, "a_up"), (GUP, "g_up")]:
        S.dma("sp", lambda e: e.dma_start(out=b[:], in_=rwp[k][:, :]), reads=[rwp[k]], writes=[b])
    S0all = C.sb("S0all", [128, 64, 8])
    zc = C.sb("zc", [128, 1920]); zp = C.sb("zp", [128, 1920]); zn = C.sb("zn", [128, 1920]); zs = C.sb("zs", [128, 1920])
    lin = C.sb("lin", [128, 384]); LT = C.sb("LT", [128, 3, 128])
    f = {k: C.sb(k, [128, 512]) for k in ["kk", "logw", "a", "kmod", "g", "rrk", "tmp", "cum", "ep", "em", "eex",
                                          "At", "Rt", "Bt", "Kt", "Bh", "Kh", "pCb", "ysum", "yout"]}
    ss = C.sb("ss", [128, 8]); sd = C.sb("sd", [128, 8]); gmv = C.sb("gmv", [128, 8, 2]); gst = C.sb("gst", [128, 8, 6])
    TTt = C.sb("TTt", [128, 4, 4, 128])
    Dg = C.sb("Dg", [64, 8, 64])
    Nm = C.sb("Nm", [128, 128])
    NP = [C.sb("NP", [128, 2, 128]) for _ in range(2)]
    PRB = C.sb("PRB", [128, 2, 128]); AKRK = C.sb("AKRK", [128, 2, 128])
    X = [C.sb("X", [128, 128]) for _ in range(2)]
    McT = C.sb("McT", [64, 64]); NcS = C.sb("NcS", [64, 64]); QT = C.sb("QT", [64, 128])
    Hst = C.sb("Hst", [64, 8, 64])
    M2 = [C.sb("M2", [128, 2, 128]) for _ in range(2)]
    for d, (st, inc) in enumerate([(US, UI), (LS, LI)]):
        S.op("pool", lambda e: e.tensor_copy(out=M2[d][:, 0, :], in_=st[:]), reads=[st], writes=[M2[d]])
        S.op("pool", lambda e: e.tensor_copy(out=M2[d][:, 1, :], in_=inc[:]), reads=[inc], writes=[M2[d]])
    pbig = [C.ps("pbig", [128, 512]) for _ in range(2)]
    pT = C.ps("pT", [128, 4, 128])
    pA = C.ps("pA", [128, 2, 128]); pB = C.ps("pB", [128, 2, 128])
    pX = C.ps("pX", [128, 2, 128])
    pM = C.ps("pM", [64, 4, 64])
    pY = C.ps("pY", [128, 512])
    v3 = lambda b: b.t[:].rearrange("p (h d) -> p h d", h=8)
    NEG = -float(np.exp(-0.5))

    def features(n, d):
        t0 = n * 128
        first = n in (0, NT); lastt = n in (NT - 1, NTT - 1)
        S.dma("sp", lambda e: e.dma_start(out=zc[:], in_=zscr[t0:t0 + 128, RW0:RW0 + 1920]), reads=[zscr], writes=[zc])
        if first:
            S.op("pool", lambda e: e.memset(zp[:], 0.0), writes=[zp])
            S.dma("sp", lambda e: e.dma_start(out=zp[1:128, :], in_=zscr[t0:t0 + 127, RW0:RW0 + 1920]), reads=[zscr], writes=[zp])
        else:
            S.dma("sp", lambda e: e.dma_start(out=zp[:], in_=zscr[t0 - 1:t0 + 127, RW0:RW0 + 1920]), reads=[zscr], writes=[zp])
        if lastt:
            S.op("pool", lambda e: e.memset(zn[:], 0.0), writes=[zn])
            S.dma("sp", lambda e: e.dma_start(out=zn[0:127, :], in_=zscr[t0 + 1:t0 + 128, RW0:RW0 + 1920]), reads=[zscr], writes=[zn])
        else:
            S.dma("sp", lambda e: e.dma_start(out=zn[:], in_=zscr[t0 + 1:t0 + 129, RW0:RW0 + 1920]), reads=[zscr], writes=[zn])
        S.op("dve", lambda e: e.tensor_tensor(out=zp[:], in0=zp[:], in1=zc[:], op=ALU.subtract), reads=[zp, zc], writes=[zp])
        S.op("pool", lambda e: e.tensor_tensor(out=zn[:], in0=zn[:], in1=zc[:], op=ALU.subtract), reads=[zn, zc], writes=[zn])
        S.op("dve", lambda e: e.tensor_tensor(out=zp[:], in0=zp[:], in1=mu_p[:], op=ALU.mult), reads=[zp, mu_p], writes=[zp])
        S.op("pool", lambda e: e.tensor_tensor(out=zn[:], in0=zn[:], in1=mu_n[:], op=ALU.mult), reads=[zn, mu_n], writes=[zn])
        S.op("dve", lambda e: e.tensor_tensor(out=zs[:], in0=zc[:], in1=zp[:], op=ALU.add), reads=[zc, zp], writes=[zs])
        S.op("dve", lambda e: e.tensor_tensor(out=zs[:], in0=zs[:], in1=zn[:], op=ALU.add), reads=[zs, zn], writes=[zs])
        S.op("act", lambda e: e.activation(out=lin[:, 0:128], in_=zs[:, 1536:1664], func=AF.Tanh), reads=[zs], writes=[lin])
        S.op("act", lambda e: e.copy(out=lin[:, 128:256], in_=zs[:, 1664:1792]), reads=[zs], writes=[lin])
        S.op("act", lambda e: e.activation(out=lin[:, 256:384], in_=zs[:, 1792:1920], func=AF.Sigmoid), reads=[zs], writes=[lin])
        for j in range(3):
            S.op("pe", lambda e: e.transpose(out=pT[:, j, :], in_=lin[:, j * 128:(j + 1) * 128], identity=ident[:]), reads=[lin, ident], writes=[pT], pe_acc=True)
        S.op("act", lambda e: e.copy(out=LT[:], in_=pT[:, 0:3, :]), reads=[pT], writes=[LT])
        dp = slice(d * 64, (d + 1) * 64)
        S.op("pe", lambda e: e.matmul(pbig[0][:], lhsT=LT[dp, 0, :], rhs=WUP[dp, :], start=True, stop=True), reads=[LT, WUP], writes=[pbig[0]])
        S.op("dve", lambda e: e.tensor_tensor(out=f["logw"][:], in0=pbig[0][:], in1=w0[d][:], op=ALU.add), reads=[pbig[0], w0[d]], writes=[f["logw"]])
        S.op("act", lambda e: e.activation(out=f["logw"][:], in_=f["logw"][:], func=AF.Sigmoid), reads=[f["logw"]], writes=[f["logw"]])
        S.op("pool", lambda e: e.tensor_scalar_mul(out=f["logw"][:], in0=f["logw"][:], scalar1=NEG), reads=[f["logw"]], writes=[f["logw"]])
        S.op("pe", lambda e: e.matmul(pbig[1][:], lhsT=LT[dp, 1, :], rhs=AUP[dp, :], start=True, stop=True), reads=[LT, AUP], writes=[pbig[1]])
        S.op("dve", lambda e: e.tensor_tensor(out=f["a"][:], in0=pbig[1][:], in1=a0[d][:], op=ALU.add), reads=[pbig[1], a0[d]], writes=[f["a"]])
        S.op("act", lambda e: e.activation(out=f["a"][:], in_=f["a"][:], func=AF.Sigmoid), reads=[f["a"]], writes=[f["a"]])
        S.op("pe", lambda e: e.matmul(pbig[0][:], lhsT=LT[:, 2, :], rhs=GUP[:], start=True, stop=True), reads=[LT, GUP], writes=[pbig[0]])
        S.op("act", lambda e: e.copy(out=f["g"][:], in_=pbig[0][:]), reads=[pbig[0]], writes=[f["g"]])
        S.op("dve", lambda e: e.tensor_tensor(out=f["kk"][:], in0=zs[:, 512:1024], in1=k_k[:], op=ALU.mult), reads=[zs, k_k], writes=[f["kk"]])
        S.op("pool", lambda e: e.tensor_tensor(out=f["tmp"][:], in0=f["kk"][:], in1=f["kk"][:], op=ALU.mult), reads=[f["kk"]], writes=[f["tmp"]])
        S.op("dve", lambda e: e.tensor_reduce(out=ss[:], in_=v3(f["tmp"]), axis=AX.X, op=ALU.add), reads=[f["tmp"]], writes=[ss])
        S.op("dve", lambda e: e.tensor_scalar_add(out=ss[:], in0=ss[:], scalar1=1e-12), reads=[ss], writes=[ss])
        S.op("act", lambda e: e.activation(out=ss[:], in_=ss[:], func=AF.Ln), reads=[ss], writes=[ss])
        S.op("act", lambda e: e.activation(out=ss[:], in_=ss[:], func=AF.Exp, scale=-0.5), reads=[ss], writes=[ss])
        S.op("dve", lambda e: e.tensor_tensor(out=v3(f["kk"]), in0=v3(f["kk"]), in1=ss.t[:].unsqueeze(2).to_broadcast([128, 8, 64]), op=ALU.mult),
             reads=[f["kk"], ss], writes=[f["kk"]])
        S.op("dve", lambda e: e.scalar_tensor_tensor(out=f["kmod"][:], in0=f["a"][:], scalar=-1.0, in1=k_a[:], op0=ALU.add, op1=ALU.mult),
             reads=[f["a"], k_a], writes=[f["kmod"]])
        S.op("dve", lambda e: e.scalar_tensor_tensor(out=f["kmod"][:], in0=f["kmod"][:], scalar=1.0, in1=zs[:, 512:1024], op0=ALU.add, op1=ALU.mult),
             reads=[f["kmod"], zs], writes=[f["kmod"]])
        S.op("pool", lambda e: e.tensor_tensor(out=f["rrk"][:], in0=zs[:, 0:512], in1=r_k[:], op=ALU.mult), reads=[zs, r_k], writes=[f["rrk"]])
        S.op("pool", lambda e: e.tensor_tensor(out=f["tmp"][:], in0=f["rrk"][:], in1=f["kmod"][:], op=ALU.mult), reads=[f["rrk"], f["kmod"]], writes=[f["tmp"]])
        S.op("dve", lambda e: e.tensor_reduce(out=sd[:], in_=v3(f["tmp"]), axis=AX.X, op=ALU.add), reads=[f["tmp"]], writes=[sd])

    def chunk(n, d, emit, nheads=8, stage=6):
        Lm, maskN, sel = ((UI, LS, SEL127), (LI, US, SEL0))[d]
        m2 = M2[d]
        r_ = zs.t[:, 0:512]; v_ = zs.t[:, 1024:1536]
        S.op("pe", lambda e: e.matmul(pbig[1][:], lhsT=Lm[:], rhs=f["logw"][:], start=True, stop=True), reads=[Lm, f["logw"]], writes=[pbig[1]])
        S.op("act", lambda e: e.activation(out=f["ep"][:], in_=pbig[1][:], func=AF.Exp), reads=[pbig[1]], writes=[f["ep"]])
        S.op("act", lambda e: e.activation(out=f["em"][:], in_=pbig[1][:], func=AF.Exp, scale=-1.0), reads=[pbig[1]], writes=[f["em"]])
        S.op("dve", lambda e: e.tensor_tensor(out=f["cum"][:], in0=pbig[1][:], in1=f["logw"][:], op=ALU.subtract), reads=[pbig[1], f["logw"]], writes=[f["cum"]])
        S.op("act", lambda e: e.activation(out=f["eex"][:], in_=f["cum"][:], func=AF.Exp), reads=[f["cum"]], writes=[f["eex"]])
        S.op("dve", lambda e: e.scalar_tensor_tensor(out=f["At"][:], in0=f["kk"][:], scalar=-1.0, in1=f["eex"][:], op0=ALU.mult, op1=ALU.mult),
             reads=[f["kk"], f["eex"]], writes=[f["At"]])
        S.op("pool", lambda e: e.tensor_tensor(out=f["Rt"][:], in0=r_, in1=f["ep"][:], op=ALU.mult), reads=[zs, f["ep"]], writes=[f["Rt"]])
        S.op("dve", lambda e: e.tensor_tensor(out=f["Bt"][:], in0=f["kk"][:], in1=f["a"][:], op=ALU.mult), reads=[f["kk"], f["a"]], writes=[f["Bt"]])
        S.op("dve", lambda e: e.tensor_tensor(out=f["Bt"][:], in0=f["Bt"][:], in1=f["em"][:], op=ALU.mult), reads=[f["Bt"], f["em"]], writes=[f["Bt"]])
        S.op("pool", lambda e: e.tensor_tensor(out=f["Kt"][:], in0=f["kmod"][:], in1=f["em"][:], op=ALU.mult), reads=[f["kmod"], f["em"]], writes=[f["Kt"]])
        S.op("pe", lambda e: e.matmul(pbig[0][:], lhsT=sel[:], rhs=f["ep"][:], start=True, stop=True), reads=[sel, f["ep"]], writes=[pbig[0]])
        S.op("act", lambda e: e.copy(out=f["pCb"][:], in_=pbig[0][:]), reads=[pbig[0]], writes=[f["pCb"]])
        S.op("dve", lambda e: e.tensor_tensor(out=f["Bh"][:], in0=f["Bt"][:], in1=f["pCb"][:], op=ALU.mult), reads=[f["Bt"], f["pCb"]], writes=[f["Bh"]])
        S.op("pool", lambda e: e.tensor_tensor(out=f["Kh"][:], in0=f["Kt"][:], in1=f["pCb"][:], op=ALU.mult), reads=[f["Kt"], f["pCb"]], writes=[f["Kh"]])
        S.op("dve", lambda e: e.tensor_tensor(out=Dg[:], in0=f["pCb"].t[0:64, :].rearrange("p (h d) -> p h d", h=8),
                                              in1=ident.t[0:64, 0:64].unsqueeze(1).to_broadcast([64, 8, 64]), op=ALU.mult),
             reads=[f["pCb"], ident], writes=[Dg])
        for ai, key in enumerate(["At", "Rt", "Bt", "Kt"]):
            for hp in range(4):
                S.op("pe", lambda e: e.transpose(out=pT[:, hp, :], in_=f[key][:, hp * 128:(hp + 1) * 128], identity=ident[:]),
                     reads=[f[key], ident], writes=[pT], pe_acc=True)
            S.op(("act", "dve")[ai % 2], (lambda e: e.copy(out=TTt[:, :, ai, :], in_=pT[:])) if ai % 2 == 0 else
                 (lambda e: e.tensor_copy(out=TTt[:, :, ai, :], in_=pT[:])), reads=[pT], writes=[TTt])
        for h in range(nheads if stage >= 2 else 0):
            hp, j2 = divmod(h, 2)
            pr = slice(j2 * 64, (j2 + 1) * 64); hs = slice(h * 64, (h + 1) * 64)
            AR = TTt[pr, hp, 0:2, :]; AtT = TTt[pr, hp, 0, :]; BtT = TTt[pr, hp, 2, :]; KtT = TTt[pr, hp, 3, :]
            S.op("pe", lambda e: e.matmul(pA[:], lhsT=BtT, rhs=AR, start=True, stop=True), reads=[TTt], writes=[pA])
            S.op("pe", lambda e: e.matmul(pB[:], lhsT=KtT, rhs=AR, start=True, stop=True), reads=[TTt], writes=[pB])
            S.op("pe", lambda e: e.matmul(pX[:, 0, :], lhsT=AtT, rhs=BtT, start=True, stop=True), reads=[TTt], writes=[pX])
            S.op("dve", lambda e: e.tensor_tensor(out=PRB[:], in0=pA[:], in1=m2[:], op=ALU.mult), reads=[pA, m2], writes=[PRB])
            S.op("dve", lambda e: e.tensor_tensor(out=AKRK[:], in0=pB[:], in1=m2[:], op=ALU.mult), reads=[pB, m2], writes=[AKRK])
            S.op("dve", lambda e: e.tensor_tensor(out=NP[0][:, 0, :], in0=pX[:, 0, :], in1=maskN[:], op=ALU.mult), reads=[pX, maskN], writes=[NP[0]])
            S.op("dve", lambda e: e.tensor_copy(out=NP[0][:, 1, :], in_=PRB[:, 0, :]), reads=[PRB], writes=[NP[0]])
            S.op("pe", lambda e: e.matmul(pX[:, 1, 0:64], lhsT=AKRK[:, 0, :], rhs=v_[:, hs], start=True, stop=True), reads=[AKRK, zs], writes=[pX])
            S.op("dve", lambda e: e.tensor_copy(out=X[0][:, 0:64], in_=f["At"][:, hs]), reads=[f["At"]], writes=[X[0]])
            S.op("act", lambda e: e.copy(out=X[0][:, 64:128], in_=pX[:, 1, 0:64]), reads=[pX], writes=[X[0]])
            for i in range(7 if stage >= 3 else 0):
                npi, npn = NP[i % 2], NP[(i + 1) % 2]
                xi, xn = X[i % 2], X[(i + 1) % 2]
                S.op("pe", lambda e: e.matmul(pX[:, 0, :], lhsT=npi[:, 1, :], rhs=xi[:], start=True, stop=True), reads=[npi, xi], writes=[pX])
                S.op("dve", lambda e: e.tensor_tensor(out=xn[:], in0=pX[:, 0, :], in1=xi[:], op=ALU.add), reads=[pX, xi], writes=[xn])
                if i < 6:
                    S.op("pe", lambda e: e.matmul(pA[:, 0, :], lhsT=npi[:, 1, :], rhs=npi[:, 0, :], start=True, stop=True), reads=[npi], writes=[pA])
                    S.op("pe", lambda e: e.matmul(pA[:, 1, :], lhsT=npi[:, 0, :], rhs=npi[:, 1, :], start=True, stop=True), reads=[npi], writes=[pA], pe_acc=True)
                    S.op("act", lambda e: e.copy(out=npn[:], in_=pA[:]), reads=[pA], writes=[npn])
            Xf = X[1]
            if stage < 4:
                continue
            G = Xf[:, 0:64]; Ul = Xf[:, 64:128]
            sub = 0
            if sub in (0, 1):
                S.op("pe", lambda e: e.matmul(pM[:, 0, :], lhsT=G, rhs=f["Bh"][:, hs], start=True, stop=True), reads=[Xf, f["Bh"]], writes=[pM])
            if sub in (0, 2):
                S.op("pe", lambda e: e.matmul(pM[:, 1, :], lhsT=f["Bh"][:, hs], rhs=Ul, start=True, stop=True), reads=[Xf, f["Bh"]], writes=[pM], pe_acc=True)
                S.op("pe", lambda e: e.matmul(pM[:, 3, :], lhsT=f["Kh"][:, hs], rhs=v_[:, hs], start=True, stop=True), reads=[f["Kh"], zs], writes=[pM], pe_acc=True)
            if sub in (0, 1):
                S.op("dve", lambda e: e.tensor_tensor(out=McT[:], in0=pM[:, 0, :], in1=Dg[:, h, :], op=ALU.add), reads=[pM, Dg], writes=[McT])
            if sub in (0, 2):
                S.op("dve", lambda e: e.tensor_copy(out=NcS[:], in_=pM[:, 1, :]), reads=[pM], writes=[NcS])
                S.op("dve", lambda e: e.tensor_tensor(out=NcS[:], in0=pM[:, 3, :], in1=NcS[:], op=ALU.add), reads=[pM, NcS], writes=[NcS])
            if emit and stage >= 5:
                S.op("pe", lambda e: e.matmul(pB[0:64, 0, :], lhsT=G, rhs=PRB[:, 1, :], start=True, stop=True), reads=[Xf, PRB], writes=[pB])
                S.op("pe", lambda e: e.matmul(pB[0:64, 1, :], lhsT=f["Rt"][:, hs], rhs=ident[:], start=True, stop=True), reads=[f["Rt"], ident], writes=[pB], pe_acc=True)
                S.op("dve", lambda e: e.tensor_copy(out=QT[:], in_=pB[0:64, 0, :]), reads=[pB], writes=[QT])
                S.op("dve", lambda e: e.tensor_tensor(out=QT[:], in0=pB[0:64, 1, :], in1=QT[:], op=ALU.add), reads=[pB, QT], writes=[QT])
                S.op("pe", lambda e: e.matmul(pY[:, hs], lhsT=QT[:], rhs=Hst[:, h, :], start=True, stop=False), reads=[QT, Hst], writes=[pY], pe_acc=(h > 0))
                S.op("pe", lambda e: e.matmul(pY[:, hs], lhsT=PRB[:, 1, :], rhs=Ul, start=False, stop=False), reads=[PRB, Xf], writes=[pY], pe_acc=True)
                S.op("pe", lambda e: e.matmul(pY[:, hs], lhsT=AKRK[:, 1, :], rhs=v_[:, hs], start=False, stop=True), reads=[AKRK, zs], writes=[pY], pe_acc=True)
            if stage < 6:
                continue
            S.op("pe", lambda e: e.matmul(pM[:, 2, :], lhsT=McT[:], rhs=Hst[:, h, :], start=True, stop=True), reads=[McT, Hst], writes=[pM])
            S.op("dve", lambda e: e.tensor_tensor(out=Hst[:, h, :], in0=pM[:, 2, :], in1=NcS[:], op=ALU.add), reads=[pM, NcS], writes=[Hst])

    if stop_after == "C_feat":
        dd = Buf("dbg", nc.dram_tensor("dbg", [2, 5, 128, 512], F32, kind="ExternalOutput").ap())
        for ti, (n, d) in enumerate([(0, 0), (63, 1)]):
            features(n, d)
            for ki, key in enumerate(["logw", "a", "kmod", "kk", "g"]):
                S.dma("sp", lambda e: e.dma_start(out=dd[ti, ki], in_=f[key][:]), reads=[f[key]], writes=[dd])
        C.barrier()
        print("ninst", S.ninst, "nwaits", S.nwaits)
        return nc
    if stop_after == "C_one":
        nh = 8
        dd = Buf("dbg", nc.dram_tensor("dbg", [128, 512], F32, kind="ExternalOutput").ap())
        dh = Buf("dbgh", nc.dram_tensor("dbgh", [64, 512], F32, kind="ExternalOutput").ap())
        S.op("pool", lambda e: e.memset(Hst[:], 0.0), writes=[Hst])
        features(0, 0)
        stg = 6
        chunk(0, 0, True, nheads=nh, stage=stg)
        if stg >= 5:
            S.op("act", lambda e: e.copy(out=f["yout"][:, 0:nh * 64], in_=pY[:, 0:nh * 64]), reads=[pY], writes=[f["yout"]])
        else:
            src = {1: f["Bh"], 2: X[0], 3: X[1], 4: X[1]}[stg]
            S.op("act", lambda e: e.copy(out=f["yout"][:, 0:128], in_=src[:, 0:128]), reads=[src], writes=[f["yout"]])
        S.dma("sp", lambda e: e.dma_start(out=dd[:, 0:nh * 64], in_=f["yout"][:, 0:nh * 64]), reads=[f["yout"]], writes=[dd])
        S.dma("sp", lambda e: e.dma_start(out=dh[:, :], in_=Hst.t[:].rearrange("p h d -> p (h d)")), reads=[Hst], writes=[dh])
        C.barrier()
        print("ninst", S.ninst, "nwaits", S.nwaits)
        return nc
    dbg_rows = []
    for d in range(2):
        if d == 0:
            order = [NT, NT + 1] + (list(range(NT)) if not short else [0, 1])
        else:
            order = [NT + 1, NT] + (list(range(NT - 1, -1, -1)) if not short else [63, 62])
        S.op("pool", lambda e: e.memset(Hst[:], 0.0), writes=[Hst])
        for n in order:
            emit = n < NT
            features(n, d)
            chunk(n, d, emit)
            if not emit:
                continue
            rows = slice(n * 128, (n + 1) * 128)
            if d == 0:
                S.op("act", lambda e: e.copy(out=f["yout"][:], in_=pY[:]), reads=[pY], writes=[f["yout"]])
                S.op("pool", lambda e: e.tensor_copy(out=S0all[:, n, :], in_=sd[:]), reads=[sd], writes=[S0all])
                S.dma("sp", lambda e: e.dma_start(out=yfscr[rows, :], in_=f["yout"][:]), reads=[f["yout"]], writes=[yfscr])
                continue
            if short:
                S.op("act", lambda e: e.copy(out=f["yout"][:], in_=pY[:]), reads=[pY], writes=[f["yout"]])
                S.dma("sp", lambda e: e.dma_start(out=yrwscr[rows, :], in_=f["yout"][:]), reads=[f["yout"]], writes=[yrwscr])
                continue
            S.dma("sp", lambda e: e.dma_start(out=f["ysum"][:], in_=yfscr[rows, :]), reads=[yfscr], writes=[f["ysum"]])
            S.op("dve", lambda e: e.tensor_tensor(out=f["ysum"][:], in0=pY[:], in1=f["ysum"][:], op=ALU.add), reads=[pY, f["ysum"]], writes=[f["ysum"]])
            for h in range(8):
                S.op("dve", lambda e: e.bn_stats(out=gst[:, h, :], in_=f["ysum"][:, h * 64:(h + 1) * 64]), reads=[f["ysum"]], writes=[gst])
                S.op("dve", lambda e: e.bn_aggr(out=gmv[:, h, :], in_=gst[:, h, :]), reads=[gst], writes=[gmv])
            S.op("dve", lambda e: e.tensor_scalar_add(out=ss[:], in0=gmv[:, :, 1], scalar1=GN_EPS), reads=[gmv], writes=[ss])
            S.op("act", lambda e: e.activation(out=ss[:], in_=ss[:], func=AF.Ln), reads=[ss], writes=[ss])
            S.op("act", lambda e: e.activation(out=ss[:], in_=ss[:], func=AF.Exp, scale=-0.5), reads=[ss], writes=[ss])
            y3 = v3(f["ysum"])
            S.op("dve", lambda e: e.tensor_tensor(out=y3, in0=y3, in1=gmv.t[:, :, 0:1].to_broadcast([128, 8, 64]), op=ALU.subtract), reads=[f["ysum"], gmv], writes=[f["ysum"]])
            S.op("dve", lambda e: e.tensor_tensor(out=y3, in0=y3, in1=ss.t[:].unsqueeze(2).to_broadcast([128, 8, 64]), op=ALU.mult), reads=[f["ysum"], ss], writes=[f["ysum"]])
            S.op("pool", lambda e: e.tensor_tensor(out=f["ysum"][:], in0=f["ysum"][:], in1=gn_w[:], op=ALU.mult), reads=[f["ysum"], gn_w], writes=[f["ysum"]])
            S.op("pool", lambda e: e.tensor_tensor(out=f["ysum"][:], in0=f["ysum"][:], in1=gn_b[:], op=ALU.add), reads=[f["ysum"], gn_b], writes=[f["ysum"]])
            S.op("dve", lambda e: e.tensor_tensor(out=sd[:], in0=sd[:], in1=S0all[:, n, :], op=ALU.add), reads=[sd, S0all], writes=[sd])
            S.op("dve", lambda e: e.tensor_tensor(out=v3(f["tmp"]), in0=zs.t[:, 1024:1536].rearrange("p (h d) -> p h d", h=8),
                                                  in1=sd.t[:].unsqueeze(2).to_broadcast([128, 8, 64]), op=ALU.mult), reads=[zs, sd], writes=[f["tmp"]])
            S.op("dve", lambda e: e.tensor_tensor(out=f["ysum"][:], in0=f["ysum"][:], in1=f["tmp"][:], op=ALU.add), reads=[f["ysum"], f["tmp"]], writes=[f["ysum"]])
            S.op("pool", lambda e: e.tensor_tensor(out=f["yout"][:], in0=f["ysum"][:], in1=f["g"][:], op=ALU.mult), reads=[f["ysum"], f["g"]], writes=[f["yout"]])
            S.dma("sp", lambda e: e.dma_start(out=yrwscr[rows, :], in_=f["yout"][:]), reads=[f["yout"]], writes=[yrwscr])
    C.release(mC)
    if short:
        dd = Buf("dbg", nc.dram_tensor("dbg", [512, 512], F32, kind="ExternalOutput").ap())
        S.dma("sp", lambda e: e.dma_start(out=dd[0:256, :], in_=yfscr[0:256, :]), reads=[yfscr], writes=[dd])
        S.dma("sp", lambda e: e.dma_start(out=dd[256:512, :], in_=yrwscr[62 * 128:64 * 128, :]), reads=[yrwscr], writes=[dd])
        C.barrier()
        print("ninst", S.ninst, "nwaits", S.nwaits)
        return nc
    return yrwscr


def build_phase_def(nc, C, S, xin, zscr, modscr, ynascr, yrwscr, wD, identb, ident, out_d, stop_after):
    h1scr = C.dram("h1scr", [T, D]); u2Tscr = C.dram("u2Tscr", [8, 128, T], BF16)
    AFF = C.sb("AFF", [128, NT, NE]); GM = C.sb("GM", [128, NT, NE])

    def bcast(name, src_buf, src_ap, eng="sp"):
        b = C.sb(name, [128, D])
        S.dma(eng, lambda e: e.dma_start(out=b[:], in_=src_ap.partition_broadcast(128)), reads=[src_buf], writes=[b])
        return b

    def ln_rows(xt, stats, mv, rstd, eps):
        for hf in range(2):
            S.op("dve", lambda e: e.bn_stats(out=stats[:, hf, :], in_=xt[:, hf * 512:(hf + 1) * 512]), reads=[xt], writes=[stats])
        S.op("dve", lambda e: e.bn_aggr(out=mv[:], in_=stats[:]), reads=[stats], writes=[mv])
        S.op("dve", lambda e: e.tensor_scalar_add(out=rstd[:], in0=mv[:, 1:2], scalar1=eps), reads=[mv], writes=[rstd])
        S.op("act", lambda e: e.activation(out=rstd[:], in_=rstd[:], func=AF.Ln), reads=[rstd], writes=[rstd])
        S.op("act", lambda e: e.activation(out=rstd[:], in_=rstd[:], func=AF.Exp, scale=-0.5), reads=[rstd], writes=[rstd])
        S.op("dve", lambda e: e.tensor_scalar(out=xt[:], in0=xt[:], scalar1=mv[:, 0:1], scalar2=rstd[:, 0:1], op0=ALU.subtract, op1=ALU.mult),
             reads=[xt, mv, rstd], writes=[xt])

    mD = C.mark()
    wst = C.sb("wstD", [128, 8, D])
    wpa = C.sb("wpa", [128, 4, D], BF16); wpr = C.sb("wpr", [128, 4, D], BF16); wo = C.sb("wo", [128, 8, D], BF16)
    wr = C.sb("wr", [128, 8, NE], BF16); wrs = C.sb("wrs", [128, 8, NE])
    for dst, key, nk in [(wpa, "w_pa", 4), (wpr, "w_pr", 4), (wo, "w_o", 8)]:
        S.dma("sp", lambda e: e.dma_start(out=wst[:, 0:nk, :], in_=wD[key].t.rearrange("(kc p) n -> p kc n", p=128)), reads=[wD[key]], writes=[wst])
        S.op("dve", lambda e: e.tensor_copy(out=dst[:], in_=wst[:, 0:nk, :]), reads=[wst], writes=[dst])
    S.dma("sp", lambda e: e.dma_start(out=wrs[:], in_=wD["w_router"].t.rearrange("(kc p) n -> p kc n", p=128)), reads=[wD["w_router"]], writes=[wrs])
    S.op("dve", lambda e: e.tensor_copy(out=wr[:], in_=wrs[:]), reads=[wrs], writes=[wr])
    gt1 = bcast("gt1", modscr, modscr[0, 2 * D:3 * D]); sh2 = bcast("sh2", modscr, modscr[0, 3 * D:4 * D]); sc2 = bcast("sc2", modscr, modscr[0, 4 * D:5 * D])
    l1g = bcast("l1g", wD["ln1_g"], wD["ln1_g"].t); l1b = bcast("l1b", wD["ln1_b"], wD["ln1_b"].t)
    yna = C.sb("yna", [128, 1024]); ynb = C.sb("ynb", [128, 1024], BF16)
    zg = C.sb("zg", [128, 2048]); xt = C.sb("xtD", [128, D]); pre = C.sb("pre", [128, D]); t1 = C.sb("t1", [128, D])
    mb = C.sb("mb", [128, D], BF16); u2b = C.sb("u2b", [128, D], BF16)
    yT = C.sb("yT", [128, 8, 128], BF16); mT = C.sb("mT", [128, 8, 128], BF16); u2T = C.sb("u2T", [128, 8, 128], BF16)
    stats = C.sb("statsD", [128, 2, 6]); mv = C.sb("mvD", [128, 2]); rstd = C.sb("rstdD", [128, 1])
    ex = C.sb("ex", [128, NE]); sm = C.sb("sm", [128, 1])
    ptp = C.ps("ptpD", [128, 8, 128], BF16)
    pa = [C.ps("paD", [128, 512]) for _ in range(2)]; pr_ = [C.ps("prD", [128, 512]) for _ in range(2)]
    pl = C.ps("plD", [128, NE])
    tilesD = range(NT) if stop_after != "F_short" else range(0)
    for n in tilesD:
        rows = slice(n * 128, (n + 1) * 128)
        S.dma("sp", lambda e: e.dma_start(out=yna[:, 0:512], in_=ynascr[rows, :]), reads=[ynascr], writes=[yna])
        S.dma("sp", lambda e: e.dma_start(out=yna[:, 512:1024], in_=yrwscr[rows, :]), reads=[yrwscr], writes=[yna])
        S.dma("sp", lambda e: e.dma_start(out=zg[:], in_=zscr[rows, G0:G0 + 2048]), reads=[zscr], writes=[zg])
        S.dma("sp", lambda e: e.dma_start(out=xt[:], in_=xin[rows, :]), reads=[xin], writes=[xt])
        S.op("pool", lambda e: e.tensor_copy(out=ynb[:], in_=yna[:]), reads=[yna], writes=[ynb])
        for c in range(8):
            S.op("pe", lambda e: e.transpose(out=ptp[:, c, :], in_=ynb[:, c * 128:(c + 1) * 128], identity=identb[:]), reads=[ynb, identb], writes=[ptp], pe_acc=True)
        S.op("act", lambda e: e.copy(out=yT[:], in_=ptp[:]), reads=[ptp], writes=[yT])
        S.op("act", lambda e: e.activation(out=zg[:], in_=zg[:], func=AF.Sigmoid), reads=[zg], writes=[zg])
        for hf in range(2):
            cs = slice(hf * 512, (hf + 1) * 512)
            for c in range(4):
                S.op("pe", lambda e: e.matmul(pa[hf][:], lhsT=yT[:, c, :], rhs=wpa[:, c, cs], start=(c == 0), stop=(c == 3)), reads=[yT, wpa], writes=[pa[hf]], pe_acc=(c > 0))
            for c in range(4):
                S.op("pe", lambda e: e.matmul(pr_[hf][:], lhsT=yT[:, 4 + c, :], rhs=wpr[:, c, cs], start=(c == 0), stop=(c == 3)), reads=[yT, wpr], writes=[pr_[hf]], pe_acc=(c > 0))
            S.op("dve", lambda e: e.tensor_tensor(out=pre[:, cs], in0=pa[hf][:], in1=zg[:, hf * 512:(hf + 1) * 512], op=ALU.mult), reads=[pa[hf], zg], writes=[pre])
            S.op("dve", lambda e: e.tensor_tensor(out=t1[:, cs], in0=pr_[hf][:], in1=zg[:, 1024 + hf * 512:1024 + (hf + 1) * 512], op=ALU.mult), reads=[pr_[hf], zg], writes=[t1])
        S.op("pool", lambda e: e.tensor_tensor(out=mb[:], in0=pre[:], in1=t1[:], op=ALU.add), reads=[pre, t1], writes=[mb])
        for c in range(8):
            S.op("pe", lambda e: e.transpose(out=ptp[:, c, :], in_=mb[:, c * 128:(c + 1) * 128], identity=identb[:]), reads=[mb, identb], writes=[ptp], pe_acc=(c > 0))
        S.op("act", lambda e: e.copy(out=mT[:], in_=ptp[:]), reads=[ptp], writes=[mT])
        for hf in range(2):
            cs = slice(hf * 512, (hf + 1) * 512)
            for c in range(8):
                S.op("pe", lambda e: e.matmul(pa[hf][:], lhsT=mT[:, c, :], rhs=wo[:, c, cs], start=(c == 0), stop=(c == 7)), reads=[mT, wo], writes=[pa[hf]], pe_acc=(c > 0))
            S.op("dve", lambda e: e.tensor_tensor(out=pre[:, cs], in0=pa[hf][:], in1=gt1[:, cs], op=ALU.mult), reads=[pa[hf], gt1], writes=[pre])
        S.op("dve", lambda e: e.scalar_tensor_tensor(out=pre[:], in0=xt[:], scalar=float(ALPHA), in1=pre[:], op0=ALU.mult, op1=ALU.add), reads=[xt, pre], writes=[pre])
        ln_rows(pre, stats, mv, rstd, LN_EPS)
        S.op("pool", lambda e: e.tensor_tensor(out=pre[:], in0=pre[:], in1=l1g[:], op=ALU.mult), reads=[pre, l1g], writes=[pre])
        S.op("pool", lambda e: e.tensor_tensor(out=pre[:], in0=pre[:], in1=l1b[:], op=ALU.add), reads=[pre, l1b], writes=[pre])
        S.dma("sp", lambda e: e.dma_start(out=h1scr[rows, :], in_=pre[:]), reads=[pre], writes=[h1scr])
        S.op("dve", lambda e: e.tensor_copy(out=t1[:], in_=pre[:]), reads=[pre], writes=[t1])
        ln_rows(t1, stats, mv, rstd, LN_EPS)
        S.op("pool", lambda e: e.tensor_tensor(out=t1[:], in0=t1[:], in1=sc2[:], op=ALU.mult), reads=[t1, sc2], writes=[t1])
        S.op("pool", lambda e: e.tensor_tensor(out=u2b[:], in0=t1[:], in1=sh2[:], op=ALU.add), reads=[t1, sh2], writes=[u2b])
        for c in range(8):
            S.op("pe", lambda e: e.transpose(out=ptp[:, c, :], in_=u2b[:, c * 128:(c + 1) * 128], identity=identb[:]), reads=[u2b, identb], writes=[ptp], pe_acc=(c > 0))
        S.op("act", lambda e: e.copy(out=u2T[:], in_=ptp[:]), reads=[ptp], writes=[u2T])
        S.dma("sp", lambda e: e.dma_start(out=u2Tscr.t[:, :, rows].rearrange("k p t -> p k t"), in_=u2T[:]), reads=[u2T], writes=[u2Tscr])
        for c in range(8):
            S.op("pe", lambda e: e.matmul(pl[:], lhsT=u2T[:, c, :], rhs=wr[:, c, :], start=(c == 0), stop=(c == 7)), reads=[u2T, wr], writes=[pl], pe_acc=(c > 0))
        S.op("act", lambda e: e.activation(out=ex[:], in_=pl[:], func=AF.Exp), reads=[pl], writes=[ex])
        S.op("dve", lambda e: e.tensor_reduce(out=sm[:], in_=ex[:], axis=AX.X, op=ALU.add), reads=[ex], writes=[sm])
        S.op("dve", lambda e: e.reciprocal(out=sm[:], in_=sm[:]), reads=[sm], writes=[sm])
        S.op("dve", lambda e: e.tensor_scalar(out=AFF[:, n, :], in0=ex[:], scalar1=sm[:, 0:1], scalar2=None, op0=ALU.mult), reads=[ex, sm], writes=[AFF])
    C.release(mD)

    mE = C.mark()
    ones = C.sb("ones", [128, 128]); lo = C.sb("lo", [128, NE]); hi = C.sb("hi", [128, NE]); mid = C.sb("mid", [128, NE])
    cmpb = C.sb("cmpb", [128, NT, NE]); cp = C.sb("cp", [128, NE]); ge = C.sb("ge", [128, NE]); dl = C.sb("dl", [128, NE])
    pc = C.ps("pcE", [128, NE])
    S.op("pool", lambda e: e.memset(ones[:], 1.0), writes=[ones])
    S.op("pool", lambda e: e.memset(lo[:], 0.0), writes=[lo])
    S.op("pool", lambda e: e.memset(hi[:], 1.0), writes=[hi])
    for it in range(34 if stop_after != "F_short" else 1):
        S.op("dve", lambda e: e.tensor_tensor(out=mid[:], in0=lo[:], in1=hi[:], op=ALU.add), reads=[lo, hi], writes=[mid])
        S.op("dve", lambda e: e.tensor_scalar_mul(out=mid[:], in0=mid[:], scalar1=0.5), reads=[mid], writes=[mid])
        S.op("dve", lambda e: e.tensor_tensor(out=cmpb[:], in0=AFF[:], in1=mid.t[:].unsqueeze(1).to_broadcast([128, NT, NE]), op=ALU.is_ge), reads=[AFF, mid], writes=[cmpb])
        S.op("dve", lambda e: e.tensor_reduce(out=cp[:], in_=cmpb.t[:].rearrange("p n e -> p e n"), axis=AX.X, op=ALU.add), reads=[cmpb], writes=[cp])
        S.op("pe", lambda e: e.matmul(pc[:], lhsT=ones[:], rhs=cp[:], start=True, stop=True), reads=[ones, cp], writes=[pc])
        S.op("dve", lambda e: e.tensor_single_scalar(out=ge[:], in_=pc[:], scalar=float(CAP) - 0.5, op=ALU.is_ge), reads=[pc], writes=[ge])
        S.op("dve", lambda e: e.tensor_tensor(out=dl[:], in0=mid[:], in1=lo[:], op=ALU.subtract), reads=[mid, lo], writes=[dl])
        S.op("dve", lambda e: e.tensor_tensor(out=dl[:], in0=dl[:], in1=ge[:], op=ALU.mult), reads=[dl, ge], writes=[dl])
        S.op("dve", lambda e: e.tensor_tensor(out=lo[:], in0=lo[:], in1=dl[:], op=ALU.add), reads=[lo, dl], writes=[lo])
        S.op("dve", lambda e: e.tensor_tensor(out=dl[:], in0=hi[:], in1=mid[:], op=ALU.subtract), reads=[hi, mid], writes=[dl])
        S.op("dve", lambda e: e.tensor_tensor(out=dl[:], in0=dl[:], in1=ge[:], op=ALU.mult), reads=[dl, ge], writes=[dl])
        S.op("dve", lambda e: e.tensor_tensor(out=hi[:], in0=mid[:], in1=dl[:], op=ALU.add), reads=[mid, dl], writes=[hi])
    S.op("dve", lambda e: e.tensor_tensor(out=cmpb[:], in0=AFF[:], in1=lo.t[:].unsqueeze(1).to_broadcast([128, NT, NE]), op=ALU.is_ge), reads=[AFF, lo], writes=[cmpb])
    S.op("dve", lambda e: e.tensor_tensor(out=GM[:], in0=AFF[:], in1=cmpb[:], op=ALU.mult), reads=[AFF, cmpb], writes=[GM])
    C.release(mE)

    mF = C.mark()
    gt2 = bcast("gt2", modscr, modscr[0, 5 * D:6 * D]); l2g = bcast("l2g", wD["ln2_g"], wD["ln2_g"].t); l2b = bcast("l2b", wD["ln2_b"], wD["ln2_b"].t)
    TB = 512
    u2blk = C.sb("u2blk", [128, 8, TB], BF16)
    hT = C.sb("hT", [128, 22, TB], BF16); W2b = C.sb("W2b", [128, 22, D], BF16)
    acc = C.sb("acc", [128, 4, D])
    w1s = [C.sb("w1s", [128, 8, 128]) for _ in range(2)]; w3s = [C.sb("w3s", [128, 8, 128]) for _ in range(2)]; w2s = [C.sb("w2s", [128, D]) for _ in range(2)]
    w1b = [C.sb("w1b", [128, 8, 128], BF16) for _ in range(2)]; w3b = [C.sb("w3b", [128, 8, 128], BF16) for _ in range(2)]
    sg = [C.sb("sg", [128, TB]) for _ in range(2)]
    h1t = C.sb("h1t", [128, D]); stats = C.sb("statsF", [128, 2, 6]); mv = C.sb("mvF", [128, 2]); rstd = C.sb("rstdF", [128, 1])
    ph1 = [C.ps("ph1", [128, TB]) for _ in range(2)]; ph3 = [C.ps("ph3", [128, TB]) for _ in range(2)]
    py = [C.ps("py", [128, 512]) for _ in range(2)]
    w1v = wD["w_e1"].t.rearrange("e (kc p) f -> e p kc f", p=128); w3v = wD["w_e3"].t.rearrange("e (kc p) f -> e p kc f", p=128)
    cnt = 0
    for bk in range(T // TB if stop_after != "F_short" else 1):
        t0 = bk * TB
        S.dma("sp", lambda e: e.dma_start(out=u2blk[:], in_=u2Tscr.t[:, :, t0:t0 + TB].rearrange("k p t -> p k t")), reads=[u2Tscr], writes=[u2blk])
        S.op("pool", lambda e: e.memset(acc[:], 0.0), writes=[acc])
        for ex_ in range(NE if stop_after != "F_short" else 2):
            for j in range(22):
                a1, a3, a2, b1, b3 = rr(w1s, cnt), rr(w3s, cnt), rr(w2s, cnt), rr(w1b, cnt), rr(w3b, cnt)
                p1, p3, sg_ = rr(ph1, cnt), rr(ph3, cnt), rr(sg, cnt); cnt += 1
                fs = slice(j * 128, (j + 1) * 128)
                S.dma("sp", lambda e: e.dma_start(out=a1[:], in_=w1v[ex_, :, :, fs]), reads=[wD["w_e1"]], writes=[a1])
                S.dma("sp", lambda e: e.dma_start(out=a3[:], in_=w3v[ex_, :, :, fs]), reads=[wD["w_e3"]], writes=[a3])
                S.dma("sp", lambda e: e.dma_start(out=a2[:], in_=wD["w_e2"][ex_, fs, :]), reads=[wD["w_e2"]], writes=[a2])
                S.op("act", lambda e: e.copy(out=b1[:], in_=a1[:]), reads=[a1], writes=[b1])
                S.op("dve", lambda e: e.tensor_copy(out=b3[:], in_=a3[:]), reads=[a3], writes=[b3])
                S.op("act", lambda e: e.copy(out=W2b[:, j, :], in_=a2[:]), reads=[a2], writes=[W2b])
                for kc in range(8):
                    S.op("pe", lambda e: e.matmul(p1[:], lhsT=b1[:, kc, :], rhs=u2blk[:, kc, :], start=(kc == 0), stop=(kc == 7)), reads=[b1, u2blk], writes=[p1], pe_acc=(kc > 0))
                for kc in range(8):
                    S.op("pe", lambda e: e.matmul(p3[:], lhsT=b3[:, kc, :], rhs=u2blk[:, kc, :], start=(kc == 0), stop=(kc == 7)), reads=[b3, u2blk], writes=[p3], pe_acc=(kc > 0))
                S.op("act", lambda e: e.activation(out=sg_[:], in_=p1[:], func=AF.Silu), reads=[p1], writes=[sg_])
                S.op("dve", lambda e: e.tensor_tensor(out=hT[:, j, :], in0=p3[:], in1=sg_[:], op=ALU.mult), reads=[p3, sg_], writes=[hT])
            for tt in range(4):
                n = bk * 4 + tt
                for hf in range(2):
                    p = rr(py, tt * 2 + hf)
                    for j in range(22):
                        S.op("pe", lambda e: e.matmul(p[:], lhsT=hT[:, j, tt * 128:(tt + 1) * 128], rhs=W2b[:, j, hf * 512:(hf + 1) * 512], start=(j == 0), stop=(j == 21)),
                             reads=[hT, W2b], writes=[p], pe_acc=(j > 0))
                    S.op("dve", lambda e: e.scalar_tensor_tensor(out=acc[:, tt, hf * 512:(hf + 1) * 512], in0=p[:], scalar=GM[:, n, ex_:ex_ + 1],
                                                                 in1=acc[:, tt, hf * 512:(hf + 1) * 512], op0=ALU.mult, op1=ALU.add), reads=[p, GM, acc], writes=[acc])
        for tt in range(4):
            rows = slice(t0 + tt * 128, t0 + (tt + 1) * 128)
            S.dma("sp", lambda e: e.dma_start(out=h1t[:], in_=h1scr[rows, :]), reads=[h1scr], writes=[h1t])
            S.op("pool", lambda e: e.tensor_tensor(out=acc[:, tt, :], in0=acc[:, tt, :], in1=gt2[:], op=ALU.mult), reads=[acc, gt2], writes=[acc])
            S.op("dve", lambda e: e.scalar_tensor_tensor(out=h1t[:], in0=h1t[:], scalar=float(ALPHA), in1=acc[:, tt, :], op0=ALU.mult, op1=ALU.add), reads=[h1t, acc], writes=[h1t])
            ln_rows(h1t, stats, mv, rstd, LN_EPS)
            S.op("pool", lambda e: e.tensor_tensor(out=h1t[:], in0=h1t[:], in1=l2g[:], op=ALU.mult), reads=[h1t, l2g], writes=[h1t])
            S.op("pool", lambda e: e.tensor_tensor(out=h1t[:], in0=h1t[:], in1=l2b[:], op=ALU.add), reads=[h1t, l2b], writes=[h1t])
            S.dma("sp", lambda e: e.dma_start(out=out_d[rows, :], in_=h1t[:]), reads=[h1t], writes=[out_d])
    C.release(mF)


def host_consts():
    ident = np.eye(128, dtype=np.float32)
    t = np.arange(T)
    pos = np.stack([t // 64, t % 64], -1).astype(np.float32)
    nf = 16
    inv = np.power(np.float32(10000.0), -np.arange(nf, dtype=np.float32) / nf).astype(np.float32)
    ang = pos[:, :, None] * inv
    rope = np.stack([np.cos(ang), np.sin(ang)], 1).astype(np.float32)
    i = np.arange(128)
    US = (i[:, None] < i[None, :]); LS = (i[:, None] > i[None, :]); UI = (i[:, None] <= i[None, :]); LI = (i[:, None] >= i[None, :])
    SEL127 = np.broadcast_to((i == 127)[:, None], (128, 128)); SEL0 = np.broadcast_to((i == 0)[:, None], (128, 128))
    tri = np.stack([US, LS, UI, LI, SEL127, SEL0]).astype(np.float32)
    return {"ident": ident, "rope": rope, "tri": tri}


def na_bias_table(rpb):
    qi = np.arange(8)[:, None, None, None]
    p = np.arange(128)[None, :, None, None]
    jt = np.arange(4)[None, None, :, None]
    j = np.arange(64)[None, None, None, :]
    kr = 2 * jt + p // 64
    c = p % 64
    cs = np.clip(j - 8, 0, 48)
    valid = (c >= cs) & (c < cs + 16)
    ro = np.broadcast_to(kr - qi + 7, (8, 128, 4, 64))
    co = np.clip(np.broadcast_to(c - j + 15, (8, 128, 4, 64)), 0, 30)
    g = rpb[:, ro, co]
    g = np.where(np.broadcast_to(valid, g.shape), g, np.float32(-30000.0))
    return np.ascontiguousarray(g.transpose(1, 2, 0, 3, 4).reshape(8, 128, 8, 256)).astype(np.float32)


def make_in_maps(inputs, consts):
    maps = []
    nab = na_bias_table(inputs["rpb"][0])
    for b in range(4):
        m = dict(consts)
        m["x"] = np.ascontiguousarray(np.concatenate([inputs["x"][b], inputs["ctx"][b]], 0))
        m["cc"] = np.ascontiguousarray(np.stack([inputs["c"][b], inputs["c_ctx"]], 0))
        m["w_mod"] = inputs["w_mod"][0]; m["b_mod"] = inputs["b_mod"][0]
        m["w_in"] = inputs["w_in"][0]
        m["nab"] = nab
        for k in ("mu_prev", "mu_next", "k_k", "k_a", "gn_w", "gn_b", "w0", "a0", "g_up"):
            m[k] = np.ascontiguousarray(inputs[k][0])
        m["r_k"] = np.ascontiguousarray(inputs["r_k"][0].reshape(512))
        for k in ("w_pa", "w_pr", "w_o", "w_router", "ln1_g", "ln1_b", "ln2_g", "ln2_b", "w_e1", "w_e3", "w_e2"):
            m[k] = inputs[k][0]
        m["w_up"] = np.ascontiguousarray(inputs["w_up"][0].reshape(128, 512))
        m["a_up"] = np.ascontiguousarray(inputs["a_up"][0].reshape(128, 512))
        maps.append(m)
    return maps


def kernel(**inputs):
    inputs = {k: np.asarray(v) for k, v in inputs.items()}
    nc = build_program()
    maps = make_in_maps(inputs, host_consts())
    res = run_bass_kernel_spmd(nc, maps, core_ids=list(range(4)))
    return np.stack([r["out"] for r in res.results], 0).astype(np.float32)
```
